# Optimizing a Trainium2 kernel written in Bass

```python
import jax, jax.numpy as jnp
from jax import lax
import numpy as np

D_MODEL = 2048
BATCH = 1
SEQ = 8192
DEPTH = 1

MEM_LEN = 256
CHUNK = 64
RET_HEADS = 4
RET_DK = 256
RET_DV = 256
GLA_HEADS = 4
GLA_DK = 128
GLA_DV = 256
GLA_LOWRANK = 16
GLA_TAU = 16.0
MIX_WIDTH = RET_HEADS * RET_DV + GLA_HEADS * GLA_DV
ROPE_BASE = 10000.0
MEM_HEADS = 4
MEM_HEAD_DIM = D_MODEL // MEM_HEADS
N_GROUPS = 4
EXPERTS_PER_GROUP = 8
N_EXPERTS = N_GROUPS * EXPERTS_PER_GROUP
EXPERT_FF = 512
FINE_TOP_K = 2
MOE_BLOCK = 128
LN_EPS = 1e-5
DEEPNORM_ALPHA = (2 * DEPTH) ** 0.25
DEEPNORM_BETA = (8 * DEPTH) ** -0.25
IN_SIZES = (RET_HEADS * RET_DK, RET_HEADS * RET_DK, RET_HEADS * RET_DV, RET_HEADS * RET_DV,
            GLA_HEADS * GLA_DK, GLA_HEADS * GLA_DK, GLA_HEADS * GLA_DV, GLA_HEADS * GLA_DV,
            GLA_LOWRANK)
IN_WIDTH = sum(IN_SIZES)

kernel_name = "hymba_retnet_gla_hmoe_deepnorm"


def layer_norm(x, g, b):
    xf = x.astype(jnp.float32)
    mu = xf.mean(-1, keepdims=True)
    var = jnp.mean(jnp.square(xf - mu), -1, keepdims=True)
    y = (xf - mu) * lax.rsqrt(var + LN_EPS) * g.astype(jnp.float32) + b.astype(jnp.float32)
    return y.astype(x.dtype)


def head_group_norm(t):
    mu = t.mean(-1, keepdims=True)
    var = jnp.mean(jnp.square(t - mu), -1, keepdims=True)
    return (t - mu) * lax.rsqrt(var + LN_EPS)


def head_rms_norm(t, g):
    return t * lax.rsqrt(jnp.mean(jnp.square(t), -1, keepdims=True) + LN_EPS) * g.astype(jnp.float32)


def rotary(t, positions):
    half = t.shape[-1] // 2
    inv_freq = ROPE_BASE ** (-jnp.arange(half, dtype=jnp.float32) / half)
    ang = positions.astype(jnp.float32)[:, :, None, None] * inv_freq
    cos, sin = jnp.cos(ang), jnp.sin(ang)
    t1, t2 = t[..., :half], t[..., half:]
    return jnp.concatenate([t1 * cos - t2 * sin, t1 * sin + t2 * cos], axis=-1)


def to_chunks(t):
    B, S, H, d = t.shape
    return t.reshape(B, S // CHUNK, CHUNK, H, d).transpose(1, 0, 3, 2, 4)


def from_chunks(t):
    N, B, H, C, d = t.shape
    return t.transpose(1, 0, 3, 2, 4).reshape(B, N * C, H, d)


def retention_chunkwise(q, k, v):
    B, S, H, dk = q.shape
    dv = v.shape[-1]
    log_gamma = jnp.log1p(-jnp.exp2(-5.0 - jnp.arange(H, dtype=jnp.float32)))
    n = jnp.arange(CHUNK, dtype=jnp.float32)
    rel = n[:, None] - n[None, :]
    causal = rel >= 0
    decay_intra = jnp.where(causal, jnp.exp(log_gamma[:, None, None] * jnp.maximum(rel, 0.0)), 0.0)
    decay_q = jnp.exp(log_gamma[:, None] * (n + 1.0))[..., None]
    decay_k = jnp.exp(log_gamma[:, None] * (CHUNK - 1.0 - n))[..., None]
    decay_chunk = jnp.exp(log_gamma * CHUNK)[:, None, None]

    def step(R, inp):
        qc, kc, vc = inp
        scores = jnp.einsum('bhid,bhjd->bhij', qc, kc) * decay_intra
        o = (jnp.einsum('bhij,bhje->bhie', scores, vc)
             + jnp.einsum('bhid,bhde->bhie', qc * decay_q, R))
        R = decay_chunk * R + jnp.einsum('bhjd,bhje->bhde', kc * decay_k, vc)
        return R, o

    R0 = jnp.zeros((B, H, dk, dv), jnp.float32)
    _, o = lax.scan(step, R0, (to_chunks(q), to_chunks(k), to_chunks(v)))
    return from_chunks(o)


def gla_chunkwise(q, k, v, log_a):
    B, S, H, dk = q.shape
    dv = v.shape[-1]
    causal = jnp.tril(jnp.ones((CHUNK, CHUNK), bool))[:, :, None]

    def step(St, inp):
        qc, kc, vc, ac = inp
        b = jnp.cumsum(ac, axis=-2)
        diff = b[:, :, :, None, :] - b[:, :, None, :, :]
        gate = jnp.where(causal, jnp.exp(jnp.where(causal, diff, 0.0)), 0.0)
        scores = jnp.einsum('bhid,bhjd,bhijd->bhij', qc, kc, gate)
        o = (jnp.einsum('bhij,bhje->bhie', scores, vc)
             + jnp.einsum('bhid,bhde->bhie', qc * jnp.exp(b), St))
        b_last = b[:, :, -1:, :]
        St = (jnp.exp(b_last[:, :, 0, :])[..., None] * St
              + jnp.einsum('bhjd,bhje->bhde', kc * jnp.exp(b_last - b), vc))
        return St, o

    S0 = jnp.zeros((B, H, dk, dv), jnp.float32)
    _, o = lax.scan(step, S0, (to_chunks(q), to_chunks(k), to_chunks(v), to_chunks(log_a)))
    return from_chunks(o)


def hybrid_mixer(x, positions, w_in, w_gla_a2, b_gla_a, g_gla_norm, w_mix_out):
    B, S, _ = x.shape
    h = x @ w_in
    split_at = [int(i) for i in np.cumsum(IN_SIZES)[:-1]]
    rq, rk, rv, rg, gq, gk, gv, gg, glr = jnp.split(h, split_at, axis=-1)
    f32 = jnp.float32
    rq = rotary(rq.astype(f32).reshape(B, S, RET_HEADS, RET_DK), positions)
    rk = rotary(rk.astype(f32).reshape(B, S, RET_HEADS, RET_DK), positions) * RET_DK ** -0.5
    rv = rv.astype(f32).reshape(B, S, RET_HEADS, RET_DV)
    ret = head_group_norm(retention_chunkwise(rq, rk, rv)).reshape(B, S, RET_HEADS * RET_DV)
    ret = jax.nn.silu(rg.astype(f32)) * ret
    gq = gq.astype(f32).reshape(B, S, GLA_HEADS, GLA_DK) * GLA_DK ** -0.5
    gk = gk.astype(f32).reshape(B, S, GLA_HEADS, GLA_DK)
    gv = gv.astype(f32).reshape(B, S, GLA_HEADS, GLA_DV)
    log_a = (jax.nn.log_sigmoid((glr @ w_gla_a2 + b_gla_a).astype(f32)) / GLA_TAU
             ).reshape(B, S, GLA_HEADS, GLA_DK)
    gla = head_rms_norm(gla_chunkwise(gq, gk, gv, log_a), g_gla_norm).reshape(B, S, GLA_HEADS * GLA_DV)
    gla = jax.nn.silu(gg.astype(f32)) * gla
    mixed = jnp.concatenate([ret, gla], axis=-1).astype(x.dtype)
    return mixed @ w_mix_out


def memory_cross_attention(x, mem, w_mq, w_mk, w_mv, w_mo):
    B, S, _ = x.shape
    M = mem.shape[1]
    q = (x @ w_mq).reshape(B, S, MEM_HEADS, MEM_HEAD_DIM)
    k = (mem @ w_mk).reshape(B, M, MEM_HEADS, MEM_HEAD_DIM)
    v = (mem @ w_mv).reshape(B, M, MEM_HEADS, MEM_HEAD_DIM)
    s = jnp.einsum('bshd,bmhd->bhsm', q, k).astype(jnp.float32) * MEM_HEAD_DIM ** -0.5
    p = jax.nn.softmax(s, axis=-1).astype(v.dtype)
    o = jnp.einsum('bhsm,bmhd->bshd', p, v).reshape(B, S, D_MODEL)
    return o @ w_mo


def hierarchical_moe(x, w_route_group, b_route_group, w_route_expert, b_route_expert,
                     w_exp_gate, w_exp_up, w_exp_down):
    B, S, D = x.shape
    T = B * S
    xf = x.reshape(T, D)
    f32 = jnp.float32
    pg = jax.nn.softmax((xf @ w_route_group + b_route_group).astype(f32), axis=-1)
    grp = jnp.argmax(pg, axis=-1)
    pg_sel = jnp.take_along_axis(pg, grp[:, None], axis=-1)
    fine = (xf @ w_route_expert).reshape(T, N_GROUPS, EXPERTS_PER_GROUP) + b_route_expert
    fine_sel = jnp.take_along_axis(fine, grp[:, None, None], axis=1)[:, 0].astype(f32)
    top_p, top_i = lax.top_k(jax.nn.softmax(fine_sel, axis=-1), FINE_TOP_K)
    gate = pg_sel * top_p / jnp.sum(top_p, axis=-1, keepdims=True)
    expert = grp[:, None] * EXPERTS_PER_GROUP + top_i
    A = T * FINE_TOP_K
    flat_e = expert.reshape(A)
    flat_t = jnp.repeat(jnp.arange(T, dtype=jnp.int32), FINE_TOP_K)
    flat_w = gate.reshape(A)
    order = jnp.argsort(flat_e)
    se = flat_e[order]
    counts = jnp.bincount(flat_e, length=N_EXPERTS)
    padded = (counts + MOE_BLOCK - 1) // MOE_BLOCK * MOE_BLOCK
    ends = jnp.cumsum(padded)
    pstart = ends - padded
    start = jnp.cumsum(counts) - counts
    dest = pstart[se] + jnp.arange(A) - start[se]
    P = A + N_EXPERTS * MOE_BLOCK
    nb = P // MOE_BLOCK
    buf_t = jnp.full((P,), T, jnp.int32).at[dest].set(flat_t[order])
    buf_w = jnp.zeros((P,), xf.dtype).at[dest].set(flat_w[order].astype(xf.dtype))
    block_e = jnp.minimum(jnp.searchsorted(ends, jnp.arange(nb) * MOE_BLOCK, side='right'),
                          N_EXPERTS - 1)
    x_pad = jnp.concatenate([xf, jnp.zeros((1, D), xf.dtype)], axis=0)
    xb = x_pad[buf_t].reshape(nb, MOE_BLOCK, D)

    def expert_block(args):
        xblk, e = args
        hid = jax.nn.silu(xblk @ w_exp_gate[e]) * (xblk @ w_exp_up[e])
        return hid @ w_exp_down[e]

    yb = lax.map(expert_block, (xb, block_e)).reshape(P, D)
    out = jnp.zeros((T + 1, D), xf.dtype).at[buf_t].add(yb * buf_w[:, None])[:T]
    return out.reshape(B, S, D)


def setup_inputs(seed: int = 0) -> dict:
    key = jax.random.key(seed)
    ks = jax.random.split(key, 26)
    nrm = jax.random.normal
    L, D = DEPTH, D_MODEL
    offset = jax.random.randint(ks[2], (BATCH, 1), 0, 1024, dtype=jnp.int32)
    positions = offset + jnp.arange(SEQ, dtype=jnp.int32)[None, :]
    return {
        "x": nrm(ks[0], (BATCH, SEQ, D), jnp.float32),
        "mem": nrm(ks[1], (BATCH, MEM_LEN, D), jnp.float32),
        "positions": positions,
        "w_in": nrm(ks[3], (L, D, IN_WIDTH)) * D ** -0.5,
        "w_gla_a2": nrm(ks[4], (L, GLA_LOWRANK, GLA_HEADS * GLA_DK)) * GLA_LOWRANK ** -0.5,
        "b_gla_a": 0.1 * nrm(ks[5], (L, GLA_HEADS * GLA_DK)),
        "g_gla_norm": 1.0 + 0.02 * nrm(ks[6], (L, GLA_DV)),
        "w_mix_out": nrm(ks[7], (L, MIX_WIDTH, D)) * MIX_WIDTH ** -0.5 * DEEPNORM_BETA,
        "ln1_g": 1.0 + 0.02 * nrm(ks[8], (L, D)),
        "ln1_b": 0.02 * nrm(ks[9], (L, D)),
        "w_mq": nrm(ks[10], (L, D, D)) * D ** -0.5,
        "w_mk": nrm(ks[11], (L, D, D)) * D ** -0.5,
        "w_mv": nrm(ks[12], (L, D, D)) * D ** -0.5,
        "w_mo": nrm(ks[13], (L, D, D)) * D ** -0.5 * DEEPNORM_BETA,
        "ln2_g": 1.0 + 0.02 * nrm(ks[14], (L, D)),
        "ln2_b": 0.02 * nrm(ks[15], (L, D)),
        "w_route_group": nrm(ks[16], (L, D, N_GROUPS)) * D ** -0.5,
        "b_route_group": 0.01 * nrm(ks[17], (L, N_GROUPS)),
        "w_route_expert": nrm(ks[18], (L, D, N_EXPERTS)) * D ** -0.5,
        "b_route_expert": 0.01 * nrm(ks[19], (L, N_GROUPS, EXPERTS_PER_GROUP)),
        "w_exp_gate": nrm(ks[20], (L, N_EXPERTS, D, EXPERT_FF)) * D ** -0.5,
        "w_exp_up": nrm(ks[21], (L, N_EXPERTS, D, EXPERT_FF)) * D ** -0.5,
        "w_exp_down": nrm(ks[22], (L, N_EXPERTS, EXPERT_FF, D)) * EXPERT_FF ** -0.5 * DEEPNORM_BETA,
        "ln3_g": 1.0 + 0.02 * nrm(ks[23], (L, D)),
        "ln3_b": 0.02 * nrm(ks[24], (L, D)),
    }


def reference(x, mem, positions, w_in, w_gla_a2, b_gla_a, g_gla_norm, w_mix_out, ln1_g, ln1_b,
              w_mq, w_mk, w_mv, w_mo, ln2_g, ln2_b, w_route_group, b_route_group,
              w_route_expert, b_route_expert, w_exp_gate, w_exp_up, w_exp_down, ln3_g, ln3_b):
    h = x
    for l in range(DEPTH):
        mix = hybrid_mixer(h, positions, w_in[l], w_gla_a2[l], b_gla_a[l], g_gla_norm[l], w_mix_out[l])
        h = layer_norm(DEEPNORM_ALPHA * h + mix, ln1_g[l], ln1_b[l])
        cross = memory_cross_attention(h, mem, w_mq[l], w_mk[l], w_mv[l], w_mo[l])
        h = layer_norm(DEEPNORM_ALPHA * h + cross, ln2_g[l], ln2_b[l])
        ffn = hierarchical_moe(h, w_route_group[l], b_route_group[l], w_route_expert[l],
                               b_route_expert[l], w_exp_gate[l], w_exp_up[l], w_exp_down[l])
        h = layer_norm(DEEPNORM_ALPHA * h + ffn, ln3_g[l], ln3_b[l])
    return h
```

```python
import math
from contextlib import ExitStack
import numpy as np
import concourse.bass as bass
import concourse.mybir as mybir
from concourse.bass_utils import run_bass_kernel_spmd

F32 = mybir.dt.float32
BF16 = mybir.dt.bfloat16
I32 = mybir.dt.int32
AF = mybir.ActivationFunctionType
ALU = mybir.AluOpType
AX = mybir.AxisListType

NCORE = 8
D = 2048
SEQ = 8192
TOK = SEQ // NCORE
NT = TOK // 128
NPRE = (NCORE - 1) * NT
KC = D // 128
EPS = 1e-5
ALPHA = 2.0 ** 0.25
NEXP = 32
CAP = 128
TWO_PI = 2.0 * math.pi
GAMMAS = [1.0 - 2.0 ** (-5.0 - h) for h in range(4)]

DEBUG = False
STOP = None


def configure(seq=8192, stop=None, debug=False):
    global SEQ, TOK, NT, NPRE, STOP, DEBUG
    SEQ = seq
    TOK = SEQ // NCORE
    NT = TOK // 128
    NPRE = (NCORE - 1) * NT
    STOP = stop
    DEBUG = debug
    _CACHE.clear()


_CACHE = {}
DECLARED = []


class Tile:
    def __init__(self, name="", excl=False):
        self.name = name
        self.w = []
        self.r = []
        self.excl = excl


class DSem:
    def __init__(self, nc, es, name):
        self.sem = es.enter_context(nc.semaphore(name))
        self.count = 0


class Eng:
    def __init__(self, nc, es, e, name):
        self.e = e
        self.sem = es.enter_context(nc.semaphore(name))
        self.n = 0
        self.seen = {}

    def wait(self, tks):
        best = {}
        for sem, val in tks:
            if val > best.get(sem, 0):
                best[sem] = val
        for sem, val in best.items():
            if self.seen.get(sem, 0) < val:
                self.e.wait_ge(sem, val)
                self.seen[sem] = val

    def tick(self, ins):
        self.n += 1
        ins.then_inc(self.sem, 1)
        return (self.sem, self.n)


def op(E, fn, reads=(), writes=()):
    tks = []
    for t in reads:
        tks += t.w
        if t.excl:
            tks += [k for k in t.r if k[0] is not E.sem]
    for t in writes:
        tks += t.w
        tks += t.r
    E.wait(tks)
    ins = fn()
    tk = E.tick(ins)
    for t in reads:
        t.r = [k for k in t.r if k[0] is not E.sem] + [tk]
    for t in writes:
        t.w = [tk]
        t.r = []
    return tk


def mmgroup(E, fns, reads=(), writes=()):
    tks = []
    for t in reads:
        tks += t.w
        if t.excl:
            tks += [k for k in t.r if k[0] is not E.sem]
    for t in writes:
        tks += t.w
        tks += t.r
    E.wait(tks)
    ins = None
    for f in fns:
        ins = f()
    tk = E.tick(ins)
    for t in reads:
        t.r = [k for k in t.r if k[0] is not E.sem] + [tk]
    for t in writes:
        t.w = [tk]
        t.r = []
    return tk


def dma(Q, ds, out, in_, dst=None, src=None):
    tks = []
    dsts = [] if dst is None else (list(dst) if isinstance(dst, (list, tuple)) else [dst])
    for d in dsts:
        tks += d.w + d.r
    if src is not None:
        tks += src.w
    Q.wait(tks)
    ds.count += 16
    Q.e.dma_start(out=out, in_=in_).then_inc(ds.sem, 16)
    tk = (ds.sem, ds.count)
    for d in dsts:
        d.w = [tk]
        d.r = []
    if src is not None:
        src.r = [k for k in src.r if k[0] is not ds.sem] + [tk]
    return tk


def _consts():
    c = {}
    i = np.arange(128)
    DT = np.zeros((4, 128, 128), np.float64)
    for h, g in enumerate(GAMMAS):
        rel = i[None, :] - i[:, None]
        DT[h] = np.where(rel >= 0, np.exp(np.log(g) * np.maximum(rel, 0)), 0.0) / 16.0
    c["DT"] = DT.transpose(1, 0, 2).reshape(128, 512)
    c["CAUS"] = (i[None, :] >= i[:, None]).astype(np.float64)
    c["DQ"] = np.tile(np.stack([np.exp(np.log(g) * (i + 1.0)) for g in GAMMAS]).reshape(1, 512), (128, 1))
    dk = np.zeros((128, 8))
    for h, g in enumerate(GAMMAS):
        dk[:, 2 * h] = np.exp(np.log(g) * (127.0 - i)) / 16.0
    c["DK"] = dk
    half = np.arange(128, dtype=np.float32)
    invf = (np.float32(10000.0) ** (-half / np.float32(128.0))).astype(np.float32)
    c["INVF"] = np.tile(invf.reshape(1, 128), (128, 1))
    c["UT"] = -(i[:, None] <= i[None, :]).astype(np.float64) / 16.0
    c["UT2"] = -(i[:, None] > i[None, :]).astype(np.float64) / 16.0
    c["NCOL"] = np.full((128, 1), -1.0 / 16.0)
    c["IOTA"] = np.tile(i.reshape(1, 128).astype(np.float64), (128, 1))
    c["TRIS"] = (i[:, None] < i[None, :]).astype(np.float64)
    c["ONES"] = np.ones((128, 128))
    pers = ["IOTA", "TRIS", "ONES"]
    offs = {}
    cols1, cols2 = [], []
    o = 0
    for k, v in c.items():
        if k in pers:
            continue
        offs[k] = (1, o, v.shape[1])
        o += v.shape[1]
        cols1.append(v.astype(np.float32))
    o = 0
    for k in pers:
        v = c[k]
        offs[k] = (0, o, v.shape[1])
        o += v.shape[1]
        cols2.append(v.astype(np.float32))
    return (np.ascontiguousarray(np.concatenate(cols2, axis=1)), np.ascontiguousarray(np.concatenate(cols1, axis=1)), offs)


CONST_ARR, CONST1_ARR, COFF = _consts()

A_RET = [(h * 512, 512) for h in range(4)]
A_GLA = [(2048 + h * 384, 384) for h in range(4)]
A_LR = (3584, 16)
A_W = 3600
B_RET = [(3600 + h * 512, 512) for h in range(4)]
B_GLA = [(5648 + h * 384, 384) for h in range(4)]


def _perm_w_in(w_in):
    rq, rk, rv, rg, gq, gk, gv, gg, glr = np.split(w_in, np.cumsum([1024, 1024, 1024, 1024, 512, 512, 1024, 1024])[:8], axis=1)
    cols = []
    for h in range(4):
        cols += [rk[:, h * 256:(h + 1) * 256], rv[:, h * 256:(h + 1) * 256]]
    for h in range(4):
        cols += [gk[:, h * 128:(h + 1) * 128], gv[:, h * 256:(h + 1) * 256]]
    cols += [glr]
    for h in range(4):
        cols += [rq[:, h * 256:(h + 1) * 256], rg[:, h * 256:(h + 1) * 256]]
    for h in range(4):
        cols += [gq[:, h * 128:(h + 1) * 128], gg[:, h * 256:(h + 1) * 256]]
    return np.ascontiguousarray(np.concatenate(cols, axis=1))


def build_program():
    nc = bass.Bass("TRN2", target_bir_lowering=False)
    es = ExitStack()

    del DECLARED[:]
    need = {"1b": 0, "2": 1, "3": 2, None: 3}.get(STOP, 0)
    lvl = {"w_mix": 1, "ln": 1, "xown": 1, "w_mq": 2, "w_mk": 2, "w_mv": 2, "w_mo": 2, "memT": 2,
           "wroute": 3, "broute": 3, "w_eg": 3, "w_eu": 3, "w_ed": 3}

    def din(name, shape, dt=F32):
        if lvl.get(name, 0) > need:
            return None
        DECLARED.append(name)
        return nc.dram_tensor(name, list(shape), dt, kind="ExternalInput").ap()

    xTp = din("xTp", [NPRE + NT, 128, D])
    xown = din("xown", [TOK, D])
    posT = din("posT", [128, NPRE + NT], I32)
    consts_d = din("consts", list(CONST_ARR.shape))
    consts1_d = din("consts1", list(CONST1_ARR.shape))
    w_in = din("w_in", [D, 7184])
    wa2b_d = din("wa2b", [17, 512])
    gnorm_d = din("gnorm", [1, 256])
    w_mix = din("w_mix", [D, D])
    w_mq = din("w_mq", [D, D])
    w_mk = din("w_mk", [D, D])
    w_mv = din("w_mv", [D, D])
    w_mo = din("w_mo", [D, D])
    memT_d = din("memT", [128, KC * 256])
    ln_d = din("ln", [6, D])
    wroute_d = din("wroute", [D, 36])
    broute_d = din("broute", [1, 36])
    w_eg = din("w_eg", [NEXP, D, 512])
    w_eu = din("w_eu", [NEXP, D, 512])
    w_ed = din("w_ed", [NEXP, 512, D])
    out_d = nc.dram_tensor("out", [TOK, D], F32, kind="ExternalOutput").ap()
    if DEBUG:
        dbg_mixT = nc.dram_tensor("dbg_mixT", [128, KC * TOK], BF16, kind="ExternalOutput").ap()
        dbg_h1 = nc.dram_tensor("dbg_h1", [TOK, D], F32, kind="ExternalOutput").ap()
        dbg_h2 = nc.dram_tensor("dbg_h2", [TOK, D], F32, kind="ExternalOutput").ap()

    PE = Eng(nc, es, nc.tensor, "s_pe")
    DVE = Eng(nc, es, nc.vector, "s_dve")
    ACT = Eng(nc, es, nc.scalar, "s_act")
    POOL = Eng(nc, es, nc.gpsimd, "s_pool")
    SP = Eng(nc, es, nc.sync, "s_sp")
    ENGS = [PE, DVE, ACT, POOL, SP]
    all_dsems = []

    def dsem(name):
        d = DSem(nc, es, name)
        all_dsems.append(d)
        return d

    def barrier():
        tks = [(E.sem, E.n) for E in ENGS if E.n > 0] + [(d.sem, d.count) for d in all_dsems if d.count > 0]
        for E in ENGS:
            E.wait(tks)

    class Arena:
        def __init__(self, base, limit):
            self.base, self.top, self.limit = base, base, limit

    class Scope:
        def __init__(self, arena):
            self.arena = arena
            self.mark = arena.top

        def close(self):
            self.arena.top = self.mark

    big_holder = []

    d_dbg = dsem("d_dbg") if DEBUG else None

    def finish():
        if DEBUG:
            SP.wait([(d_dbg.sem, d_dbg.count)])
        barrier()
        es.close()
        return nc

    def sb(stack, name, shape, dt):
        if not isinstance(stack, Scope):
            return stack.enter_context(nc.sbuf_tensor(name, list(shape), dt))
        ar = stack.arena
        esz = 2 if dt == BF16 else 4
        n = 1
        for d_ in shape[1:]:
            n *= d_
        nbytes = (n * esz + 63) // 64 * 64
        off = ar.top
        assert off + nbytes <= ar.limit, (name, off, nbytes, ar.limit)
        ar.top = off + nbytes
        v = big_holder[0][0:shape[0], off // 4:(off + n * esz + 3) // 4]
        if dt != F32:
            v = v.bitcast(dt)
            v = v[:, 0:n]
        if len(shape) == 3:
            v = v.rearrange("p (a b) -> p a b", a=shape[1])
        elif len(shape) == 4:
            v = v.rearrange("p (a b c) -> p a b c", a=shape[1], b=shape[2])
        return v

    CT = sb(es, "CT", CONST_ARR.shape, F32)
    CTb = sb(es, "CTb", [128, 128 * 3], BF16)
    ident = sb(es, "ident", [128, 128], BF16)
    posf = sb(es, "posf", [128, NPRE + NT], F32)
    posi = sb(es, "posi", [128, NPRE + NT], I32)
    LO_1A = 115200
    LO_AFTER = 114688
    bigw = (nc.sbuf_bytes_remaining - 256) // 64 * 16
    big_holder.append(es.enter_context(nc.sbuf_tensor("big", [128, bigw], F32)))
    lo = Arena(0, LO_1A)
    hi = Arena(LO_1A, bigw * 4)
    s1 = Scope(hi)
    CT1 = sb(s1, "CT1", CONST1_ARR.shape, F32)
    wa2b = sb(s1, "wa2bs", [17, 512], BF16)
    gnb = sb(s1, "gnb", [128, 256], F32)
    Rst = sb(s1, "Rst", [128, 4, 512], F32)
    Rb = sb(s1, "Rb", [128, 4, 512], BF16)
    Sst = sb(s1, "Sst", [128, 4, 256], F32)
    Sb = sb(s1, "Sb", [128, 4, 256], BF16)
    NSLOT = 3
    slot_t = [Tile(f"slot{i}") for i in range(NSLOT)]
    slot_ds = [dsem(f"d_slot{i}") for i in range(NSLOT)]

    Fp = [es.enter_context(nc.psum_tensor(f"F{i}", [128, 512], F32)) for i in range(6)]
    Ft = [Tile(f"F{i}", excl=True) for i in range(6)]
    Tp = [es.enter_context(nc.psum_tensor(f"T{i}", [128, 1024], BF16)) for i in range(2)]
    Tt = [Tile(f"T{i}", excl=True) for i in range(2)]

    t_const = Tile("const")
    t_ident = Tile("ident")
    t_pos = Tile("pos")
    t_R = [Tile(f"R{h}") for h in range(4)]
    t_Rb = [Tile(f"Rb{h}") for h in range(4)]
    t_S = [Tile(f"S{h}") for h in range(4)]
    t_Sb = [Tile(f"Sb{h}") for h in range(4)]
    t_A = Tile("ACT_A")
    t_B = Tile("ACT_B")
    t_Bt = [Tile(f"ACT_B{t}") for t in range(NT)]
    t_At = [Tile(f"ACT_A{t}") for t in range(NT)]

    def C(name, lo=0, hi=None):
        which, o, w = COFF[name]
        hi = w if hi is None else hi
        return (CT1 if which else CT)[:, o + lo:o + hi]

    d_c = dsem("d_const")
    dma(SP, d_c, CT[:], consts_d)
    dma(SP, d_c, CT1[:], consts1_d)
    dma(SP, d_c, posi[:], posT)
    dma(SP, d_c, gnb[:], gnorm_d.partition_broadcast(128))
    d_c2 = dsem("d_const2")
    dma(POOL, d_c2, wa2b[:], wa2b_d)
    t_const.w = [(d_c.sem, d_c.count), (d_c2.sem, d_c2.count)]
    op(POOL, lambda: nc.gpsimd.memset(ident[:], 0.0), writes=[t_ident])
    op(POOL, lambda: nc.gpsimd.affine_select(out=ident[:], in_=ident[:], pattern=[[-1, 128]], compare_op=ALU.not_equal,
                                              fill=1.0, base=0, channel_multiplier=1), reads=[t_ident], writes=[t_ident])
    op(DVE, lambda: nc.vector.tensor_copy(out=posf[:], in_=posi[:]), reads=[t_const], writes=[t_pos])
    t_ctb = Tile("ctb")
    op(DVE, lambda: nc.vector.tensor_copy(out=CTb[:, 0:128], in_=C("ONES")), reads=[t_const], writes=[t_ctb])
    op(DVE, lambda: nc.vector.tensor_copy(out=CTb[:, 128:256], in_=C("TRIS")), reads=[t_const], writes=[t_ctb])
    for h in range(4):
        op(DVE, lambda h=h: nc.vector.memset(Rst[:, h, :], 0.0), writes=[t_R[h]])
        op(DVE, lambda h=h: nc.vector.memset(Sst[:, h, :], 0.0), writes=[t_S[h]])
        op(POOL, lambda h=h: nc.gpsimd.memset(Rb[:, h, :], 0.0), writes=[t_Rb[h]])
        op(POOL, lambda h=h: nc.gpsimd.memset(Sb[:, h, :], 0.0), writes=[t_Sb[h]])

    cs = sb(s1, "cs", [128, 2, 128], F32)
    t_cs = Tile("cs")
    tr_a = sb(s1, "tr_a", [128, 128], F32)
    tr_b = sb(s1, "tr_b", [128, 128], F32)
    tr_i = sb(s1, "tr_i", [128, 128], I32)
    t_tr = Tile("tr")
    rotA = sb(s1, "rotA", [128, 256], F32)
    rotB = sb(s1, "rotB", [128, 256], F32)
    t_rot = Tile("rot")
    kbuf = [sb(s1, f"kbuf{i}", [128, 256], BF16) for i in range(2)]
    t_k = [Tile(f"k{i}") for i in range(2)]
    vbuf = [sb(s1, f"vbuf{i}", [128, 256], BF16) for i in range(2)]
    t_v = [Tile(f"v{i}") for i in range(2)]
    vhat = [sb(s1, f"vhat{i}", [128, 256], BF16) for i in range(2)]
    t_vh = [Tile(f"vh{i}") for i in range(2)]
    glr_b = sb(s1, "glr_b", [128, 16], BF16)
    t_glr = Tile("glr")
    glrT = sb(s1, "glrT", [17, 128], BF16)
    t_glrT = Tile("glrT")
    Lg = sb(s1, "Lg", [128, 512], F32)
    t_L = Tile("L")
    etmp = sb(s1, "etmp", [128, 512], F32)
    t_et = Tile("etmp")
    gt_ekb = sb(s1, "gt_ekb", [128, 512], F32)
    gt_eb = sb(s1, "gt_eb", [128, 512], F32)
    gt_enb = sb(s1, "gt_enb", [128, 512], F32)
    gt_edec = sb(s1, "gt_edec", [128, 4], F32)
    t_gt = Tile("gt")
    op(POOL, lambda: nc.gpsimd.memset(glrT[:], 1.0), writes=[t_glrT])

    def gen_cossin(T):
        for which in range(2):
            op(DVE, lambda: nc.vector.tensor_scalar(out=tr_a[:], in0=C("INVF"), scalar1=posf[:, T:T + 1], scalar2=None,
                                                    op0=ALU.mult), reads=[t_const, t_pos], writes=[t_tr])
            if which == 0:
                op(DVE, lambda: nc.vector.tensor_scalar(out=tr_a[:], in0=tr_a[:], scalar1=math.pi / 2, scalar2=None,
                                                        op0=ALU.add), reads=[t_tr], writes=[t_tr])
            op(DVE, lambda: nc.vector.tensor_scalar(out=tr_i[:], in0=tr_a[:], scalar1=1.0 / TWO_PI, scalar2=None,
                                                    op0=ALU.mult), reads=[t_tr], writes=[t_tr])
            op(DVE, lambda: nc.vector.tensor_copy(out=tr_b[:], in_=tr_i[:]), reads=[t_tr], writes=[t_tr])
            op(DVE, lambda: nc.vector.scalar_tensor_tensor(out=tr_a[:], in0=tr_b[:], scalar=-TWO_PI, in1=tr_a[:],
                                                           op0=ALU.mult, op1=ALU.add), reads=[t_tr], writes=[t_tr])
            op(DVE, lambda: nc.vector.tensor_scalar(out=tr_b[:], in0=tr_a[:], scalar1=math.pi, scalar2=TWO_PI,
                                                    op0=ALU.is_gt, op1=ALU.mult), reads=[t_tr], writes=[t_tr])
            op(DVE, lambda: nc.vector.tensor_tensor(out=tr_a[:], in0=tr_a[:], in1=tr_b[:], op=ALU.subtract),
               reads=[t_tr], writes=[t_tr])
            op(DVE, lambda: nc.vector.tensor_scalar(out=tr_b[:], in0=tr_a[:], scalar1=-math.pi, scalar2=TWO_PI,
                                                    op0=ALU.is_lt, op1=ALU.mult), reads=[t_tr], writes=[t_tr])
            op(DVE, lambda: nc.vector.tensor_tensor(out=tr_a[:], in0=tr_a[:], in1=tr_b[:], op=ALU.add),
               reads=[t_tr], writes=[t_tr])
            op(ACT, lambda w=which: nc.scalar.activation(out=cs[:, w, :], in_=tr_a[:], func=AF.Sin),
               reads=[t_tr], writes=[t_cs])

    def rotary(ps_ap, out_ap, extra_reads, out_tile):
        p3 = ps_ap.rearrange("p (a b) -> p a b", a=2)
        cosb = cs[:, 0, :].unsqueeze(1).to_broadcast([128, 2, 128])
        sinb = cs[:, 1, :].unsqueeze(1).to_broadcast([128, 2, 128])
        op(DVE, lambda: nc.vector.tensor_tensor(out=rotA[:].rearrange("p (a b) -> p a b", a=2), in0=p3, in1=cosb, op=ALU.mult),
           reads=[t_cs] + extra_reads, writes=[t_rot])
        op(DVE, lambda: nc.vector.tensor_tensor(out=rotB[:].rearrange("p (a b) -> p a b", a=2), in0=p3, in1=sinb, op=ALU.mult),
           reads=[t_cs] + extra_reads, writes=[t_rot])
        op(DVE, lambda: nc.vector.tensor_tensor(out=out_ap[:, 0:128], in0=rotA[:, 0:128], in1=rotB[:, 128:256], op=ALU.subtract),
           reads=[t_rot], writes=[out_tile])
        op(DVE, lambda: nc.vector.tensor_tensor(out=out_ap[:, 128:256], in0=rotB[:, 0:128], in1=rotA[:, 128:256], op=ALU.add),
           reads=[t_rot], writes=[out_tile])

    def proj(xT_fn, x_tiles, w_ap_fn, w_tiles, ncols, acc_i):
        mmgroup(PE, [lambda k=k: nc.tensor.matmul(Fp[acc_i][:, 0:ncols], lhsT=xT_fn(k), rhs=w_ap_fn(k),
                                                  start=(k == 0), stop=(k == KC - 1)) for k in range(KC)],
                reads=list(x_tiles) + list(w_tiles), writes=[Ft[acc_i]])

    def gla_gates(T, xT_fn, x_tiles, wlr_fn, w_tiles, own):
        mmgroup(PE, [lambda k=k: nc.tensor.matmul(Fp[5][:, 0:16], lhsT=xT_fn(k), rhs=wlr_fn(k), start=(k == 0), stop=(k == KC - 1))
                     for k in range(KC)], reads=list(x_tiles) + list(w_tiles), writes=[Ft[5]])
        op(ACT, lambda: nc.scalar.copy(out=glr_b[:], in_=Fp[5][:, 0:16]), reads=[Ft[5]], writes=[t_glr])
        op(PE, lambda: nc.tensor.transpose(Tp[1][0:16, 0:128], glr_b[:], ident[:]), reads=[t_glr, t_ident], writes=[Tt[1]])
        op(DVE, lambda: nc.vector.tensor_copy(out=glrT[0:16, :], in_=Tp[1][0:16, 0:128]), reads=[Tt[1]], writes=[t_glrT])
        op(PE, lambda: nc.tensor.matmul(Fp[5][:, :], lhsT=glrT[:, :], rhs=wa2b[:, :], start=True, stop=True),
           reads=[t_glrT, t_const], writes=[Ft[5]])
        op(ACT, lambda: nc.scalar.activation(out=etmp[:], in_=Fp[5][:, :], func=AF.Exp, scale=-1.0), reads=[Ft[5]], writes=[t_et])
        op(ACT, lambda: nc.scalar.activation(out=Lg[:], in_=etmp[:], func=AF.Ln, bias=1.0), reads=[t_et], writes=[t_L])
        op(PE, lambda: nc.tensor.matmul(Fp[5][:, :], lhsT=C("UT2"), rhs=Lg[:], start=True, stop=True),
           reads=[t_L, t_const], writes=[Ft[5]])
        op(ACT, lambda: nc.scalar.activation(out=gt_ekb[:], in_=Fp[5][:, :], func=AF.Exp), reads=[Ft[5]], writes=[t_gt])
        mmgroup(PE, [lambda h=h: nc.tensor.matmul(Fp[5][:, h:h + 1], lhsT=Lg[:, h * 128:(h + 1) * 128], rhs=C("NCOL"),
                                                  start=True, stop=True) for h in range(4)],
                reads=[t_L, t_const], writes=[Ft[5]])
        op(ACT, lambda: nc.scalar.activation(out=gt_edec[:], in_=Fp[5][:, 0:4], func=AF.Exp), reads=[Ft[5]], writes=[t_gt])
        if own:
            op(PE, lambda: nc.tensor.matmul(Fp[5][:, :], lhsT=C("UT"), rhs=Lg[:], start=True, stop=True),
               reads=[t_L, t_const], writes=[Ft[5]])
            op(ACT, lambda: nc.scalar.activation(out=gt_eb[:], in_=Fp[5][:, :], func=AF.Exp), reads=[Ft[5]], writes=[t_gt])
            op(ACT, lambda: nc.scalar.activation(out=gt_enb[:], in_=Fp[5][:, :], func=AF.Exp, scale=-1.0), reads=[Ft[5]], writes=[t_gt])

    def ret_state_update(h, k_ap, kt, vh_ap, vht):
        mmgroup(PE, [lambda c=c: nc.tensor.matmul(Fp[4][:, c * 256:(c + 1) * 256], lhsT=k_ap[:, c * 128:(c + 1) * 128], rhs=vh_ap,
                                                  start=True, stop=True) for c in range(2)],
                reads=[kt, vht], writes=[Ft[4]])
        op(DVE, lambda: nc.vector.scalar_tensor_tensor(out=Rst[:, h, :], in0=Rst[:, h, :], scalar=float(GAMMAS[h] ** 128),
                                                       in1=Fp[4][:, :], op0=ALU.mult, op1=ALU.add),
           reads=[Ft[4], t_R[h]], writes=[t_R[h]])

    def gla_state_update(h, khat_ap, kt, v_ap, vt):
        op(PE, lambda: nc.tensor.matmul(Fp[4][:, 0:256], lhsT=khat_ap, rhs=v_ap, start=True, stop=True),
           reads=[kt, vt], writes=[Ft[4]])
        op(DVE, lambda: nc.vector.scalar_tensor_tensor(out=Sst[:, h, :], in0=Sst[:, h, :], scalar=gt_edec[:, h:h + 1],
                                                       in1=Fp[4][:, 0:256], op0=ALU.mult, op1=ALU.add),
           reads=[Ft[4], t_S[h], t_gt], writes=[t_S[h]])

    if STOP == "s":
        return finish()
    s1a = Scope(hi)
    s1lo = Scope(lo)
    WA = sb(s1lo, "WA", [128, KC, A_W], BF16)
    t_WA = Tile("WA")
    d_wa = dsem("d_wa")
    for k0 in range(0, KC, 4):
        dma(POOL, d_wa, WA[:, k0:k0 + 4, :], w_in[k0 * 128:(k0 + 4) * 128, 0:A_W].rearrange("(k p) n -> p k n", p=128))
    t_WA.w = [(d_wa.sem, d_wa.count)]
    xt_buf = [sb(s1a, f"xt{i}", [128, KC, 128], BF16) for i in range(2)]
    t_xt = [Tile(f"xt{i}") for i in range(2)]
    d_xt = [dsem(f"d_xt{i}") for i in range(2)]

    def load_xt(T):
        i = T % 2
        dma(POOL, d_xt[i], xt_buf[i][:], xTp[T].rearrange("p (k t) -> p k t", k=KC), dst=t_xt[i])

    load_xt(0)
    for T in range(NPRE):
        if T + 1 < NPRE:
            load_xt(T + 1)
        xb = xt_buf[T % 2]
        xt_ = t_xt[T % 2]
        xT_fn = lambda k, xb=xb: xb[:, k, :]
        if STOP == "a0":
            return finish()
        gen_cossin(T)
        if STOP == "a1":
            return finish()
        for h in range(4):
            c0, n = A_RET[h]
            ai = h % 2
            proj(xT_fn, [xt_], lambda k, c0=c0, n=n: WA[:, k, c0:c0 + n], [t_WA], n, ai)
            if STOP == f"h{h}b0":
                return finish()
            rotary(Fp[ai][:, 0:256], kbuf[ai], [Ft[ai]], t_k[ai])
            if STOP == f"h{h}b1":
                return finish()
            op(DVE, lambda ai=ai, h=h: nc.vector.tensor_scalar(out=vhat[ai][:], in0=Fp[ai][:, 256:512], scalar1=C("DK", 2 * h, 2 * h + 1),
                                                               scalar2=None, op0=ALU.mult), reads=[Ft[ai], t_const], writes=[t_vh[ai]])
            if STOP == f"h{h}b2":
                return finish()
            ret_state_update(h, kbuf[ai], t_k[ai], vhat[ai][:], t_vh[ai])
            if STOP == f"h{h}b3":
                return finish()
        if STOP == "a2":
            return finish()
        gla_gates(T, xT_fn, [xt_], lambda k: WA[:, k, A_LR[0]:A_LR[0] + 16], [t_WA], own=False)
        if STOP == "a3":
            return finish()
        for h in range(4):
            c0, n = A_GLA[h]
            ai = 2 + h % 2
            bi = h % 2
            proj(xT_fn, [xt_], lambda k, c0=c0, n=n: WA[:, k, c0:c0 + n], [t_WA], n, ai)
            if STOP == f"T{T}g{h}p":
                return finish()
            op(DVE, lambda ai=ai, bi=bi, h=h: nc.vector.tensor_tensor(out=kbuf[bi][:, 0:128], in0=Fp[ai][:, 0:128],
                                                                      in1=gt_ekb[:, h * 128:(h + 1) * 128], op=ALU.mult),
               reads=[Ft[ai], t_gt], writes=[t_k[bi]])
            if STOP == f"T{T}g{h}k":
                return finish()
            op(ACT, lambda ai=ai, bi=bi: nc.scalar.copy(out=vbuf[bi][:], in_=Fp[ai][:, 128:384]), reads=[Ft[ai]], writes=[t_v[bi]])
            if STOP == f"T{T}g{h}v":
                return finish()
            gla_state_update(h, kbuf[bi][:, 0:128], t_k[bi], vbuf[bi][:], t_v[bi])
            if STOP == f"T{T}g{h}":
                return finish()
        if STOP == f"T{T}":
            return finish()
    for h in range(4):
        op(ACT, lambda h=h: nc.scalar.copy(out=Rb[:, h, :], in_=Rst[:, h, :]), reads=[t_R[h]], writes=[t_Rb[h]])
        op(ACT, lambda h=h: nc.scalar.copy(out=Sb[:, h, :], in_=Sst[:, h, :]), reads=[t_S[h]], writes=[t_Sb[h]])
    barrier()
    if STOP == "1a":
        return finish()
    s1a.close()
    s1lo.close()
    plo = Scope(lo)
    ACT_A = sb(plo, "ACT_A", [128, KC, TOK], BF16)
    ACT_B = sb(plo, "ACT_B", [128, KC, TOK], BF16)
    slots = [sb(plo, f"wslot{i}", [128, KC * 512], BF16) for i in range(NSLOT)]
    assert lo.top <= LO_AFTER

    blocks = []

    def wblock(ap2d, kch, ncols):
        blocks.append((ap2d, kch, ncols))
        return len(blocks) - 1

    blk_retA = [wblock(w_in[:, c0:c0 + n], KC, n) for (c0, n) in A_RET]
    blk_retB = [wblock(w_in[:, c0:c0 + n], KC, n) for (c0, n) in B_RET]
    blk_glaA = [wblock(w_in[:, c0:c0 + n], KC, n) for (c0, n) in A_GLA]
    blk_glaB = [wblock(w_in[:, c0:c0 + n], KC, n) for (c0, n) in B_GLA]
    order = []
    for h in range(4):
        order += [blk_retA[h], blk_retB[h]]
    for h in range(4):
        order += [blk_glaA[h], blk_glaB[h]]
    if need >= 1:
        blk_mix = [wblock(w_mix[:, j * 512:(j + 1) * 512], KC, 512) for j in range(4)]
        order += blk_mix
    if need >= 2:
        blk_mk = [wblock(w_mk[:, j * 512:(j + 1) * 512], KC, 512) for j in range(4)]
        blk_mv = [wblock(w_mv[:, j * 512:(j + 1) * 512], KC, 512) for j in range(4)]
        blk_mq = [wblock(w_mq[:, j * 512:(j + 1) * 512], KC, 512) for j in range(4)]
        blk_mo = [wblock(w_mo[:, j * 512:(j + 1) * 512], KC, 512) for j in range(4)]
        order += blk_mk + blk_mv + blk_mq + blk_mo
    blk_e = []
    for e in range(NEXP if need >= 3 else 0):
        g_ = wblock(w_eg[e], KC, 512)
        u_ = wblock(w_eu[e], KC, 512)
        d_ = wblock(w_ed[e], 4, 2048)
        blk_e.append((g_, u_, d_))
        order += [g_, u_, d_]
    pos_in_order = {b: i for i, b in enumerate(order)}
    issued = [0]
    blk_slot = {}

    def issue_upto(i):
        while issued[0] <= min(i, len(order) - 1):
            j = issued[0]
            b = order[j]
            ap2d, kch, ncols = blocks[b]
            s = j % NSLOT
            dst = slots[s][:, 0:kch * ncols].rearrange("p (k n) -> p k n", k=kch)
            dma(POOL, slot_ds[s], dst, ap2d.rearrange("(k p) n -> p k n", p=128), dst=slot_t[s])
            blk_slot[b] = s
            issued[0] += 1

    def wget(b, pf=2):
        i = pos_in_order[b]
        issue_upto(i + pf)
        s = blk_slot[b]
        ap2d, kch, ncols = blocks[b]
        v = slots[s][:, 0:kch * ncols].rearrange("p (k n) -> p k n", k=kch)
        return v, slot_t[s]

    d_xo = dsem("d_xo")
    for t in range(NT):
        dma(POOL, d_xo, ACT_A[:, :, t * 128:(t + 1) * 128], xTp[NPRE + t].rearrange("p (k t) -> p k t", k=KC))
    t_A.w = [(d_xo.sem, d_xo.count)]

    own = Scope(hi)
    cs_own = sb(own, "cs_own", [128, NT, 256], F32)
    t_cso = Tile("cs_own")
    g_ekb = sb(own, "g_ekb", [128, NT, 512], BF16)
    g_eb = sb(own, "g_eb", [128, NT, 512], BF16)
    g_enb = sb(own, "g_enb", [128, NT, 512], F32)
    g_edec = sb(own, "g_edec", [128, NT, 4], F32)
    t_gown = Tile("gown")
    qbuf = sb(own, "qbuf", [128, 256], BF16)
    t_q = Tile("q")
    sgbuf = sb(own, "sgbuf", [128, 256], BF16)
    t_sg = Tile("sg")
    qkT = sb(own, "qkT", [128, 6, 128], BF16)
    t_qkT = Tile("qkT")
    sTb = sb(own, "sTb", [128, 128], BF16)
    t_sT = Tile("sT")
    stat = sb(own, "stat", [128, 8], F32)
    t_stat = Tile("stat")
    bst = sb(own, "bst", [128, 6], F32)
    ynorm = sb(own, "ynorm", [128, 256], F32)
    t_yn = Tile("yn")
    mixb = sb(own, "mixb", [128, 256], BF16)
    t_mixb = Tile("mixb")
    junk = sb(own, "junk", [128, 256], F32)
    t_junk = Tile("junk")

    def xTown(t):
        return lambda k: ACT_A[:, k, t * 128:(t + 1) * 128]

    for t in range(NT):
        gen_cossin(NPRE + t)
        op(DVE, lambda t=t: nc.vector.tensor_copy(out=cs_own[:, t, :], in_=cs[:].rearrange("p a b -> p (a b)")),
           reads=[t_cs], writes=[t_cso])

    def finish_head_tile(hcol, t, is_ret, o_acc):
        o_ps = Fp[o_acc][:, 0:256]
        if is_ret:
            op(DVE, lambda: nc.vector.bn_stats(out=bst[:], in_=o_ps), reads=[Ft[o_acc]], writes=[t_stat])
            op(DVE, lambda: nc.vector.bn_aggr(out=stat[:, 0:2], in_=bst[:]), reads=[t_stat], writes=[t_stat])
            op(DVE, lambda: nc.vector.tensor_scalar(out=stat[:, 2:3], in0=stat[:, 1:2], scalar1=EPS, scalar2=None, op0=ALU.add),
               reads=[t_stat], writes=[t_stat])
        else:
            op(DVE, lambda: nc.vector.memset(stat[:, 0:1], 0.0), writes=[t_stat])
            op(ACT, lambda: nc.scalar.activation(out=junk[:], in_=o_ps, func=AF.Square, accum_out=stat[:, 0:1]),
               reads=[Ft[o_acc]], writes=[t_junk, t_stat])
            op(DVE, lambda: nc.vector.tensor_scalar(out=stat[:, 2:3], in0=stat[:, 0:1], scalar1=1.0 / 256.0, scalar2=EPS,
                                                    op0=ALU.mult, op1=ALU.add), reads=[t_stat], writes=[t_stat])
        op(ACT, lambda: nc.scalar.activation(out=stat[:, 3:4], in_=stat[:, 2:3], func=AF.Sqrt), reads=[t_stat], writes=[t_stat])
        op(DVE, lambda: nc.vector.reciprocal(out=stat[:, 4:5], in_=stat[:, 3:4]), reads=[t_stat], writes=[t_stat])
        if is_ret:
            op(DVE, lambda: nc.vector.tensor_scalar(out=ynorm[:], in0=o_ps, scalar1=stat[:, 0:1], scalar2=stat[:, 4:5],
                                                    op0=ALU.subtract, op1=ALU.mult), reads=[Ft[o_acc], t_stat], writes=[t_yn])
            op(DVE, lambda: nc.vector.tensor_tensor(out=mixb[:], in0=ynorm[:], in1=sgbuf[:], op=ALU.mult),
               reads=[t_yn, t_sg], writes=[t_mixb])
        else:
            op(DVE, lambda: nc.vector.scalar_tensor_tensor(out=mixb[:], in0=o_ps, scalar=stat[:, 4:5], in1=sgbuf[:],
                                                           op0=ALU.mult, op1=ALU.mult), reads=[Ft[o_acc], t_stat, t_sg], writes=[t_mixb])
        mmgroup(PE, [lambda c=c: nc.tensor.transpose(Tp[1][:, c * 128:(c + 1) * 128], mixb[:, c * 128:(c + 1) * 128], ident[:])
                     for c in range(2)], reads=[t_mixb, t_ident], writes=[Tt[1]])
        kk = hcol * 2
        op(ACT, lambda: nc.scalar.copy(out=ACT_B[:, kk:kk + 2, t * 128:(t + 1) * 128],
                                       in_=Tp[1][:, 0:256].rearrange("p (c t) -> p c t", c=2)), reads=[Tt[1]], writes=[t_Bt[t]])

    for h in range(4):
        WAv, WAt = wget(blk_retA[h], 2)
        WBv, WBt = wget(blk_retB[h], 1)
        for t in range(NT):
            xf = xTown(t)
            proj(xf, [t_A], lambda k: WAv[:, k, :], [WAt], 512, 0)
            proj(xf, [t_A], lambda k: WBv[:, k, :], [WBt], 512, 1)
            op(DVE, lambda t=t: nc.vector.tensor_copy(out=cs[:].rearrange("p a b -> p (a b)"), in_=cs_own[:, t, :]),
               reads=[t_cso], writes=[t_cs])
            rotary(Fp[0][:, 0:256], kbuf[0], [Ft[0]], t_k[0])
            rotary(Fp[1][:, 0:256], qbuf, [Ft[1]], t_q)
            op(ACT, lambda: nc.scalar.copy(out=vbuf[0][:], in_=Fp[0][:, 256:512]), reads=[Ft[0]], writes=[t_v[0]])
            op(DVE, lambda h=h: nc.vector.tensor_scalar(out=vhat[0][:], in0=Fp[0][:, 256:512], scalar1=C("DK", 2 * h, 2 * h + 1),
                                                        scalar2=None, op0=ALU.mult), reads=[Ft[0], t_const], writes=[t_vh[0]])
            op(ACT, lambda: nc.scalar.activation(out=sgbuf[:], in_=Fp[1][:, 256:512], func=AF.Silu), reads=[Ft[1]], writes=[t_sg])
            mmgroup(PE, [lambda c=c: nc.tensor.transpose(Tp[0][:, c * 128:(c + 1) * 128], qbuf[:, c * 128:(c + 1) * 128], ident[:])
                         for c in range(2)] +
                        [lambda c=c: nc.tensor.transpose(Tp[0][:, (2 + c) * 128:(3 + c) * 128], kbuf[0][:, c * 128:(c + 1) * 128], ident[:])
                         for c in range(2)], reads=[t_q, t_k[0], t_ident], writes=[Tt[0]])
            op(ACT, lambda: nc.scalar.copy(out=qkT[:, 0:4, :], in_=Tp[0][:, 0:512].rearrange("p (c t) -> p c t", c=4)),
               reads=[Tt[0]], writes=[t_qkT])
            op(DVE, lambda h=h: nc.vector.tensor_tensor(out=qkT[:, 4:6, :], in0=Tp[0][:, 0:256].rearrange("p (c t) -> p c t", c=2),
                                                        in1=C("DQ", h * 128, (h + 1) * 128).unsqueeze(1).to_broadcast([128, 2, 128]),
                                                        op=ALU.mult), reads=[Tt[0], t_const], writes=[t_qkT])
            mmgroup(PE, [lambda c=c: nc.tensor.matmul(Fp[2][:, 0:128], lhsT=qkT[:, 2 + c, :], rhs=qkT[:, c, :], start=(c == 0), stop=(c == 1))
                         for c in range(2)], reads=[t_qkT], writes=[Ft[2]])
            op(DVE, lambda h=h: nc.vector.tensor_tensor(out=sTb[:], in0=Fp[2][:, 0:128], in1=C("DT", h * 128, (h + 1) * 128), op=ALU.mult),
               reads=[Ft[2], t_const], writes=[t_sT])
            mmgroup(PE, [lambda: nc.tensor.matmul(Fp[3][:, 0:256], lhsT=sTb[:], rhs=vbuf[0][:], start=True, stop=False)] +
                        [lambda c=c, h=h: nc.tensor.matmul(Fp[3][:, 0:256], lhsT=qkT[:, 4 + c, :], rhs=Rb[:, h, c * 256:(c + 1) * 256],
                                                           start=False, stop=(c == 1)) for c in range(2)],
                    reads=[t_sT, t_v[0], t_qkT, t_Rb[h]], writes=[Ft[3]])
            ret_state_update(h, kbuf[0], t_k[0], vhat[0][:], t_vh[0])
            op(ACT, lambda h=h: nc.scalar.copy(out=Rb[:, h, :], in_=Rst[:, h, :]), reads=[t_R[h]], writes=[t_Rb[h]])
            finish_head_tile(h, t, True, 3)

    WAv0, WAt0 = None, None
    d_lr = dsem("d_lr")
    wlr = sb(own, "wlr", [128, KC, 16], BF16)
    t_wlr = Tile("wlr")
    dma(POOL, d_lr, wlr[:], w_in[:, A_LR[0]:A_LR[0] + 16].rearrange("(k p) n -> p k n", p=128), dst=t_wlr)
    for t in range(NT):
        gla_gates(NPRE + t, xTown(t), [t_A], lambda k: wlr[:, k, :], [t_wlr], own=True)
        op(DVE, lambda t=t: nc.vector.tensor_copy(out=g_ekb[:, t, :], in_=gt_ekb[:]), reads=[t_gt], writes=[t_gown])
        op(DVE, lambda t=t: nc.vector.tensor_copy(out=g_eb[:, t, :], in_=gt_eb[:]), reads=[t_gt], writes=[t_gown])
        op(DVE, lambda t=t: nc.vector.tensor_copy(out=g_enb[:, t, :], in_=gt_enb[:]), reads=[t_gt], writes=[t_gown])
        op(DVE, lambda t=t: nc.vector.tensor_copy(out=g_edec[:, t, :], in_=gt_edec[:]), reads=[t_gt], writes=[t_gown])

    for h in range(4):
        WAv, WAt = wget(blk_glaA[h], 2)
        WBv, WBt = wget(blk_glaB[h], 1)
        hs = slice(h * 128, (h + 1) * 128)
        for t in range(NT):
            xf = xTown(t)
            proj(xf, [t_A], lambda k: WAv[:, k, :], [WAt], 384, 0)
            proj(xf, [t_A], lambda k: WBv[:, k, :], [WBt], 384, 1)
            op(DVE, lambda t=t: nc.vector.tensor_tensor(out=kbuf[0][:, 0:128], in0=Fp[0][:, 0:128], in1=g_enb[:, t, hs], op=ALU.mult),
               reads=[Ft[0], t_gown], writes=[t_k[0]])
            op(DVE, lambda t=t: nc.vector.tensor_tensor(out=kbuf[1][:, 0:128], in0=Fp[0][:, 0:128], in1=g_ekb[:, t, hs], op=ALU.mult),
               reads=[Ft[0], t_gown], writes=[t_k[1]])
            op(DVE, lambda t=t: nc.vector.scalar_tensor_tensor(out=qbuf[:, 0:128], in0=Fp[1][:, 0:128], scalar=128.0 ** -0.5,
                                                               in1=g_eb[:, t, hs], op0=ALU.mult, op1=ALU.mult),
               reads=[Ft[1], t_gown], writes=[t_q])
            op(ACT, lambda: nc.scalar.copy(out=vbuf[0][:], in_=Fp[0][:, 128:384]), reads=[Ft[0]], writes=[t_v[0]])
            op(ACT, lambda: nc.scalar.activation(out=ynorm[:], in_=Fp[1][:, 128:384], func=AF.Silu), reads=[Ft[1]], writes=[t_yn])
            op(DVE, lambda: nc.vector.tensor_tensor(out=sgbuf[:], in0=ynorm[:], in1=gnb[:], op=ALU.mult),
               reads=[t_yn, t_const], writes=[t_sg])
            mmgroup(PE, [lambda: nc.tensor.transpose(Tp[0][:, 0:128], qbuf[:, 0:128], ident[:]),
                         lambda: nc.tensor.transpose(Tp[0][:, 128:256], kbuf[0][:, 0:128], ident[:])],
                    reads=[t_q, t_k[0], t_ident], writes=[Tt[0]])
            op(ACT, lambda: nc.scalar.copy(out=qkT[:, 0:2, :], in_=Tp[0][:, 0:256].rearrange("p (c t) -> p c t", c=2)),
               reads=[Tt[0]], writes=[t_qkT])
            op(PE, lambda: nc.tensor.matmul(Fp[2][:, 0:128], lhsT=qkT[:, 1, :], rhs=qkT[:, 0, :], start=True, stop=True),
               reads=[t_qkT], writes=[Ft[2]])
            op(DVE, lambda: nc.vector.tensor_tensor(out=sTb[:], in0=Fp[2][:, 0:128], in1=C("CAUS"), op=ALU.mult),
               reads=[Ft[2], t_const], writes=[t_sT])
            mmgroup(PE, [lambda: nc.tensor.matmul(Fp[3][:, 0:256], lhsT=sTb[:], rhs=vbuf[0][:], start=True, stop=False),
                         lambda h=h: nc.tensor.matmul(Fp[3][:, 0:256], lhsT=qkT[:, 0, :], rhs=Sb[:, h, :], start=False, stop=True)],
                    reads=[t_sT, t_v[0], t_qkT, t_Sb[h]], writes=[Ft[3]])
            op(PE, lambda: nc.tensor.matmul(Fp[4][:, 0:256], lhsT=kbuf[1][:, 0:128], rhs=vbuf[0][:], start=True, stop=True),
               reads=[t_k[1], t_v[0]], writes=[Ft[4]])
            op(DVE, lambda h=h, t=t: nc.vector.scalar_tensor_tensor(out=Sst[:, h, :], in0=Sst[:, h, :], scalar=g_edec[:, t, h:h + 1],
                                                                    in1=Fp[4][:, 0:256], op0=ALU.mult, op1=ALU.add),
               reads=[Ft[4], t_S[h], t_gown], writes=[t_S[h]])
            op(ACT, lambda h=h: nc.scalar.copy(out=Sb[:, h, :], in_=Sst[:, h, :]), reads=[t_S[h]], writes=[t_Sb[h]])
            finish_head_tile(4 + h, t, False, 3)

    if DEBUG:
        for t in range(NT):
            t_B.w += t_Bt[t].w
        dma(SP, d_dbg, dbg_mixT, ACT_B[:].rearrange("p k t -> p (k t)"), src=t_B)
    barrier()
    own.close()
    s1.close()
    if STOP == "1b":
        return finish()

    assert hi.top == hi.base
    hi.base = hi.top = LO_AFTER
    s2 = Scope(hi)
    RES = sb(s2, "RES", [128, NT, D], F32)
    t_res = [Tile(f"res{t}") for t in range(NT)]
    t_ln = Tile("ln")
    d_ln = dsem("d_ln")
    d_x = dsem("d_x")
    for t in range(NT):
        dma(SP, d_x, RES[:, t, :], xown[t * 128:(t + 1) * 128, :])
    for t in range(NT):
        t_res[t].w = [(d_x.sem, d_x.count)]
    t_lnst = Tile("lnst")
    t_hb = Tile("hb")
    lnS = Scope(hi)
    lnt = sb(lnS, "lnt", [128, 2, D], F32)
    lnst = sb(lnS, "lnst", [128, 4, 6], F32)
    lnmv = sb(lnS, "lnmv", [128, 8], F32)
    hb = sb(lnS, "hb", [128, D], BF16)

    def load_ln(i):
        dma(SP, d_ln, lnt[:, 0, :], ln_d[2 * i:2 * i + 1, :].partition_broadcast(128), dst=t_ln)
        dma(SP, d_ln, lnt[:, 1, :], ln_d[2 * i + 1:2 * i + 2, :].partition_broadcast(128), dst=t_ln)

    def linear(AT, at_tiles, blks, epilogue, ntiles=NT):
        for j, b in enumerate(blks):
            Wv, Wt = wget(b)
            for t in range(ntiles):
                ai = (j * ntiles + t) % 2
                mmgroup(PE, [lambda k=k, t=t: nc.tensor.matmul(Fp[ai][:, :], lhsT=AT[:, k, t * 128:(t + 1) * 128], rhs=Wv[:, k, :],
                                                               start=(k == 0), stop=(k == KC - 1)) for k in range(KC)],
                        reads=list(at_tiles(t)) + [Wt], writes=[Ft[ai]])
                epilogue(j, t, ai)

    def resid_epi(j, t, ai):
        op(DVE, lambda: nc.vector.scalar_tensor_tensor(out=RES[:, t, j * 512:(j + 1) * 512], in0=RES[:, t, j * 512:(j + 1) * 512],
                                                       scalar=ALPHA, in1=Fp[ai][:, :], op0=ALU.mult, op1=ALU.add),
           reads=[Ft[ai], t_res[t]], writes=[t_res[t]])

    def layernorm(t, dstT, dst_tiles, want_tok_bf16=None):
        for c in range(4):
            op(DVE, lambda c=c: nc.vector.bn_stats(out=lnst[:, c, :], in_=RES[:, t, c * 512:(c + 1) * 512]),
               reads=[t_res[t]], writes=[t_lnst])
        op(DVE, lambda: nc.vector.bn_aggr(out=lnmv[:, 0:2], in_=lnst[:].rearrange("p a b -> p (a b)")), reads=[t_lnst], writes=[t_lnst])
        op(DVE, lambda: nc.vector.tensor_scalar(out=lnmv[:, 2:3], in0=lnmv[:, 1:2], scalar1=EPS, scalar2=None, op0=ALU.add),
           reads=[t_lnst], writes=[t_lnst])
        op(ACT, lambda: nc.scalar.activation(out=lnmv[:, 3:4], in_=lnmv[:, 2:3], func=AF.Sqrt), reads=[t_lnst], writes=[t_lnst])
        op(DVE, lambda: nc.vector.reciprocal(out=lnmv[:, 4:5], in_=lnmv[:, 3:4]), reads=[t_lnst], writes=[t_lnst])
        op(DVE, lambda: nc.vector.tensor_scalar(out=RES[:, t, :], in0=RES[:, t, :], scalar1=lnmv[:, 0:1], scalar2=lnmv[:, 4:5],
                                                op0=ALU.subtract, op1=ALU.mult), reads=[t_res[t], t_lnst], writes=[t_res[t]])
        op(DVE, lambda: nc.vector.tensor_tensor(out=RES[:, t, :], in0=RES[:, t, :], in1=lnt[:, 0, :], op=ALU.mult),
           reads=[t_res[t], t_ln], writes=[t_res[t]])
        op(DVE, lambda: nc.vector.tensor_tensor(out=RES[:, t, :], in0=RES[:, t, :], in1=lnt[:, 1, :], op=ALU.add),
           reads=[t_res[t], t_ln], writes=[t_res[t]])
        if dstT is not None:
            hbt = hb[:] if want_tok_bf16 is None else want_tok_bf16
            hbt_tile = t_hb
            op(ACT, lambda: nc.scalar.copy(out=hbt, in_=RES[:, t, :]), reads=[t_res[t]], writes=[hbt_tile])
            for half in range(2):
                mmgroup(PE, [lambda c=c: nc.tensor.transpose(Tp[half][:, c * 128:(c + 1) * 128],
                                                             hbt[:, (half * 8 + c) * 128:(half * 8 + c + 1) * 128], ident[:])
                             for c in range(8)], reads=[hbt_tile, t_ident], writes=[Tt[half]])
                op(ACT if half == 0 else DVE,
                   (lambda half=half: nc.scalar.copy(out=dstT[:, half * 8:(half + 1) * 8, t * 128:(t + 1) * 128],
                                                     in_=Tp[half][:].rearrange("p (c t) -> p c t", c=8))) if half == 0 else
                   (lambda half=half: nc.vector.tensor_copy(out=dstT[:, half * 8:(half + 1) * 8, t * 128:(t + 1) * 128],
                                                            in_=Tp[half][:].rearrange("p (c t) -> p c t", c=8))),
                   reads=[Tt[half]], writes=[dst_tiles[t]])

    load_ln(0)
    linear(ACT_B, lambda t: [t_Bt[t]], blk_mix, resid_epi)
    for t in range(NT):
        layernorm(t, ACT_A, t_At)
    if DEBUG:
        for t in range(NT):
            dma(SP, d_dbg, dbg_h1[t * 128:(t + 1) * 128, :], RES[:, t, :], src=t_res[t])

    barrier()
    lnS.close()
    if STOP == "2":
        return finish()
    s3 = Scope(hi)
    memT = ACT_B[:, :, 0:256]
    mem_tiles = [t_Bt[0], t_Bt[1]]
    d_mem = dsem("d_mem")
    dma(POOL, d_mem, memT, memT_d.rearrange("p (k m) -> p k m", k=KC), dst=mem_tiles)
    KT = sb(s3, "KT", [128, KC, 256], BF16)
    t_KT = Tile("KT")
    Vm = sb(s3, "Vm", [128, 2, D], BF16)
    t_Vm = Tile("Vm")
    kvb = sb(s3, "kvb", [128, 512], BF16)
    t_kvb = Tile("kvb")
    qb = sb(s3, "qb", [128, 512], BF16)
    t_qb = Tile("qb")
    qTh = sb(s3, "qTh", [128, 4, 128], BF16)
    t_qTh = Tile("qTh")
    pexp = sb(s3, "pexp", [128, 256], BF16)
    t_pexp = Tile("pexp")
    pT = sb(s3, "pT", [128, 2, 128], BF16)
    t_pT = Tile("pT")
    sm = sb(s3, "sm", [128, 8], F32)
    t_sm = Tile("sm")
    ob = sb(s3, "ob", [128, 512], BF16)
    t_ob = Tile("ob")

    def k_epi(j, mt, ai):
        op(ACT, lambda: nc.scalar.copy(out=kvb[:], in_=Fp[ai][:, :]), reads=[Ft[ai]], writes=[t_kvb])
        mmgroup(PE, [lambda c=c: nc.tensor.transpose(Tp[0][:, c * 128:(c + 1) * 128], kvb[:, c * 128:(c + 1) * 128], ident[:])
                     for c in range(4)], reads=[t_kvb, t_ident], writes=[Tt[0]])
        op(DVE, lambda: nc.vector.tensor_copy(out=KT[:, j * 4:(j + 1) * 4, mt * 128:(mt + 1) * 128],
                                              in_=Tp[0][:, 0:512].rearrange("p (c t) -> p c t", c=4)), reads=[Tt[0]], writes=[t_KT])

    def v_epi(j, mt, ai):
        op(ACT, lambda: nc.scalar.copy(out=Vm[:, mt, j * 512:(j + 1) * 512], in_=Fp[ai][:, :]), reads=[Ft[ai]], writes=[t_Vm])

    linear(memT, lambda t: mem_tiles, blk_mk, k_epi, ntiles=2)
    linear(memT, lambda t: mem_tiles, blk_mv, v_epi, ntiles=2)

    SCL = 512.0 ** -0.5

    def q_epi(j, t, ai):
        op(ACT, lambda: nc.scalar.mul(out=qb[:], in_=Fp[ai][:, :], mul=SCL), reads=[Ft[ai]], writes=[t_qb])
        mmgroup(PE, [lambda c=c: nc.tensor.transpose(Tp[0][:, c * 128:(c + 1) * 128], qb[:, c * 128:(c + 1) * 128], ident[:])
                     for c in range(4)], reads=[t_qb, t_ident], writes=[Tt[0]])
        op(DVE, lambda: nc.vector.tensor_copy(out=qTh[:], in_=Tp[0][:, 0:512].rearrange("p (c t) -> p c t", c=4)),
           reads=[Tt[0]], writes=[t_qTh])
        mmgroup(PE, [lambda c=c: nc.tensor.matmul(Fp[2][:, 0:256], lhsT=qTh[:, c, :], rhs=KT[:, j * 4 + c, :], start=(c == 0), stop=(c == 3))
                     for c in range(4)], reads=[t_qTh, t_KT], writes=[Ft[2]])
        op(DVE, lambda: nc.vector.reduce_max(out=sm[:, 0:1], in_=Fp[2][:, 0:256], axis=AX.X), reads=[Ft[2]], writes=[t_sm])
        op(DVE, lambda: nc.vector.tensor_scalar(out=sm[:, 2:3], in0=sm[:, 0:1], scalar1=-1.0, scalar2=None, op0=ALU.mult),
           reads=[t_sm], writes=[t_sm])
        op(DVE, lambda: nc.vector.memset(sm[:, 4:5], 0.0), writes=[t_sm])
        op(ACT, lambda: nc.scalar.activation(out=pexp[:], in_=Fp[2][:, 0:256], func=AF.Exp, bias=sm[:, 2:3], accum_out=sm[:, 4:5]),
           reads=[Ft[2], t_sm], writes=[t_pexp, t_sm])
        op(DVE, lambda: nc.vector.reciprocal(out=sm[:, 6:7], in_=sm[:, 4:5]), reads=[t_sm], writes=[t_sm])
        mmgroup(PE, [lambda c=c: nc.tensor.transpose(Tp[1][:, c * 128:(c + 1) * 128], pexp[:, c * 128:(c + 1) * 128], ident[:])
                     for c in range(2)], reads=[t_pexp, t_ident], writes=[Tt[1]])
        op(DVE, lambda: nc.vector.tensor_copy(out=pT[:], in_=Tp[1][:, 0:256].rearrange("p (c t) -> p c t", c=2)),
           reads=[Tt[1]], writes=[t_pT])
        mmgroup(PE, [lambda mt=mt: nc.tensor.matmul(Fp[3][:, :], lhsT=pT[:, mt, :], rhs=Vm[:, mt, j * 512:(j + 1) * 512],
                                                    start=(mt == 0), stop=(mt == 1)) for mt in range(2)],
                reads=[t_pT, t_Vm], writes=[Ft[3]])
        op(DVE, lambda: nc.vector.tensor_scalar(out=ob[:], in0=Fp[3][:, :], scalar1=sm[:, 6:7], scalar2=None, op0=ALU.mult),
           reads=[Ft[3], t_sm], writes=[t_ob])
        mmgroup(PE, [lambda c=c: nc.tensor.transpose(Tp[0][:, c * 128:(c + 1) * 128], ob[:, c * 128:(c + 1) * 128], ident[:])
                     for c in range(4)], reads=[t_ob, t_ident], writes=[Tt[0]])
        op(DVE, lambda: nc.vector.tensor_copy(out=ACT_B[:, j * 4:(j + 1) * 4, t * 128:(t + 1) * 128],
                                              in_=Tp[0][:, 0:512].rearrange("p (c t) -> p c t", c=4)), reads=[Tt[0]], writes=[t_Bt[t]])

    linear(ACT_A, lambda t: [t_At[t]], blk_mq, q_epi)
    linear(ACT_B, lambda t: [t_Bt[t]], blk_mo, resid_epi)
    barrier()
    s3.close()

    lnS = Scope(hi)
    lnt = sb(lnS, "lnt2", [128, 2, D], F32)
    lnst = sb(lnS, "lnst2", [128, 4, 6], F32)
    lnmv = sb(lnS, "lnmv2", [128, 8], F32)
    hb = sb(lnS, "hb2", [128, D], BF16)
    load_ln(1)
    for t in range(NT):
        layernorm(t, ACT_A, t_At)
    if DEBUG:
        for t in range(NT):
            dma(SP, d_dbg, dbg_h2[t * 128:(t + 1) * 128, :], RES[:, t, :], src=t_res[t])
    barrier()
    lnS.close()
    if STOP == "3":
        return finish()
    s4 = Scope(hi)
    wr = sb(s4, "wr", [128, KC, 36], BF16)
    t_wr = Tile("wr")
    d_wr = dsem("d_wr")
    dma(POOL, d_wr, wr[:], wroute_d.rearrange("(k p) n -> p k n", p=128), dst=t_wr)
    brt = sb(s4, "brt", [128, 36], F32)
    d_wr2 = dsem("d_wr2")
    dma(SP, d_wr2, brt[:], broute_d.partition_broadcast(128))
    t_wr.w = [(d_wr.sem, d_wr.count), (d_wr2.sem, d_wr2.count)]
    lg = sb(s4, "lg", [128, 36], F32)
    t_lg = Tile("lg")
    rs = sb(s4, "rs", [128, 16], F32)
    t_rs = Tile("rs")
    ohg = sb(s4, "ohg", [128, 4], F32)
    f48 = sb(s4, "f48", [128, 32], F32)
    fsel = sb(s4, "fsel", [128, 8], F32)
    oh1 = sb(s4, "oh1", [128, 8], F32)
    oh2 = sb(s4, "oh2", [128, 8], F32)
    fm = sb(s4, "fm", [128, 8], F32)
    gw8 = sb(s4, "gw8", [128, 8], F32)
    Gd = sb(s4, "Gd", [128, NT, 32], F32)
    Md = sb(s4, "Md", [128, NT, 32], F32)
    Mb = sb(s4, "Mb", [128, NT, 32], BF16)
    Ghl = sb(s4, "Ghl", [128, NT, 32, 2], BF16)
    Gtmp = sb(s4, "Gtmp", [128, 32], F32)
    posd = sb(s4, "posd", [128, NT, 32], F32)
    t_rt = [Tile(f"rt{t}") for t in range(NT)]
    t_M = Tile("M")

    H2 = ACT_B[:].rearrange("p k t -> p (k t)").rearrange("p (a d) -> p a d", a=NT)
    for t in range(NT):
        op(ACT, lambda t=t: nc.scalar.copy(out=H2[:, t, :], in_=RES[:, t, :]), reads=[t_res[t]], writes=t_Bt + [t_B])
    for t in range(NT):
        mmgroup(PE, [lambda k=k, t=t: nc.tensor.matmul(Fp[2][:, 0:36], lhsT=ACT_A[:, k, t * 128:(t + 1) * 128], rhs=wr[:, k, :],
                                                       start=(k == 0), stop=(k == KC - 1)) for k in range(KC)],
                reads=[t_At[t], t_wr], writes=[Ft[2]])
        R_ = [t_lg, t_rs]
        op(DVE, lambda: nc.vector.tensor_tensor(out=lg[:], in0=Fp[2][:, 0:36], in1=brt[:], op=ALU.add), reads=[Ft[2], t_wr], writes=[t_lg])
        op(DVE, lambda: nc.vector.reduce_max(out=rs[:, 0:1], in_=lg[:, 0:4], axis=AX.X), reads=[t_lg], writes=[t_rs])
        op(DVE, lambda: nc.vector.tensor_scalar(out=ohg[:], in0=lg[:, 0:4], scalar1=rs[:, 0:1], scalar2=None, op0=ALU.is_equal),
           reads=R_, writes=[t_rs])
        op(DVE, lambda: nc.vector.tensor_scalar(out=rs[:, 14:15], in0=rs[:, 0:1], scalar1=-1.0, scalar2=None, op0=ALU.mult),
           reads=R_, writes=[t_rs])
        op(DVE, lambda: nc.vector.memset(rs[:, 2:3], 0.0), reads=R_, writes=[t_rs])
        op(ACT, lambda: nc.scalar.activation(out=rs[:, 4:8], in_=lg[:, 0:4], func=AF.Exp, bias=rs[:, 14:15], accum_out=rs[:, 2:3]),
           reads=R_, writes=[t_rs])
        op(DVE, lambda: nc.vector.reciprocal(out=rs[:, 3:4], in_=rs[:, 2:3]), reads=R_, writes=[t_rs])
        op(DVE, lambda: nc.vector.tensor_tensor(out=f48[:].rearrange("p (g e) -> p g e", g=4),
                                                in0=lg[:, 4:36].rearrange("p (g e) -> p g e", g=4),
                                                in1=ohg[:].unsqueeze(2).to_broadcast([128, 4, 8]), op=ALU.mult), reads=R_, writes=[t_rs])
        op(DVE, lambda: nc.vector.reduce_sum(out=fsel[:], in_=f48[:].rearrange("p (g e) -> p e g", g=4), axis=AX.X),
           reads=R_, writes=[t_rs])
        op(DVE, lambda: nc.vector.reduce_max(out=rs[:, 8:9], in_=fsel[:], axis=AX.X), reads=R_, writes=[t_rs])
        op(DVE, lambda: nc.vector.tensor_scalar(out=oh1[:], in0=fsel[:], scalar1=rs[:, 8:9], scalar2=None, op0=ALU.is_equal),
           reads=R_, writes=[t_rs])
        op(DVE, lambda: nc.vector.scalar_tensor_tensor(out=fm[:], in0=oh1[:], scalar=-1e30, in1=fsel[:], op0=ALU.mult, op1=ALU.add),
           reads=R_, writes=[t_rs])
        op(DVE, lambda: nc.vector.reduce_max(out=rs[:, 9:10], in_=fm[:], axis=AX.X), reads=R_, writes=[t_rs])
        op(DVE, lambda: nc.vector.tensor_scalar(out=oh2[:], in0=fm[:], scalar1=rs[:, 9:10], scalar2=None, op0=ALU.is_equal),
           reads=R_, writes=[t_rs])
        op(DVE, lambda: nc.vector.tensor_tensor(out=rs[:, 10:11], in0=rs[:, 8:9], in1=rs[:, 9:10], op=ALU.subtract), reads=R_, writes=[t_rs])
        op(ACT, lambda: nc.scalar.activation(out=rs[:, 11:12], in_=rs[:, 10:11], func=AF.Sigmoid), reads=R_, writes=[t_rs])
        op(DVE, lambda: nc.vector.tensor_tensor(out=rs[:, 12:13], in0=rs[:, 11:12], in1=rs[:, 3:4], op=ALU.mult), reads=R_, writes=[t_rs])
        op(DVE, lambda: nc.vector.tensor_tensor(out=rs[:, 13:14], in0=rs[:, 3:4], in1=rs[:, 12:13], op=ALU.subtract), reads=R_, writes=[t_rs])
        op(DVE, lambda: nc.vector.tensor_scalar(out=gw8[:], in0=oh1[:], scalar1=rs[:, 12:13], scalar2=None, op0=ALU.mult), reads=R_, writes=[t_rs])
        op(DVE, lambda: nc.vector.scalar_tensor_tensor(out=gw8[:], in0=oh2[:], scalar=rs[:, 13:14], in1=gw8[:], op0=ALU.mult, op1=ALU.add),
           reads=R_, writes=[t_rs])
        op(DVE, lambda t=t: nc.vector.tensor_tensor(out=Gd[:, t, :].rearrange("p (g e) -> p g e", g=4),
                                                    in0=ohg[:].unsqueeze(2).to_broadcast([128, 4, 8]),
                                                    in1=gw8[:].unsqueeze(1).to_broadcast([128, 4, 8]), op=ALU.mult), reads=R_, writes=[t_rt[t]])
        op(DVE, lambda: nc.vector.tensor_tensor(out=oh1[:], in0=oh1[:], in1=oh2[:], op=ALU.add), reads=R_, writes=[t_rs])
        op(DVE, lambda t=t: nc.vector.tensor_tensor(out=Md[:, t, :].rearrange("p (g e) -> p g e", g=4),
                                                    in0=ohg[:].unsqueeze(2).to_broadcast([128, 4, 8]),
                                                    in1=oh1[:].unsqueeze(1).to_broadcast([128, 4, 8]), op=ALU.mult), reads=R_, writes=[t_rt[t]])
        op(DVE, lambda t=t: nc.vector.tensor_copy(out=Mb[:, t, :], in_=Md[:, t, :]), reads=[t_rt[t]], writes=[t_rt[t]])
        op(DVE, lambda t=t: nc.vector.tensor_copy(out=Ghl[:, t, :, 0], in_=Gd[:, t, :]), reads=[t_rt[t]], writes=[t_rt[t]])
        op(DVE, lambda t=t: nc.vector.tensor_tensor(out=Gtmp[:], in0=Gd[:, t, :], in1=Ghl[:, t, :, 0], op=ALU.subtract),
           reads=[t_rt[t]], writes=[t_rs])
        op(DVE, lambda t=t: nc.vector.tensor_copy(out=Ghl[:, t, :, 1], in_=Gtmp[:]), reads=[t_rs], writes=[t_rt[t]])
        op(POOL, lambda t=t: nc.gpsimd.tensor_scalar(out=RES[:, t, :], in0=RES[:, t, :], scalar1=ALPHA, scalar2=None, op0=ALU.mult),
           reads=[t_res[t], t_Bt[t]], writes=[t_res[t]])
    for t in range(NT):
        fns = [lambda tp=tp: nc.tensor.matmul(Fp[2][:, 0:32], lhsT=CTb[:, 0:128], rhs=Mb[:, tp, :], start=(tp == 0), stop=False)
               for tp in range(t)]
        fns.append(lambda t=t: nc.tensor.matmul(Fp[2][:, 0:32], lhsT=CTb[:, 128:256], rhs=Mb[:, t, :], start=(t == 0), stop=True))
        mmgroup(PE, fns, reads=[t_rt[tp] for tp in range(t + 1)] + [t_ctb], writes=[Ft[2]])
        op(DVE, lambda t=t: nc.vector.tensor_copy(out=posd[:, t, :], in_=Fp[2][:, 0:32]), reads=[Ft[2]], writes=[t_rt[t]])

    Psel = sb(s4, "Psel", [128, NT, CAP], BF16)
    t_Psel = Tile("Psel")
    PselT = [sb(s4, f"PselT{i}", [128, NT, 128], BF16) for i in range(2)]
    t_PselT = [Tile(f"PselT{i}") for i in range(2)]
    xeT = sb(s4, "xeT", [128, KC, CAP], BF16)
    t_xeT = Tile("xeT")
    gs = sb(s4, "gs", [128, 2], F32)
    t_gs = Tile("gs")
    hg = sb(s4, "hg", [128, 512], BF16)
    t_hg = Tile("hg")
    hid = sb(s4, "hid", [128, 512], BF16)
    t_hid = Tile("hid")
    hidT = sb(s4, "hidT", [128, 4, CAP], BF16)
    t_hidT = Tile("hidT")
    yb = [sb(s4, f"yb{i}", [128, D], BF16) for i in range(2)]
    t_yb = [Tile(f"yb{i}") for i in range(2)]
    t_rt_all = t_rt

    for e in range(NEXP):
        gB, uB, dB = blk_e[e]
        pi = e % 2
        for t in range(NT):
            op(DVE, lambda t=t, e=e: nc.vector.tensor_scalar(out=Psel[:, t, :], in0=C("IOTA"), scalar1=posd[:, t, e:e + 1],
                                                             scalar2=Md[:, t, e:e + 1], op0=ALU.is_equal, op1=ALU.mult),
               reads=[t_rt[t], t_const], writes=[t_Psel])
        mmgroup(PE, [lambda t=t: nc.tensor.transpose(Tp[0][:, t * 128:(t + 1) * 128], Psel[:, t, :], ident[:]) for t in range(NT)],
                reads=[t_Psel, t_ident], writes=[Tt[0]])
        op(ACT, lambda pi=pi: nc.scalar.copy(out=PselT[pi][:], in_=Tp[0][:, 0:NT * 128].rearrange("p (a t) -> p a t", a=NT)),
           reads=[Tt[0]], writes=[t_PselT[pi]])
        mmgroup(PE, [lambda t=t, e=e: nc.tensor.matmul(Fp[5][:, 0:2], lhsT=Psel[:, t, :], rhs=Ghl[:, t, e, :], start=(t == 0), stop=(t == NT - 1))
                     for t in range(NT)], reads=[t_Psel] + t_rt_all, writes=[Ft[5]])
        op(DVE, lambda: nc.vector.reduce_sum(out=gs[:, 0:1], in_=Fp[5][:, 0:2], axis=AX.X), reads=[Ft[5]], writes=[t_gs])
        for kq in range(4):
            ai = kq % 2
            fns = []
            for kk in range(4):
                k = kq * 4 + kk
                for t in range(NT):
                    fns.append(lambda k=k, kk=kk, t=t: nc.tensor.matmul(Fp[ai][:, kk * 128:(kk + 1) * 128], lhsT=H2[:, t, k * 128:(k + 1) * 128],
                                                                        rhs=Psel[:, t, :], start=(t == 0), stop=(t == NT - 1)))
            mmgroup(PE, fns, reads=[t_Psel, t_B], writes=[Ft[ai]])
            op(ACT if kq % 2 == 0 else DVE,
               (lambda kq=kq, ai=ai: nc.scalar.copy(out=xeT[:, kq * 4:(kq + 1) * 4, :], in_=Fp[ai][:, :].rearrange("p (c s) -> p c s", c=4)))
               if kq % 2 == 0 else
               (lambda kq=kq, ai=ai: nc.vector.tensor_copy(out=xeT[:, kq * 4:(kq + 1) * 4, :], in_=Fp[ai][:, :].rearrange("p (c s) -> p c s", c=4))),
               reads=[Ft[ai]], writes=[t_xeT])
        Wg, Wgt = wget(gB)
        mmgroup(PE, [lambda k=k: nc.tensor.matmul(Fp[2][:, :], lhsT=xeT[:, k, :], rhs=Wg[:, k, :], start=(k == 0), stop=(k == KC - 1))
                     for k in range(KC)], reads=[t_xeT, Wgt], writes=[Ft[2]])
        Wu, Wut = wget(uB)
        mmgroup(PE, [lambda k=k: nc.tensor.matmul(Fp[3][:, :], lhsT=xeT[:, k, :], rhs=Wu[:, k, :], start=(k == 0), stop=(k == KC - 1))
                     for k in range(KC)], reads=[t_xeT, Wut], writes=[Ft[3]])
        op(ACT, lambda: nc.scalar.activation(out=hg[:], in_=Fp[2][:, :], func=AF.Silu), reads=[Ft[2]], writes=[t_hg])
        op(DVE, lambda: nc.vector.tensor_tensor(out=hid[:], in0=hg[:], in1=Fp[3][:, :], op=ALU.mult), reads=[t_hg, Ft[3]], writes=[t_hid])
        mmgroup(PE, [lambda c=c: nc.tensor.transpose(Tp[1][:, c * 128:(c + 1) * 128], hid[:, c * 128:(c + 1) * 128], ident[:])
                     for c in range(4)], reads=[t_hid, t_ident], writes=[Tt[1]])
        op(ACT, lambda: nc.scalar.copy(out=hidT[:], in_=Tp[1][:, 0:512].rearrange("p (c s) -> p c s", c=4)), reads=[Tt[1]], writes=[t_hidT])
        Wd, Wdt = wget(dB)
        for cb in range(4):
            ai = cb % 2
            mmgroup(PE, [lambda c=c, cb=cb: nc.tensor.matmul(Fp[ai][:, :], lhsT=hidT[:, c, :], rhs=Wd[:, c, cb * 512:(cb + 1) * 512],
                                                             start=(c == 0), stop=(c == 3)) for c in range(4)],
                    reads=[t_hidT, Wdt], writes=[Ft[ai]])
            op(DVE, lambda cb=cb, ai=ai, pi=pi: nc.vector.tensor_scalar(out=yb[pi][:, cb * 512:(cb + 1) * 512], in0=Fp[ai][:, :],
                                                                        scalar1=gs[:, 0:1], scalar2=None, op0=ALU.mult),
               reads=[Ft[ai], t_gs], writes=[t_yb[pi]])
        if e % 2 == 1:
            for t in range(NT):
                for cb in range(4):
                    ai = 4 + (t * 4 + cb) % 2
                    mmgroup(PE, [lambda q=q, t=t, cb=cb: nc.tensor.matmul(Fp[ai][:, :], lhsT=PselT[q][:, t, :], rhs=yb[q][:, cb * 512:(cb + 1) * 512],
                                                                          start=(q == 0), stop=(q == 1)) for q in range(2)],
                            reads=t_PselT + t_yb, writes=[Ft[ai]])
                    op(DVE, (lambda t=t, cb=cb, ai=ai: nc.vector.tensor_tensor(out=RES[:, t, cb * 512:(cb + 1) * 512],
                                                                               in0=RES[:, t, cb * 512:(cb + 1) * 512],
                                                                               in1=Fp[ai][:, :], op=ALU.add)),
                       reads=[Ft[ai], t_res[t]], writes=[t_res[t]])

    barrier()
    s4.close()
    lnS = Scope(hi)
    lnt = sb(lnS, "lnt3", [128, 2, D], F32)
    lnst = sb(lnS, "lnst3", [128, 4, 6], F32)
    lnmv = sb(lnS, "lnmv3", [128, 8], F32)
    hb = sb(lnS, "hb3", [128, D], BF16)
    load_ln(2)
    d_out = dsem("d_out")
    for t in range(NT):
        layernorm(t, None, None)
        dma(SP, d_out, out_d[t * 128:(t + 1) * 128, :], RES[:, t, :], src=t_res[t])
    SP.wait([(d_out.sem, d_out.count)])
    if DEBUG:
        SP.wait([(d_dbg.sem, d_dbg.count)])
    barrier()
    lnS.close()
    s2.close()
    es.close()
    return nc


def kernel(x, mem, positions, w_in, w_gla_a2, b_gla_a, g_gla_norm, w_mix_out, ln1_g, ln1_b,
           w_mq, w_mk, w_mv, w_mo, ln2_g, ln2_b, w_route_group, b_route_group,
           w_route_expert, b_route_expert, w_exp_gate, w_exp_up, w_exp_down, ln3_g, ln3_b):
    f = lambda a: np.ascontiguousarray(np.asarray(a))
    x = f(x)[0][:SEQ]
    pos = f(positions)[0].astype(np.int32)[:SEQ]
    shared = {
        "consts": CONST_ARR, "consts1": CONST1_ARR,
        "w_in": _perm_w_in(f(w_in)[0]),
        "wa2b": np.ascontiguousarray(np.concatenate([f(w_gla_a2)[0], f(b_gla_a)[0][None, :]], axis=0)),
        "gnorm": f(g_gla_norm)[0][None, :].copy(),
        "w_mix": f(w_mix_out)[0], "w_mq": f(w_mq)[0], "w_mk": f(w_mk)[0], "w_mv": f(w_mv)[0], "w_mo": f(w_mo)[0],
        "memT": np.ascontiguousarray(f(mem)[0].reshape(256, KC, 128).transpose(2, 1, 0).reshape(128, KC * 256)),
        "ln": np.ascontiguousarray(np.stack([f(ln1_g)[0], f(ln1_b)[0], f(ln2_g)[0], f(ln2_b)[0], f(ln3_g)[0], f(ln3_b)[0]])),
        "wroute": np.ascontiguousarray(np.concatenate([f(w_route_group)[0], f(w_route_expert)[0]], axis=1)),
        "broute": np.ascontiguousarray(np.concatenate([f(b_route_group)[0], f(b_route_expert)[0].reshape(-1)])[None, :]),
        "w_eg": f(w_exp_gate)[0], "w_eu": f(w_exp_up)[0], "w_ed": f(w_exp_down)[0],
    }
    in_maps = []
    for c in range(NCORE):
        npad = (NCORE - 1 - c) * TOK
        xs = np.concatenate([np.zeros((npad, D), np.float32), x[0:(c + 1) * TOK]], axis=0)
        ps = np.concatenate([np.zeros((npad,), np.int32), pos[0:(c + 1) * TOK]], axis=0)
        xTp = np.ascontiguousarray(xs.reshape(NPRE + NT, 128, KC, 128).transpose(0, 3, 2, 1)).reshape(NPRE + NT, 128, D)
        m = dict(shared)
        m["xTp"] = xTp
        m["xown"] = np.ascontiguousarray(x[c * TOK:(c + 1) * TOK])
        m["posT"] = np.ascontiguousarray(ps.reshape(NPRE + NT, 128).T)
        in_maps.append(m)
    if "nc" not in _CACHE:
        _CACHE["nc"] = build_program()
    in_maps = [{k: v for k, v in m.items() if k in DECLARED} for m in in_maps]
    res = run_bass_kernel_spmd(_CACHE["nc"], in_maps, core_ids=list(range(NCORE)))
    if DEBUG:
        _CACHE["dbg"] = res.results
    out = np.concatenate([np.asarray(r["out"]) for r in res.results], axis=0).astype(np.float32)
    return out.reshape(1, SEQ, D)
```

```python
import math
from contextlib import ExitStack
import numpy as np
import concourse.bass as bass
import concourse.mybir as mybir
from concourse.bass_utils import run_bass_kernel_spmd

F32 = mybir.dt.float32
BF16 = mybir.dt.bfloat16
I32 = mybir.dt.int32
AF = mybir.ActivationFunctionType
ALU = mybir.AluOpType
AX = mybir.AxisListType

NCORE = 8
D = 2048
SEQ = 8192
TOK = SEQ // NCORE
NT = TOK // 128
NPRE = (NCORE - 1) * NT
KC = D // 128
EPS = 1e-5
ALPHA = 2.0 ** 0.25
NEXP = 32
CAP = 128
TWO_PI = 2.0 * math.pi
GAMMAS = [1.0 - 2.0 ** (-5.0 - h) for h in range(4)]

DEBUG = False
STOP = None


def configure(seq=8192, stop=None, debug=False):
    global SEQ, TOK, NT, NPRE, STOP, DEBUG
    SEQ = seq
    TOK = SEQ // NCORE
    NT = TOK // 128
    NPRE = (NCORE - 1) * NT
    STOP = stop
    DEBUG = debug
    _CACHE.clear()


_CACHE = {}
DECLARED = []


class Tile:
    def __init__(self, name="", excl=False):
        self.name = name
        self.w = []
        self.r = []
        self.excl = excl


class DSem:
    def __init__(self, nc, es, name):
        self.sem = es.enter_context(nc.semaphore(name))
        self.count = 0


class Eng:
    def __init__(self, nc, es, e, name):
        self.e = e
        self.sem = es.enter_context(nc.semaphore(name))
        self.n = 0
        self.seen = {}

    def wait(self, tks):
        best = {}
        for sem, val in tks:
            if val > best.get(sem, 0):
                best[sem] = val
        for sem, val in best.items():
            if self.seen.get(sem, 0) < val:
                self.e.wait_ge(sem, val)
                self.seen[sem] = val

    def tick(self, ins):
        self.n += 1
        ins.then_inc(self.sem, 1)
        return (self.sem, self.n)


def op(E, fn, reads=(), writes=()):
    tks = []
    for t in reads:
        tks += t.w
        if t.excl:
            tks += [k for k in t.r if k[0] is not E.sem]
    for t in writes:
        tks += t.w
        tks += t.r
    E.wait(tks)
    ins = fn()
    tk = E.tick(ins)
    for t in reads:
        t.r = [k for k in t.r if k[0] is not E.sem] + [tk]
    for t in writes:
        t.w = [tk]
        t.r = []
    return tk


def mmgroup(E, fns, reads=(), writes=()):
    tks = []
    for t in reads:
        tks += t.w
        if t.excl:
            tks += [k for k in t.r if k[0] is not E.sem]
    for t in writes:
        tks += t.w
        tks += t.r
    E.wait(tks)
    ins = None
    for f in fns:
        ins = f()
    tk = E.tick(ins)
    for t in reads:
        t.r = [k for k in t.r if k[0] is not E.sem] + [tk]
    for t in writes:
        t.w = [tk]
        t.r = []
    return tk


def dma(Q, ds, out, in_, dst=None, src=None):
    tks = []
    dsts = [] if dst is None else (list(dst) if isinstance(dst, (list, tuple)) else [dst])
    for d in dsts:
        tks += d.w + d.r
    if src is not None:
        tks += src.w
    Q.wait(tks)
    ds.count += 16
    Q.e.dma_start(out=out, in_=in_).then_inc(ds.sem, 16)
    tk = (ds.sem, ds.count)
    for d in dsts:
        d.w = [tk]
        d.r = []
    if src is not None:
        src.r = [k for k in src.r if k[0] is not ds.sem] + [tk]
    return tk


def _consts():
    c = {}
    i = np.arange(128)
    DT = np.zeros((4, 128, 128), np.float64)
    for h, g in enumerate(GAMMAS):
        rel = i[None, :] - i[:, None]
        DT[h] = np.where(rel >= 0, np.exp(np.log(g) * np.maximum(rel, 0)), 0.0) / 16.0
    c["DT"] = DT.transpose(1, 0, 2).reshape(128, 512)
    c["CAUS"] = (i[None, :] >= i[:, None]).astype(np.float64)
    c["DQ"] = np.tile(np.stack([np.exp(np.log(g) * (i + 1.0)) for g in GAMMAS]).reshape(1, 512), (128, 1))
    dk = np.zeros((128, 8))
    for h, g in enumerate(GAMMAS):
        dk[:, 2 * h] = np.exp(np.log(g) * (127.0 - i)) / 16.0
    c["DK"] = dk
    half = np.arange(128, dtype=np.float32)
    invf = (np.float32(10000.0) ** (-half / np.float32(128.0))).astype(np.float32)
    c["INVF"] = np.tile(invf.reshape(1, 128), (128, 1))
    c["UT"] = -(i[:, None] <= i[None, :]).astype(np.float64) / 16.0
    c["UT2"] = -(i[:, None] > i[None, :]).astype(np.float64) / 16.0
    c["NCOL"] = np.full((128, 1), -1.0 / 16.0)
    c["IOTA"] = np.tile(i.reshape(1, 128).astype(np.float64), (128, 1))
    c["TRIS"] = (i[:, None] < i[None, :]).astype(np.float64)
    c["ONES"] = np.ones((128, 128))
    pers = ["IOTA", "TRIS", "ONES"]
    offs = {}
    cols1, cols2 = [], []
    o = 0
    for k, v in c.items():
        if k in pers:
            continue
        offs[k] = (1, o, v.shape[1])
        o += v.shape[1]
        cols1.append(v.astype(np.float32))
    o = 0
    for k in pers:
        v = c[k]
        offs[k] = (0, o, v.shape[1])
        o += v.shape[1]
        cols2.append(v.astype(np.float32))
    return (np.ascontiguousarray(np.concatenate(cols2, axis=1)), np.ascontiguousarray(np.concatenate(cols1, axis=1)), offs)


CONST_ARR, CONST1_ARR, COFF = _consts()

A_RET = [(h * 512, 512) for h in range(4)]
A_GLA = [(2048 + h * 384, 384) for h in range(4)]
A_LR = (3584, 16)
A_W = 3600
B_RET = [(3600 + h * 512, 512) for h in range(4)]
B_GLA = [(5648 + h * 384, 384) for h in range(4)]


def _perm_w_in(w_in):
    rq, rk, rv, rg, gq, gk, gv, gg, glr = np.split(w_in, np.cumsum([1024, 1024, 1024, 1024, 512, 512, 1024, 1024])[:8], axis=1)
    cols = []
    for h in range(4):
        cols += [rk[:, h * 256:(h + 1) * 256], rv[:, h * 256:(h + 1) * 256]]
    for h in range(4):
        cols += [gk[:, h * 128:(h + 1) * 128], gv[:, h * 256:(h + 1) * 256]]
    cols += [glr]
    for h in range(4):
        cols += [rq[:, h * 256:(h + 1) * 256], rg[:, h * 256:(h + 1) * 256]]
    for h in range(4):
        cols += [gq[:, h * 128:(h + 1) * 128], gg[:, h * 256:(h + 1) * 256]]
    return np.ascontiguousarray(np.concatenate(cols, axis=1))


def build_program():
    nc = bass.Bass("TRN2", target_bir_lowering=False)
    es = ExitStack()

    del DECLARED[:]
    need = {"1b": 0, "2": 1, "3": 2, None: 3}.get(STOP, 0)
    lvl = {"w_mix": 1, "ln": 1, "xown": 1, "w_mq": 2, "w_mk": 2, "w_mv": 2, "w_mo": 2, "memT": 2,
           "wroute": 3, "broute": 3, "w_eg": 3, "w_eu": 3, "w_ed": 3}

    def din(name, shape, dt=F32):
        if lvl.get(name, 0) > need:
            return None
        DECLARED.append(name)
        return nc.dram_tensor(name, list(shape), dt, kind="ExternalInput").ap()

    xTp = din("xTp", [NPRE + NT, 128, D])
    xown = din("xown", [TOK, D])
    posT = din("posT", [128, NPRE + NT], I32)
    consts_d = din("consts", list(CONST_ARR.shape))
    consts1_d = din("consts1", list(CONST1_ARR.shape))
    w_in = din("w_in", [D, 7184])
    wa2b_d = din("wa2b", [17, 512])
    gnorm_d = din("gnorm", [1, 256])
    w_mix = din("w_mix", [D, D])
    w_mq = din("w_mq", [D, D])
    w_mk = din("w_mk", [D, D])
    w_mv = din("w_mv", [D, D])
    w_mo = din("w_mo", [D, D])
    memT_d = din("memT", [128, KC * 256])
    ln_d = din("ln", [6, D])
    wroute_d = din("wroute", [D, 36])
    broute_d = din("broute", [1, 36])
    w_eg = din("w_eg", [NEXP, D, 512])
    w_eu = din("w_eu", [NEXP, D, 512])
    w_ed = din("w_ed", [NEXP, 512, D])
    out_d = nc.dram_tensor("out", [TOK, D], F32, kind="ExternalOutput").ap()
    if DEBUG:
        dbg_mixT = nc.dram_tensor("dbg_mixT", [128, KC * TOK], BF16, kind="ExternalOutput").ap()
        dbg_h1 = nc.dram_tensor("dbg_h1", [TOK, D], F32, kind="ExternalOutput").ap()
        dbg_h2 = nc.dram_tensor("dbg_h2", [TOK, D], F32, kind="ExternalOutput").ap()

    PE = Eng(nc, es, nc.tensor, "s_pe")
    DVE = Eng(nc, es, nc.vector, "s_dve")
    ACT = Eng(nc, es, nc.scalar, "s_act")
    POOL = Eng(nc, es, nc.gpsimd, "s_pool")
    SP = Eng(nc, es, nc.sync, "s_sp")
    ENGS = [PE, DVE, ACT, POOL, SP]
    all_dsems = []

    def dsem(name):
        d = DSem(nc, es, name)
        all_dsems.append(d)
        return d

    def barrier():
        tks = [(E.sem, E.n) for E in ENGS if E.n > 0] + [(d.sem, d.count) for d in all_dsems if d.count > 0]
        for E in ENGS:
            E.wait(tks)

    class Arena:
        def __init__(self, base, limit):
            self.base, self.top, self.limit = base, base, limit

    class Scope:
        def __init__(self, arena):
            self.arena = arena
            self.mark = arena.top

        def close(self):
            self.arena.top = self.mark

    big_holder = []

    d_dbg = dsem("d_dbg") if DEBUG else None

    def finish():
        if DEBUG:
            SP.wait([(d_dbg.sem, d_dbg.count)])
        barrier()
        es.close()
        return nc

    def sb(stack, name, shape, dt):
        if not isinstance(stack, Scope):
            return stack.enter_context(nc.sbuf_tensor(name, list(shape), dt))
        ar = stack.arena
        esz = 2 if dt == BF16 else 4
        n = 1
        for d_ in shape[1:]:
            n *= d_
        nbytes = (n * esz + 63) // 64 * 64
        off = ar.top
        assert off + nbytes <= ar.limit, (name, off, nbytes, ar.limit)
        ar.top = off + nbytes
        v = big_holder[0][0:shape[0], off // 4:(off + n * esz + 3) // 4]
        if dt != F32:
            v = v.bitcast(dt)
            v = v[:, 0:n]
        if len(shape) == 3:
            v = v.rearrange("p (a b) -> p a b", a=shape[1])
        elif len(shape) == 4:
            v = v.rearrange("p (a b c) -> p a b c", a=shape[1], b=shape[2])
        return v

    CT = sb(es, "CT", CONST_ARR.shape, F32)
    CTb = sb(es, "CTb", [128, 128 * 3], BF16)
    ident = sb(es, "ident", [128, 128], BF16)
    posf = sb(es, "posf", [128, NPRE + NT], F32)
    posi = sb(es, "posi", [128, NPRE + NT], I32)
    LO_1A = 115200
    LO_AFTER = 114688
    bigw = (nc.sbuf_bytes_remaining - 256) // 64 * 16
    big_holder.append(es.enter_context(nc.sbuf_tensor("big", [128, bigw], F32)))
    lo = Arena(0, LO_1A)
    hi = Arena(LO_1A, bigw * 4)
    s1 = Scope(hi)
    CT1 = sb(s1, "CT1", CONST1_ARR.shape, F32)
    wa2b = sb(s1, "wa2bs", [17, 512], BF16)
    gnb = sb(s1, "gnb", [128, 256], F32)
    Rst = sb(s1, "Rst", [128, 4, 512], F32)
    Rb = sb(s1, "Rb", [128, 4, 512], BF16)
    Sst = sb(s1, "Sst", [128, 4, 256], F32)
    Sb = sb(s1, "Sb", [128, 4, 256], BF16)
    NSLOT = 3
    slot_t = [Tile(f"slot{i}") for i in range(NSLOT)]
    slot_ds = [dsem(f"d_slot{i}") for i in range(NSLOT)]

    Fp = [es.enter_context(nc.psum_tensor(f"F{i}", [128, 512], F32)) for i in range(6)]
    Ft = [Tile(f"F{i}", excl=True) for i in range(6)]
    Tp = [es.enter_context(nc.psum_tensor(f"T{i}", [128, 1024], BF16)) for i in range(2)]
    Tt = [Tile(f"T{i}", excl=True) for i in range(2)]

    t_const = Tile("const")
    t_ident = Tile("ident")
    t_pos = Tile("pos")
    t_R = [Tile(f"R{h}") for h in range(4)]
    t_Rb = [Tile(f"Rb{h}") for h in range(4)]
    t_S = [Tile(f"S{h}") for h in range(4)]
    t_Sb = [Tile(f"Sb{h}") for h in range(4)]
    t_A = Tile("ACT_A")
    t_B = Tile("ACT_B")
    t_Bt = [Tile(f"ACT_B{t}") for t in range(NT)]
    t_At = [Tile(f"ACT_A{t}") for t in range(NT)]

    def C(name, lo=0, hi=None):
        which, o, w = COFF[name]
        hi = w if hi is None else hi
        return (CT1 if which else CT)[:, o + lo:o + hi]

    d_c = dsem("d_const")
    dma(SP, d_c, CT[:], consts_d)
    dma(SP, d_c, CT1[:], consts1_d)
    dma(SP, d_c, posi[:], posT)
    dma(SP, d_c, gnb[:], gnorm_d.partition_broadcast(128))
    d_c2 = dsem("d_const2")
    dma(POOL, d_c2, wa2b[:], wa2b_d)
    t_const.w = [(d_c.sem, d_c.count), (d_c2.sem, d_c2.count)]
    op(POOL, lambda: nc.gpsimd.memset(ident[:], 0.0), writes=[t_ident])
    op(POOL, lambda: nc.gpsimd.affine_select(out=ident[:], in_=ident[:], pattern=[[-1, 128]], compare_op=ALU.not_equal,
                                              fill=1.0, base=0, channel_multiplier=1), reads=[t_ident], writes=[t_ident])
    op(DVE, lambda: nc.vector.tensor_copy(out=posf[:], in_=posi[:]), reads=[t_const], writes=[t_pos])
    t_ctb = Tile("ctb")
    op(DVE, lambda: nc.vector.tensor_copy(out=CTb[:, 0:128], in_=C("ONES")), reads=[t_const], writes=[t_ctb])
    op(DVE, lambda: nc.vector.tensor_copy(out=CTb[:, 128:256], in_=C("TRIS")), reads=[t_const], writes=[t_ctb])
    for h in range(4):
        op(DVE, lambda h=h: nc.vector.memset(Rst[:, h, :], 0.0), writes=[t_R[h]])
        op(DVE, lambda h=h: nc.vector.memset(Sst[:, h, :], 0.0), writes=[t_S[h]])
        op(POOL, lambda h=h: nc.gpsimd.memset(Rb[:, h, :], 0.0), writes=[t_Rb[h]])
        op(POOL, lambda h=h: nc.gpsimd.memset(Sb[:, h, :], 0.0), writes=[t_Sb[h]])

    csb = [sb(s1, f"cs{i}", [128, 256], F32) for i in range(2)]
    t_csb = [Tile(f"cs{i}") for i in range(2)]
    tr_a = sb(s1, "tr_a", [128, 256], F32)
    tr_b = sb(s1, "tr_b", [128, 256], F32)
    tr_i = sb(s1, "tr_i", [128, 256], I32)
    t_tr = Tile("tr")
    rotA = sb(s1, "rotA", [128, 256], F32)
    rotB = sb(s1, "rotB", [128, 256], F32)
    t_rot = Tile("rot")
    kbuf = [sb(s1, f"kbuf{i}", [128, 256], BF16) for i in range(2)]
    t_k = [Tile(f"k{i}") for i in range(2)]
    gkb = [sb(s1, f"gkb{i}", [128, 128], BF16) for i in range(2)]
    t_gk = [Tile(f"gk{i}") for i in range(2)]
    vbuf = [sb(s1, f"vbuf{i}", [128, 256], BF16) for i in range(2)]
    t_v = [Tile(f"v{i}") for i in range(2)]
    vhat = [sb(s1, f"vhat{i}", [128, 256], BF16) for i in range(2)]
    t_vh = [Tile(f"vh{i}") for i in range(2)]
    glr_b = sb(s1, "glr_b", [128, 16], BF16)
    t_glr = Tile("glr")
    glrT = sb(s1, "glrT", [17, 128], BF16)
    t_glrT = Tile("glrT")
    Lg = sb(s1, "Lg", [128, 512], F32)
    t_L = Tile("L")
    etmp = sb(s1, "etmp", [128, 512], F32)
    t_et = Tile("etmp")
    gt_ekb = sb(s1, "gt_ekb", [128, 512], F32)
    gt_eb = sb(s1, "gt_eb", [128, 512], F32)
    gt_enb = sb(s1, "gt_enb", [128, 512], F32)
    gt_edec = sb(s1, "gt_edec", [128, 4], F32)
    t_gt = Tile("gt")
    op(POOL, lambda: nc.gpsimd.memset(glrT[:], 1.0), writes=[t_glrT])

    def gen_cossin(T, dst2d, dst_tile):
        G = nc.vector
        op(DVE, lambda: G.tensor_scalar(out=tr_a[:, 128:256], in0=C("INVF"), scalar1=posf[:, T:T + 1], scalar2=None,
                                         op0=ALU.mult), reads=[t_const, t_pos], writes=[t_tr])
        op(DVE, lambda: G.tensor_scalar(out=tr_a[:, 0:128], in0=tr_a[:, 128:256], scalar1=math.pi / 2, scalar2=None,
                                         op0=ALU.add), reads=[t_tr], writes=[t_tr])
        op(DVE, lambda: G.tensor_scalar(out=tr_i[:], in0=tr_a[:], scalar1=1.0 / TWO_PI, scalar2=None, op0=ALU.mult),
           reads=[t_tr], writes=[t_tr])
        op(DVE, lambda: G.tensor_copy(out=tr_b[:], in_=tr_i[:]), reads=[t_tr], writes=[t_tr])
        op(DVE, lambda: G.tensor_scalar(out=tr_b[:], in0=tr_b[:], scalar1=-TWO_PI, scalar2=None, op0=ALU.mult),
           reads=[t_tr], writes=[t_tr])
        op(DVE, lambda: G.tensor_tensor(out=tr_a[:], in0=tr_a[:], in1=tr_b[:], op=ALU.add), reads=[t_tr], writes=[t_tr])
        op(DVE, lambda: G.tensor_scalar(out=tr_b[:], in0=tr_a[:], scalar1=math.pi, scalar2=TWO_PI, op0=ALU.is_gt, op1=ALU.mult),
           reads=[t_tr], writes=[t_tr])
        op(DVE, lambda: G.tensor_tensor(out=tr_a[:], in0=tr_a[:], in1=tr_b[:], op=ALU.subtract), reads=[t_tr], writes=[t_tr])
        op(DVE, lambda: G.tensor_scalar(out=tr_b[:], in0=tr_a[:], scalar1=-math.pi, scalar2=TWO_PI, op0=ALU.is_lt, op1=ALU.mult),
           reads=[t_tr], writes=[t_tr])
        op(DVE, lambda: G.tensor_tensor(out=tr_a[:], in0=tr_a[:], in1=tr_b[:], op=ALU.add), reads=[t_tr], writes=[t_tr])
        op(ACT, lambda: nc.scalar.activation(out=dst2d, in_=tr_a[:], func=AF.Sin), reads=[t_tr], writes=[dst_tile])

    def rotary(ps_ap, out_ap, extra_reads, out_tile, cs2d, t_cs):
        p3 = ps_ap.rearrange("p (a b) -> p a b", a=2)
        cosb = cs2d[:, 0:128].unsqueeze(1).to_broadcast([128, 2, 128])
        sinb = cs2d[:, 128:256].unsqueeze(1).to_broadcast([128, 2, 128])
        op(DVE, lambda: nc.vector.tensor_tensor(out=rotA[:].rearrange("p (a b) -> p a b", a=2), in0=p3, in1=cosb, op=ALU.mult),
           reads=[t_cs] + extra_reads, writes=[t_rot])
        op(DVE, lambda: nc.vector.tensor_tensor(out=rotB[:].rearrange("p (a b) -> p a b", a=2), in0=p3, in1=sinb, op=ALU.mult),
           reads=[t_cs] + extra_reads, writes=[t_rot])
        op(DVE, lambda: nc.vector.tensor_tensor(out=out_ap[:, 0:128], in0=rotA[:, 0:128], in1=rotB[:, 128:256], op=ALU.subtract),
           reads=[t_rot], writes=[out_tile])
        op(DVE, lambda: nc.vector.tensor_tensor(out=out_ap[:, 128:256], in0=rotB[:, 0:128], in1=rotA[:, 128:256], op=ALU.add),
           reads=[t_rot], writes=[out_tile])

    def proj(xT_fn, x_tiles, w_ap_fn, w_tiles, ncols, acc_i):
        mmgroup(PE, [lambda k=k: nc.tensor.matmul(Fp[acc_i][:, 0:ncols], lhsT=xT_fn(k), rhs=w_ap_fn(k),
                                                  start=(k == 0), stop=(k == KC - 1)) for k in range(KC)],
                reads=list(x_tiles) + list(w_tiles), writes=[Ft[acc_i]])

    def gates_glr(xT_fn, x_tiles, wlr_fn, w_tiles):
        mmgroup(PE, [lambda k=k: nc.tensor.matmul(Fp[5][:, 0:16], lhsT=xT_fn(k), rhs=wlr_fn(k), start=(k == 0), stop=(k == KC - 1))
                     for k in range(KC)], reads=list(x_tiles) + list(w_tiles), writes=[Ft[5]])
        op(ACT, lambda: nc.scalar.copy(out=glr_b[:], in_=Fp[5][:, 0:16]), reads=[Ft[5]], writes=[t_glr])

    def gates_z():
        op(PE, lambda: nc.tensor.transpose(Tp[1][0:16, 0:128], glr_b[:], ident[:]), reads=[t_glr, t_ident], writes=[Tt[1]])
        op(DVE, lambda: nc.vector.tensor_copy(out=glrT[0:16, :], in_=Tp[1][0:16, 0:128]), reads=[Tt[1]], writes=[t_glrT])
        op(PE, lambda: nc.tensor.matmul(Fp[5][:, :], lhsT=glrT[:, :], rhs=wa2b[:, :], start=True, stop=True),
           reads=[t_glrT, t_const], writes=[Ft[5]])
        op(ACT, lambda: nc.scalar.activation(out=etmp[:], in_=Fp[5][:, :], func=AF.Exp, scale=-1.0), reads=[Ft[5]], writes=[t_et])
        op(ACT, lambda: nc.scalar.activation(out=Lg[:], in_=etmp[:], func=AF.Ln, bias=1.0), reads=[t_et], writes=[t_L])

    def gates_L():
        op(PE, lambda: nc.tensor.matmul(Fp[5][:, :], lhsT=C("UT2"), rhs=Lg[:], start=True, stop=True),
           reads=[t_L, t_const], writes=[Ft[5]])
        op(ACT, lambda: nc.scalar.activation(out=gt_ekb[:], in_=Fp[5][:, :], func=AF.Exp), reads=[Ft[5]], writes=[t_gt])

    def gates_dec(own):
        mmgroup(PE, [lambda h=h: nc.tensor.matmul(Fp[5][:, h:h + 1], lhsT=Lg[:, h * 128:(h + 1) * 128], rhs=C("NCOL"),
                                                  start=True, stop=True) for h in range(4)],
                reads=[t_L, t_const], writes=[Ft[5]])
        op(ACT, lambda: nc.scalar.activation(out=gt_edec[:], in_=Fp[5][:, 0:4], func=AF.Exp), reads=[Ft[5]], writes=[t_gt])
        if own:
            op(PE, lambda: nc.tensor.matmul(Fp[5][:, :], lhsT=C("UT"), rhs=Lg[:], start=True, stop=True),
               reads=[t_L, t_const], writes=[Ft[5]])
            op(ACT, lambda: nc.scalar.activation(out=gt_eb[:], in_=Fp[5][:, :], func=AF.Exp), reads=[Ft[5]], writes=[t_gt])
            op(ACT, lambda: nc.scalar.activation(out=gt_enb[:], in_=Fp[5][:, :], func=AF.Exp, scale=-1.0), reads=[Ft[5]], writes=[t_gt])

    def gla_gates(T, xT_fn, x_tiles, wlr_fn, w_tiles, own):
        gates_glr(xT_fn, x_tiles, wlr_fn, w_tiles)
        gates_z()
        gates_L()
        gates_dec(own)

    def ret_state_update(h, k_ap, kt, vh_ap, vht):
        mmgroup(PE, [lambda c=c: nc.tensor.matmul(Fp[4][:, c * 256:(c + 1) * 256], lhsT=k_ap[:, c * 128:(c + 1) * 128], rhs=vh_ap,
                                                  start=True, stop=True) for c in range(2)],
                reads=[kt, vht], writes=[Ft[4]])
        op(DVE, lambda: nc.vector.scalar_tensor_tensor(out=Rst[:, h, :], in0=Rst[:, h, :], scalar=float(GAMMAS[h] ** 128),
                                                       in1=Fp[4][:, :], op0=ALU.mult, op1=ALU.add),
           reads=[Ft[4], t_R[h]], writes=[t_R[h]])

    def gla_state_update(h, khat_ap, kt, v_ap, vt):
        op(PE, lambda: nc.tensor.matmul(Fp[4][:, 0:256], lhsT=khat_ap, rhs=v_ap, start=True, stop=True),
           reads=[kt, vt], writes=[Ft[4]])
        op(DVE, lambda: nc.vector.scalar_tensor_tensor(out=Sst[:, h, :], in0=Sst[:, h, :], scalar=gt_edec[:, h:h + 1],
                                                       in1=Fp[4][:, 0:256], op0=ALU.mult, op1=ALU.add),
           reads=[Ft[4], t_S[h], t_gt], writes=[t_S[h]])

    if STOP == "s":
        return finish()
    s1a = Scope(hi)
    s1lo = Scope(lo)
    WA = sb(s1lo, "WA", [128, KC, A_W], BF16)
    t_WA = Tile("WA")
    d_wa = dsem("d_wa")
    for k0 in range(0, KC, 4):
        dma(POOL, d_wa, WA[:, k0:k0 + 4, :], w_in[k0 * 128:(k0 + 4) * 128, 0:A_W].rearrange("(k p) n -> p k n", p=128))
    t_WA.w = [(d_wa.sem, d_wa.count)]
    xt_buf = [sb(s1a, f"xt{i}", [128, KC, 128], BF16) for i in range(2)]
    t_xt = [Tile(f"xt{i}") for i in range(2)]
    d_xt = [dsem(f"d_xt{i}") for i in range(2)]

    def load_xt(T):
        i = T % 2
        dma(POOL, d_xt[i], xt_buf[i][:], xTp[T].rearrange("p (k t) -> p k t", k=KC), dst=t_xt[i])

    load_xt(0)
    gen_cossin(0, csb[0][:], t_csb[0])
    for T in range(NPRE):
        if T + 1 < NPRE:
            load_xt(T + 1)
            gen_cossin(T + 1, csb[(T + 1) % 2][:], t_csb[(T + 1) % 2])
        xb = xt_buf[T % 2]
        xt_ = t_xt[T % 2]
        xT_fn = lambda k, xb=xb: xb[:, k, :]
        cs2d, t_cs = csb[T % 2], t_csb[T % 2]

        def ret_proj(h):
            c0, n = A_RET[h]
            ai = h % 2
            proj(xT_fn, [xt_], lambda k, c0=c0, n=n: WA[:, k, c0:c0 + n], [t_WA], n, ai)
            rotary(Fp[ai][:, 0:256], kbuf[ai], [Ft[ai]], t_k[ai], cs2d, t_cs)
            op(DVE, lambda ai=ai, h=h: nc.vector.tensor_scalar(out=vhat[ai][:], in0=Fp[ai][:, 256:512], scalar1=C("DK", 2 * h, 2 * h + 1),
                                                               scalar2=None, op0=ALU.mult), reads=[Ft[ai], t_const], writes=[t_vh[ai]])

        def ret_state(h):
            ai = h % 2
            ret_state_update(h, kbuf[ai], t_k[ai], vhat[ai][:], t_vh[ai])

        def gla_proj(h):
            c0, n = A_GLA[h]
            ai = 2 + h % 2
            bi = h % 2
            proj(xT_fn, [xt_], lambda k, c0=c0, n=n: WA[:, k, c0:c0 + n], [t_WA], n, ai)
            op(DVE, lambda ai=ai, bi=bi, h=h: nc.vector.tensor_tensor(out=gkb[bi][:], in0=Fp[ai][:, 0:128],
                                                                      in1=gt_ekb[:, h * 128:(h + 1) * 128], op=ALU.mult),
               reads=[Ft[ai], t_gt], writes=[t_gk[bi]])
            op(ACT, lambda ai=ai, bi=bi: nc.scalar.copy(out=vbuf[bi][:], in_=Fp[ai][:, 128:384]), reads=[Ft[ai]], writes=[t_v[bi]])

        def gla_state(h):
            bi = h % 2
            gla_state_update(h, gkb[bi][:], t_gk[bi], vbuf[bi][:], t_v[bi])

        wlr_fn = lambda k: WA[:, k, A_LR[0]:A_LR[0] + 16]
        gates_glr(xT_fn, [xt_], wlr_fn, [t_WA])
        ret_proj(0)
        gates_z()
        ret_proj(1)
        ret_state(0)
        gates_L()
        ret_proj(2)
        ret_state(1)
        gates_dec(False)
        ret_proj(3)
        ret_state(2)
        gla_proj(0)
        ret_state(3)
        gla_proj(1)
        gla_state(0)
        gla_proj(2)
        gla_state(1)
        gla_proj(3)
        gla_state(2)
        gla_state(3)
    for h in range(4):
        op(ACT, lambda h=h: nc.scalar.copy(out=Rb[:, h, :], in_=Rst[:, h, :]), reads=[t_R[h]], writes=[t_Rb[h]])
        op(ACT, lambda h=h: nc.scalar.copy(out=Sb[:, h, :], in_=Sst[:, h, :]), reads=[t_S[h]], writes=[t_Sb[h]])
    barrier()
    if STOP == "1a":
        return finish()
    s1a.close()
    s1lo.close()
    plo = Scope(lo)
    ACT_A = sb(plo, "ACT_A", [128, KC, TOK], BF16)
    ACT_B = sb(plo, "ACT_B", [128, KC, TOK], BF16)
    slots = [sb(plo, f"wslot{i}", [128, KC * 512], BF16) for i in range(NSLOT)]
    assert lo.top <= LO_AFTER

    blocks = []

    def wblock(ap2d, kch, ncols):
        blocks.append((ap2d, kch, ncols))
        return len(blocks) - 1

    blk_retA = [wblock(w_in[:, c0:c0 + n], KC, n) for (c0, n) in A_RET]
    blk_retB = [wblock(w_in[:, c0:c0 + n], KC, n) for (c0, n) in B_RET]
    blk_glaA = [wblock(w_in[:, c0:c0 + n], KC, n) for (c0, n) in A_GLA]
    blk_glaB = [wblock(w_in[:, c0:c0 + n], KC, n) for (c0, n) in B_GLA]
    order = []
    for h in range(4):
        order += [blk_retA[h], blk_retB[h]]
    for h in range(4):
        order += [blk_glaA[h], blk_glaB[h]]
    if need >= 1:
        blk_mix = [wblock(w_mix[:, j * 512:(j + 1) * 512], KC, 512) for j in range(4)]
        order += blk_mix
    if need >= 2:
        blk_mk = [wblock(w_mk[:, j * 512:(j + 1) * 512], KC, 512) for j in range(4)]
        blk_mv = [wblock(w_mv[:, j * 512:(j + 1) * 512], KC, 512) for j in range(4)]
        blk_mq = [wblock(w_mq[:, j * 512:(j + 1) * 512], KC, 512) for j in range(4)]
        blk_mo = [wblock(w_mo[:, j * 512:(j + 1) * 512], KC, 512) for j in range(4)]
        order += blk_mk + blk_mv + blk_mq + blk_mo
    blk_e = []
    for e in range(NEXP if need >= 3 else 0):
        g_ = wblock(w_eg[e], KC, 512)
        u_ = wblock(w_eu[e], KC, 512)
        d_ = wblock(w_ed[e], 4, 2048)
        blk_e.append((g_, u_, d_))
        order += [g_, u_, d_]
    pos_in_order = {b: i for i, b in enumerate(order)}
    issued = [0]
    blk_slot = {}

    def issue_upto(i):
        while issued[0] <= min(i, len(order) - 1):
            j = issued[0]
            b = order[j]
            ap2d, kch, ncols = blocks[b]
            s = j % NSLOT
            dst = slots[s][:, 0:kch * ncols].rearrange("p (k n) -> p k n", k=kch)
            dma(POOL, slot_ds[s], dst, ap2d.rearrange("(k p) n -> p k n", p=128), dst=slot_t[s])
            blk_slot[b] = s
            issued[0] += 1

    def wget(b, pf=2):
        i = pos_in_order[b]
        issue_upto(i + pf)
        s = blk_slot[b]
        ap2d, kch, ncols = blocks[b]
        v = slots[s][:, 0:kch * ncols].rearrange("p (k n) -> p k n", k=kch)
        return v, slot_t[s]

    d_xo = dsem("d_xo")
    for t in range(NT):
        dma(POOL, d_xo, ACT_A[:, :, t * 128:(t + 1) * 128], xTp[NPRE + t].rearrange("p (k t) -> p k t", k=KC))
    t_A.w = [(d_xo.sem, d_xo.count)]

    own = Scope(hi)
    cs_own = sb(own, "cs_own", [128, NT, 256], F32)
    t_cso = Tile("cs_own")
    g_ekb = sb(own, "g_ekb", [128, NT, 512], BF16)
    g_eb = sb(own, "g_eb", [128, NT, 512], BF16)
    g_enb = sb(own, "g_enb", [128, NT, 512], BF16)
    g_edec = sb(own, "g_edec", [128, NT, 4], F32)
    t_gown = Tile("gown")
    qbuf = sb(own, "qbuf", [128, 256], BF16)
    t_q = Tile("q")
    sgbuf = sb(own, "sgbuf", [128, 256], BF16)
    t_sg = Tile("sg")
    qkT = sb(own, "qkT", [128, 6, 128], BF16)
    t_qkT = Tile("qkT")
    sTb = sb(own, "sTb", [128, 128], BF16)
    t_sT = Tile("sT")
    stat = sb(own, "stat", [128, 8], F32)
    t_stat = Tile("stat")
    bst = sb(own, "bst", [128, 6], F32)
    ynorm = sb(own, "ynorm", [128, 256], F32)
    t_yn = Tile("yn")
    mixb = sb(own, "mixb", [128, 256], BF16)
    t_mixb = Tile("mixb")
    junk = ynorm
    t_junk = t_yn

    def xTown(t):
        return lambda k: ACT_A[:, k, t * 128:(t + 1) * 128]

    for t in range(NT):
        gen_cossin(NPRE + t, cs_own[:, t, :], t_cso)

    def finish_head_tile(hcol, t, is_ret, o_acc):
        o_ps = Fp[o_acc][:, 0:256]
        if is_ret:
            op(DVE, lambda: nc.vector.bn_stats(out=bst[:], in_=o_ps), reads=[Ft[o_acc]], writes=[t_stat])
            op(DVE, lambda: nc.vector.bn_aggr(out=stat[:, 0:2], in_=bst[:]), reads=[t_stat], writes=[t_stat])
            op(DVE, lambda: nc.vector.tensor_scalar(out=stat[:, 2:3], in0=stat[:, 1:2], scalar1=EPS, scalar2=None, op0=ALU.add),
               reads=[t_stat], writes=[t_stat])
        else:
            op(DVE, lambda: nc.vector.memset(stat[:, 0:1], 0.0), writes=[t_stat])
            op(ACT, lambda: nc.scalar.activation(out=junk[:], in_=o_ps, func=AF.Square, accum_out=stat[:, 0:1]),
               reads=[Ft[o_acc]], writes=[t_junk, t_stat])
            op(DVE, lambda: nc.vector.tensor_scalar(out=stat[:, 2:3], in0=stat[:, 0:1], scalar1=1.0 / 256.0, scalar2=EPS,
                                                    op0=ALU.mult, op1=ALU.add), reads=[t_stat], writes=[t_stat])
        op(ACT, lambda: nc.scalar.activation(out=stat[:, 3:4], in_=stat[:, 2:3], func=AF.Sqrt), reads=[t_stat], writes=[t_stat])
        op(DVE, lambda: nc.vector.reciprocal(out=stat[:, 4:5], in_=stat[:, 3:4]), reads=[t_stat], writes=[t_stat])
        if is_ret:
            op(DVE, lambda: nc.vector.tensor_scalar(out=ynorm[:], in0=o_ps, scalar1=stat[:, 0:1], scalar2=stat[:, 4:5],
                                                    op0=ALU.subtract, op1=ALU.mult), reads=[Ft[o_acc], t_stat], writes=[t_yn])
            op(DVE, lambda: nc.vector.tensor_tensor(out=mixb[:], in0=ynorm[:], in1=sgbuf[:], op=ALU.mult),
               reads=[t_yn, t_sg], writes=[t_mixb])
        else:
            op(DVE, lambda: nc.vector.scalar_tensor_tensor(out=mixb[:], in0=o_ps, scalar=stat[:, 4:5], in1=sgbuf[:],
                                                           op0=ALU.mult, op1=ALU.mult), reads=[Ft[o_acc], t_stat, t_sg], writes=[t_mixb])
        mmgroup(PE, [lambda c=c: nc.tensor.transpose(Tp[1][:, c * 128:(c + 1) * 128], mixb[:, c * 128:(c + 1) * 128], ident[:])
                     for c in range(2)], reads=[t_mixb, t_ident], writes=[Tt[1]])
        kk = hcol * 2
        op(ACT, lambda: nc.scalar.copy(out=ACT_B[:, kk:kk + 2, t * 128:(t + 1) * 128],
                                       in_=Tp[1][:, 0:256].rearrange("p (c t) -> p c t", c=2)), reads=[Tt[1]], writes=[t_Bt[t]])

    for h in range(4):
        WAv, WAt = wget(blk_retA[h], 2)
        WBv, WBt = wget(blk_retB[h], 1)
        for t in range(NT):
            xf = xTown(t)
            proj(xf, [t_A], lambda k: WAv[:, k, :], [WAt], 512, 0)
            proj(xf, [t_A], lambda k: WBv[:, k, :], [WBt], 512, 1)
            rotary(Fp[0][:, 0:256], kbuf[0], [Ft[0]], t_k[0], cs_own[:, t, :], t_cso)
            rotary(Fp[1][:, 0:256], qbuf, [Ft[1]], t_q, cs_own[:, t, :], t_cso)
            op(ACT, lambda: nc.scalar.copy(out=vbuf[0][:], in_=Fp[0][:, 256:512]), reads=[Ft[0]], writes=[t_v[0]])
            op(DVE, lambda h=h: nc.vector.tensor_scalar(out=vhat[0][:], in0=Fp[0][:, 256:512], scalar1=C("DK", 2 * h, 2 * h + 1),
                                                        scalar2=None, op0=ALU.mult), reads=[Ft[0], t_const], writes=[t_vh[0]])
            op(ACT, lambda: nc.scalar.activation(out=sgbuf[:], in_=Fp[1][:, 256:512], func=AF.Silu), reads=[Ft[1]], writes=[t_sg])
            mmgroup(PE, [lambda c=c: nc.tensor.transpose(Tp[0][:, c * 128:(c + 1) * 128], qbuf[:, c * 128:(c + 1) * 128], ident[:])
                         for c in range(2)] +
                        [lambda c=c: nc.tensor.transpose(Tp[0][:, (2 + c) * 128:(3 + c) * 128], kbuf[0][:, c * 128:(c + 1) * 128], ident[:])
                         for c in range(2)], reads=[t_q, t_k[0], t_ident], writes=[Tt[0]])
            op(ACT, lambda: nc.scalar.copy(out=qkT[:, 0:4, :], in_=Tp[0][:, 0:512].rearrange("p (c t) -> p c t", c=4)),
               reads=[Tt[0]], writes=[t_qkT])
            op(DVE, lambda h=h: nc.vector.tensor_tensor(out=qkT[:, 4:6, :], in0=Tp[0][:, 0:256].rearrange("p (c t) -> p c t", c=2),
                                                        in1=C("DQ", h * 128, (h + 1) * 128).unsqueeze(1).to_broadcast([128, 2, 128]),
                                                        op=ALU.mult), reads=[Tt[0], t_const], writes=[t_qkT])
            mmgroup(PE, [lambda c=c: nc.tensor.matmul(Fp[2][:, 0:128], lhsT=qkT[:, 2 + c, :], rhs=qkT[:, c, :], start=(c == 0), stop=(c == 1))
                         for c in range(2)], reads=[t_qkT], writes=[Ft[2]])
            op(DVE, lambda h=h: nc.vector.tensor_tensor(out=sTb[:], in0=Fp[2][:, 0:128], in1=C("DT", h * 128, (h + 1) * 128), op=ALU.mult),
               reads=[Ft[2], t_const], writes=[t_sT])
            mmgroup(PE, [lambda: nc.tensor.matmul(Fp[3][:, 0:256], lhsT=sTb[:], rhs=vbuf[0][:], start=True, stop=False)] +
                        [lambda c=c, h=h: nc.tensor.matmul(Fp[3][:, 0:256], lhsT=qkT[:, 4 + c, :], rhs=Rb[:, h, c * 256:(c + 1) * 256],
                                                           start=False, stop=(c == 1)) for c in range(2)],
                    reads=[t_sT, t_v[0], t_qkT, t_Rb[h]], writes=[Ft[3]])
            ret_state_update(h, kbuf[0], t_k[0], vhat[0][:], t_vh[0])
            op(ACT, lambda h=h: nc.scalar.copy(out=Rb[:, h, :], in_=Rst[:, h, :]), reads=[t_R[h]], writes=[t_Rb[h]])
            finish_head_tile(h, t, True, 3)

    WAv0, WAt0 = None, None
    d_lr = dsem("d_lr")
    wlr = sb(own, "wlr", [128, KC, 16], BF16)
    t_wlr = Tile("wlr")
    dma(POOL, d_lr, wlr[:], w_in[:, A_LR[0]:A_LR[0] + 16].rearrange("(k p) n -> p k n", p=128), dst=t_wlr)
    for t in range(NT):
        gla_gates(NPRE + t, xTown(t), [t_A], lambda k: wlr[:, k, :], [t_wlr], own=True)
        op(DVE, lambda t=t: nc.vector.tensor_copy(out=g_ekb[:, t, :], in_=gt_ekb[:]), reads=[t_gt], writes=[t_gown])
        op(DVE, lambda t=t: nc.vector.tensor_copy(out=g_eb[:, t, :], in_=gt_eb[:]), reads=[t_gt], writes=[t_gown])
        op(DVE, lambda t=t: nc.vector.tensor_copy(out=g_enb[:, t, :], in_=gt_enb[:]), reads=[t_gt], writes=[t_gown])
        op(DVE, lambda t=t: nc.vector.tensor_copy(out=g_edec[:, t, :], in_=gt_edec[:]), reads=[t_gt], writes=[t_gown])

    for h in range(4):
        WAv, WAt = wget(blk_glaA[h], 2)
        WBv, WBt = wget(blk_glaB[h], 1)
        hs = slice(h * 128, (h + 1) * 128)
        for t in range(NT):
            xf = xTown(t)
            proj(xf, [t_A], lambda k: WAv[:, k, :], [WAt], 384, 0)
            proj(xf, [t_A], lambda k: WBv[:, k, :], [WBt], 384, 1)
            op(DVE, lambda t=t: nc.vector.tensor_tensor(out=kbuf[0][:, 0:128], in0=Fp[0][:, 0:128], in1=g_enb[:, t, hs], op=ALU.mult),
               reads=[Ft[0], t_gown], writes=[t_k[0]])
            op(DVE, lambda t=t: nc.vector.tensor_tensor(out=kbuf[1][:, 0:128], in0=Fp[0][:, 0:128], in1=g_ekb[:, t, hs], op=ALU.mult),
               reads=[Ft[0], t_gown], writes=[t_k[1]])
            op(DVE, lambda t=t: nc.vector.scalar_tensor_tensor(out=qbuf[:, 0:128], in0=Fp[1][:, 0:128], scalar=128.0 ** -0.5,
                                                               in1=g_eb[:, t, hs], op0=ALU.mult, op1=ALU.mult),
               reads=[Ft[1], t_gown], writes=[t_q])
            op(ACT, lambda: nc.scalar.copy(out=vbuf[0][:], in_=Fp[0][:, 128:384]), reads=[Ft[0]], writes=[t_v[0]])
            op(ACT, lambda: nc.scalar.activation(out=ynorm[:], in_=Fp[1][:, 128:384], func=AF.Silu), reads=[Ft[1]], writes=[t_yn])
            op(DVE, lambda: nc.vector.tensor_tensor(out=sgbuf[:], in0=ynorm[:], in1=gnb[:], op=ALU.mult),
               reads=[t_yn, t_const], writes=[t_sg])
            mmgroup(PE, [lambda: nc.tensor.transpose(Tp[0][:, 0:128], qbuf[:, 0:128], ident[:]),
                         lambda: nc.tensor.transpose(Tp[0][:, 128:256], kbuf[0][:, 0:128], ident[:])],
                    reads=[t_q, t_k[0], t_ident], writes=[Tt[0]])
            op(ACT, lambda: nc.scalar.copy(out=qkT[:, 0:2, :], in_=Tp[0][:, 0:256].rearrange("p (c t) -> p c t", c=2)),
               reads=[Tt[0]], writes=[t_qkT])
            op(PE, lambda: nc.tensor.matmul(Fp[2][:, 0:128], lhsT=qkT[:, 1, :], rhs=qkT[:, 0, :], start=True, stop=True),
               reads=[t_qkT], writes=[Ft[2]])
            op(DVE, lambda: nc.vector.tensor_tensor(out=sTb[:], in0=Fp[2][:, 0:128], in1=C("CAUS"), op=ALU.mult),
               reads=[Ft[2], t_const], writes=[t_sT])
            mmgroup(PE, [lambda: nc.tensor.matmul(Fp[3][:, 0:256], lhsT=sTb[:], rhs=vbuf[0][:], start=True, stop=False),
                         lambda h=h: nc.tensor.matmul(Fp[3][:, 0:256], lhsT=qkT[:, 0, :], rhs=Sb[:, h, :], start=False, stop=True)],
                    reads=[t_sT, t_v[0], t_qkT, t_Sb[h]], writes=[Ft[3]])
            op(PE, lambda: nc.tensor.matmul(Fp[4][:, 0:256], lhsT=kbuf[1][:, 0:128], rhs=vbuf[0][:], start=True, stop=True),
               reads=[t_k[1], t_v[0]], writes=[Ft[4]])
            op(DVE, lambda h=h, t=t: nc.vector.scalar_tensor_tensor(out=Sst[:, h, :], in0=Sst[:, h, :], scalar=g_edec[:, t, h:h + 1],
                                                                    in1=Fp[4][:, 0:256], op0=ALU.mult, op1=ALU.add),
               reads=[Ft[4], t_S[h], t_gown], writes=[t_S[h]])
            op(ACT, lambda h=h: nc.scalar.copy(out=Sb[:, h, :], in_=Sst[:, h, :]), reads=[t_S[h]], writes=[t_Sb[h]])
            finish_head_tile(4 + h, t, False, 3)

    if DEBUG:
        for t in range(NT):
            t_B.w += t_Bt[t].w
        dma(SP, d_dbg, dbg_mixT, ACT_B[:].rearrange("p k t -> p (k t)"), src=t_B)
    barrier()
    own.close()
    s1.close()
    if STOP == "1b":
        return finish()

    assert hi.top == hi.base
    hi.base = hi.top = LO_AFTER
    s2 = Scope(hi)
    RES = sb(s2, "RES", [128, NT, D], F32)
    t_res = [Tile(f"res{t}") for t in range(NT)]
    t_ln = Tile("ln")
    d_ln = dsem("d_ln")
    d_x = dsem("d_x")
    for t in range(NT):
        dma(SP, d_x, RES[:, t, :], xown[t * 128:(t + 1) * 128, :])
    for t in range(NT):
        t_res[t].w = [(d_x.sem, d_x.count)]
    t_lnst = Tile("lnst")
    t_hb = Tile("hb")
    lnS = Scope(hi)
    lnt = sb(lnS, "lnt", [128, 2, D], F32)
    lnst = sb(lnS, "lnst", [128, 4, 6], F32)
    lnmv = sb(lnS, "lnmv", [128, 8], F32)
    hb = sb(lnS, "hb", [128, D], BF16)

    def load_ln(i):
        dma(SP, d_ln, lnt[:, 0, :], ln_d[2 * i:2 * i + 1, :].partition_broadcast(128), dst=t_ln)
        dma(SP, d_ln, lnt[:, 1, :], ln_d[2 * i + 1:2 * i + 2, :].partition_broadcast(128), dst=t_ln)

    def linear(AT, at_tiles, blks, epilogue, ntiles=NT):
        for j, b in enumerate(blks):
            Wv, Wt = wget(b)
            for t in range(ntiles):
                ai = (j * ntiles + t) % 2
                mmgroup(PE, [lambda k=k, t=t: nc.tensor.matmul(Fp[ai][:, :], lhsT=AT[:, k, t * 128:(t + 1) * 128], rhs=Wv[:, k, :],
                                                               start=(k == 0), stop=(k == KC - 1)) for k in range(KC)],
                        reads=list(at_tiles(t)) + [Wt], writes=[Ft[ai]])
                epilogue(j, t, ai)

    def resid_epi(j, t, ai):
        op(DVE, lambda: nc.vector.scalar_tensor_tensor(out=RES[:, t, j * 512:(j + 1) * 512], in0=RES[:, t, j * 512:(j + 1) * 512],
                                                       scalar=ALPHA, in1=Fp[ai][:, :], op0=ALU.mult, op1=ALU.add),
           reads=[Ft[ai], t_res[t]], writes=[t_res[t]])

    def layernorm(t, dstT, dst_tiles, want_tok_bf16=None):
        for c in range(4):
            op(DVE, lambda c=c: nc.vector.bn_stats(out=lnst[:, c, :], in_=RES[:, t, c * 512:(c + 1) * 512]),
               reads=[t_res[t]], writes=[t_lnst])
        op(DVE, lambda: nc.vector.bn_aggr(out=lnmv[:, 0:2], in_=lnst[:].rearrange("p a b -> p (a b)")), reads=[t_lnst], writes=[t_lnst])
        op(DVE, lambda: nc.vector.tensor_scalar(out=lnmv[:, 2:3], in0=lnmv[:, 1:2], scalar1=EPS, scalar2=None, op0=ALU.add),
           reads=[t_lnst], writes=[t_lnst])
        op(ACT, lambda: nc.scalar.activation(out=lnmv[:, 3:4], in_=lnmv[:, 2:3], func=AF.Sqrt), reads=[t_lnst], writes=[t_lnst])
        op(DVE, lambda: nc.vector.reciprocal(out=lnmv[:, 4:5], in_=lnmv[:, 3:4]), reads=[t_lnst], writes=[t_lnst])
        op(DVE, lambda: nc.vector.tensor_scalar(out=RES[:, t, :], in0=RES[:, t, :], scalar1=lnmv[:, 0:1], scalar2=lnmv[:, 4:5],
                                                op0=ALU.subtract, op1=ALU.mult), reads=[t_res[t], t_lnst], writes=[t_res[t]])
        op(DVE, lambda: nc.vector.tensor_tensor(out=RES[:, t, :], in0=RES[:, t, :], in1=lnt[:, 0, :], op=ALU.mult),
           reads=[t_res[t], t_ln], writes=[t_res[t]])
        op(DVE, lambda: nc.vector.tensor_tensor(out=RES[:, t, :], in0=RES[:, t, :], in1=lnt[:, 1, :], op=ALU.add),
           reads=[t_res[t], t_ln], writes=[t_res[t]])
        if dstT is not None:
            hbt = hb[:] if want_tok_bf16 is None else want_tok_bf16
            hbt_tile = t_hb
            op(ACT, lambda: nc.scalar.copy(out=hbt, in_=RES[:, t, :]), reads=[t_res[t]], writes=[hbt_tile])
            for half in range(2):
                mmgroup(PE, [lambda c=c: nc.tensor.transpose(Tp[half][:, c * 128:(c + 1) * 128],
                                                             hbt[:, (half * 8 + c) * 128:(half * 8 + c + 1) * 128], ident[:])
                             for c in range(8)], reads=[hbt_tile, t_ident], writes=[Tt[half]])
                op(ACT if half == 0 else DVE,
                   (lambda half=half: nc.scalar.copy(out=dstT[:, half * 8:(half + 1) * 8, t * 128:(t + 1) * 128],
                                                     in_=Tp[half][:].rearrange("p (c t) -> p c t", c=8))) if half == 0 else
                   (lambda half=half: nc.vector.tensor_copy(out=dstT[:, half * 8:(half + 1) * 8, t * 128:(t + 1) * 128],
                                                            in_=Tp[half][:].rearrange("p (c t) -> p c t", c=8))),
                   reads=[Tt[half]], writes=[dst_tiles[t]])

    load_ln(0)
    linear(ACT_B, lambda t: [t_Bt[t]], blk_mix, resid_epi)
    for t in range(NT):
        layernorm(t, ACT_A, t_At)
    if DEBUG:
        for t in range(NT):
            dma(SP, d_dbg, dbg_h1[t * 128:(t + 1) * 128, :], RES[:, t, :], src=t_res[t])

    barrier()
    lnS.close()
    if STOP == "2":
        return finish()
    s3 = Scope(hi)
    memT = ACT_B[:, :, 0:256]
    mem_tiles = [t_Bt[0], t_Bt[1]]
    d_mem = dsem("d_mem")
    dma(POOL, d_mem, memT, memT_d.rearrange("p (k m) -> p k m", k=KC), dst=mem_tiles)
    KT = sb(s3, "KT", [128, KC, 256], BF16)
    t_KT = Tile("KT")
    Vm = sb(s3, "Vm", [128, 2, D], BF16)
    t_Vm = Tile("Vm")
    kvb = sb(s3, "kvb", [128, 512], BF16)
    t_kvb = Tile("kvb")
    qb = sb(s3, "qb", [128, 512], BF16)
    t_qb = Tile("qb")
    qTh = sb(s3, "qTh", [128, 4, 128], BF16)
    t_qTh = Tile("qTh")
    pexp = sb(s3, "pexp", [128, 256], BF16)
    t_pexp = Tile("pexp")
    pT = sb(s3, "pT", [128, 2, 128], BF16)
    t_pT = Tile("pT")
    sm = sb(s3, "sm", [128, 8], F32)
    t_sm = Tile("sm")
    ob = sb(s3, "ob", [128, 512], BF16)
    t_ob = Tile("ob")

    def k_epi(j, mt, ai):
        op(ACT, lambda: nc.scalar.copy(out=kvb[:], in_=Fp[ai][:, :]), reads=[Ft[ai]], writes=[t_kvb])
        mmgroup(PE, [lambda c=c: nc.tensor.transpose(Tp[0][:, c * 128:(c + 1) * 128], kvb[:, c * 128:(c + 1) * 128], ident[:])
                     for c in range(4)], reads=[t_kvb, t_ident], writes=[Tt[0]])
        op(DVE, lambda: nc.vector.tensor_copy(out=KT[:, j * 4:(j + 1) * 4, mt * 128:(mt + 1) * 128],
                                              in_=Tp[0][:, 0:512].rearrange("p (c t) -> p c t", c=4)), reads=[Tt[0]], writes=[t_KT])

    def v_epi(j, mt, ai):
        op(ACT, lambda: nc.scalar.copy(out=Vm[:, mt, j * 512:(j + 1) * 512], in_=Fp[ai][:, :]), reads=[Ft[ai]], writes=[t_Vm])

    linear(memT, lambda t: mem_tiles, blk_mk, k_epi, ntiles=2)
    linear(memT, lambda t: mem_tiles, blk_mv, v_epi, ntiles=2)

    SCL = 512.0 ** -0.5

    def q_epi(j, t, ai):
        op(ACT, lambda: nc.scalar.mul(out=qb[:], in_=Fp[ai][:, :], mul=SCL), reads=[Ft[ai]], writes=[t_qb])
        mmgroup(PE, [lambda c=c: nc.tensor.transpose(Tp[0][:, c * 128:(c + 1) * 128], qb[:, c * 128:(c + 1) * 128], ident[:])
                     for c in range(4)], reads=[t_qb, t_ident], writes=[Tt[0]])
        op(DVE, lambda: nc.vector.tensor_copy(out=qTh[:], in_=Tp[0][:, 0:512].rearrange("p (c t) -> p c t", c=4)),
           reads=[Tt[0]], writes=[t_qTh])
        mmgroup(PE, [lambda c=c: nc.tensor.matmul(Fp[2][:, 0:256], lhsT=qTh[:, c, :], rhs=KT[:, j * 4 + c, :], start=(c == 0), stop=(c == 3))
                     for c in range(4)], reads=[t_qTh, t_KT], writes=[Ft[2]])
        op(DVE, lambda: nc.vector.reduce_max(out=sm[:, 0:1], in_=Fp[2][:, 0:256], axis=AX.X), reads=[Ft[2]], writes=[t_sm])
        op(DVE, lambda: nc.vector.tensor_scalar(out=sm[:, 2:3], in0=sm[:, 0:1], scalar1=-1.0, scalar2=None, op0=ALU.mult),
           reads=[t_sm], writes=[t_sm])
        op(DVE, lambda: nc.vector.memset(sm[:, 4:5], 0.0), writes=[t_sm])
        op(ACT, lambda: nc.scalar.activation(out=pexp[:], in_=Fp[2][:, 0:256], func=AF.Exp, bias=sm[:, 2:3], accum_out=sm[:, 4:5]),
           reads=[Ft[2], t_sm], writes=[t_pexp, t_sm])
        op(DVE, lambda: nc.vector.reciprocal(out=sm[:, 6:7], in_=sm[:, 4:5]), reads=[t_sm], writes=[t_sm])
        mmgroup(PE, [lambda c=c: nc.tensor.transpose(Tp[1][:, c * 128:(c + 1) * 128], pexp[:, c * 128:(c + 1) * 128], ident[:])
                     for c in range(2)], reads=[t_pexp, t_ident], writes=[Tt[1]])
        op(DVE, lambda: nc.vector.tensor_copy(out=pT[:], in_=Tp[1][:, 0:256].rearrange("p (c t) -> p c t", c=2)),
           reads=[Tt[1]], writes=[t_pT])
        mmgroup(PE, [lambda mt=mt: nc.tensor.matmul(Fp[3][:, :], lhsT=pT[:, mt, :], rhs=Vm[:, mt, j * 512:(j + 1) * 512],
                                                    start=(mt == 0), stop=(mt == 1)) for mt in range(2)],
                reads=[t_pT, t_Vm], writes=[Ft[3]])
        op(DVE, lambda: nc.vector.tensor_scalar(out=ob[:], in0=Fp[3][:, :], scalar1=sm[:, 6:7], scalar2=None, op0=ALU.mult),
           reads=[Ft[3], t_sm], writes=[t_ob])
        mmgroup(PE, [lambda c=c: nc.tensor.transpose(Tp[0][:, c * 128:(c + 1) * 128], ob[:, c * 128:(c + 1) * 128], ident[:])
                     for c in range(4)], reads=[t_ob, t_ident], writes=[Tt[0]])
        op(DVE, lambda: nc.vector.tensor_copy(out=ACT_B[:, j * 4:(j + 1) * 4, t * 128:(t + 1) * 128],
                                              in_=Tp[0][:, 0:512].rearrange("p (c t) -> p c t", c=4)), reads=[Tt[0]], writes=[t_Bt[t]])

    linear(ACT_A, lambda t: [t_At[t]], blk_mq, q_epi)
    linear(ACT_B, lambda t: [t_Bt[t]], blk_mo, resid_epi)
    barrier()
    s3.close()

    lnS = Scope(hi)
    lnt = sb(lnS, "lnt2", [128, 2, D], F32)
    lnst = sb(lnS, "lnst2", [128, 4, 6], F32)
    lnmv = sb(lnS, "lnmv2", [128, 8], F32)
    hb = sb(lnS, "hb2", [128, D], BF16)
    load_ln(1)
    for t in range(NT):
        layernorm(t, ACT_A, t_At)
    if DEBUG:
        for t in range(NT):
            dma(SP, d_dbg, dbg_h2[t * 128:(t + 1) * 128, :], RES[:, t, :], src=t_res[t])
    barrier()
    lnS.close()
    if STOP == "3":
        return finish()
    s4 = Scope(hi)
    wr = sb(s4, "wr", [128, KC, 36], BF16)
    t_wr = Tile("wr")
    d_wr = dsem("d_wr")
    dma(POOL, d_wr, wr[:], wroute_d.rearrange("(k p) n -> p k n", p=128), dst=t_wr)
    brt = sb(s4, "brt", [128, 36], F32)
    d_wr2 = dsem("d_wr2")
    dma(SP, d_wr2, brt[:], broute_d.partition_broadcast(128))
    t_wr.w = [(d_wr.sem, d_wr.count), (d_wr2.sem, d_wr2.count)]
    lg = sb(s4, "lg", [128, 36], F32)
    t_lg = Tile("lg")
    rs = sb(s4, "rs", [128, 16], F32)
    t_rs = Tile("rs")
    ohg = sb(s4, "ohg", [128, 4], F32)
    f48 = sb(s4, "f48", [128, 32], F32)
    fsel = sb(s4, "fsel", [128, 8], F32)
    oh1 = sb(s4, "oh1", [128, 8], F32)
    oh2 = sb(s4, "oh2", [128, 8], F32)
    fm = sb(s4, "fm", [128, 8], F32)
    gw8 = sb(s4, "gw8", [128, 8], F32)
    Gd = sb(s4, "Gd", [128, NT, 32], F32)
    Md = sb(s4, "Md", [128, NT, 32], F32)
    Mb = sb(s4, "Mb", [128, NT, 32], BF16)
    Ghl = sb(s4, "Ghl", [128, NT, 32, 2], BF16)
    Gtmp = sb(s4, "Gtmp", [128, 32], F32)
    posd = sb(s4, "posd", [128, NT, 32], F32)
    t_rt = [Tile(f"rt{t}") for t in range(NT)]
    t_M = Tile("M")

    H2 = ACT_B[:].rearrange("p k t -> p (k t)").rearrange("p (a d) -> p a d", a=NT)
    for t in range(NT):
        op(ACT, lambda t=t: nc.scalar.copy(out=H2[:, t, :], in_=RES[:, t, :]), reads=[t_res[t]], writes=t_Bt + [t_B])
    for t in range(NT):
        mmgroup(PE, [lambda k=k, t=t: nc.tensor.matmul(Fp[2][:, 0:36], lhsT=ACT_A[:, k, t * 128:(t + 1) * 128], rhs=wr[:, k, :],
                                                       start=(k == 0), stop=(k == KC - 1)) for k in range(KC)],
                reads=[t_At[t], t_wr], writes=[Ft[2]])
        R_ = [t_lg, t_rs]
        op(DVE, lambda: nc.vector.tensor_tensor(out=lg[:], in0=Fp[2][:, 0:36], in1=brt[:], op=ALU.add), reads=[Ft[2], t_wr], writes=[t_lg])
        op(DVE, lambda: nc.vector.reduce_max(out=rs[:, 0:1], in_=lg[:, 0:4], axis=AX.X), reads=[t_lg], writes=[t_rs])
        op(DVE, lambda: nc.vector.tensor_scalar(out=ohg[:], in0=lg[:, 0:4], scalar1=rs[:, 0:1], scalar2=None, op0=ALU.is_equal),
           reads=R_, writes=[t_rs])
        op(DVE, lambda: nc.vector.tensor_scalar(out=rs[:, 14:15], in0=rs[:, 0:1], scalar1=-1.0, scalar2=None, op0=ALU.mult),
           reads=R_, writes=[t_rs])
        op(DVE, lambda: nc.vector.memset(rs[:, 2:3], 0.0), reads=R_, writes=[t_rs])
        op(ACT, lambda: nc.scalar.activation(out=rs[:, 4:8], in_=lg[:, 0:4], func=AF.Exp, bias=rs[:, 14:15], accum_out=rs[:, 2:3]),
           reads=R_, writes=[t_rs])
        op(DVE, lambda: nc.vector.reciprocal(out=rs[:, 3:4], in_=rs[:, 2:3]), reads=R_, writes=[t_rs])
        op(DVE, lambda: nc.vector.tensor_tensor(out=f48[:].rearrange("p (g e) -> p g e", g=4),
                                                in0=lg[:, 4:36].rearrange("p (g e) -> p g e", g=4),
                                                in1=ohg[:].unsqueeze(2).to_broadcast([128, 4, 8]), op=ALU.mult), reads=R_, writes=[t_rs])
        op(DVE, lambda: nc.vector.reduce_sum(out=fsel[:], in_=f48[:].rearrange("p (g e) -> p e g", g=4), axis=AX.X),
           reads=R_, writes=[t_rs])
        op(DVE, lambda: nc.vector.reduce_max(out=rs[:, 8:9], in_=fsel[:], axis=AX.X), reads=R_, writes=[t_rs])
        op(DVE, lambda: nc.vector.tensor_scalar(out=oh1[:], in0=fsel[:], scalar1=rs[:, 8:9], scalar2=None, op0=ALU.is_equal),
           reads=R_, writes=[t_rs])
        op(DVE, lambda: nc.vector.scalar_tensor_tensor(out=fm[:], in0=oh1[:], scalar=-1e30, in1=fsel[:], op0=ALU.mult, op1=ALU.add),
           reads=R_, writes=[t_rs])
        op(DVE, lambda: nc.vector.reduce_max(out=rs[:, 9:10], in_=fm[:], axis=AX.X), reads=R_, writes=[t_rs])
        op(DVE, lambda: nc.vector.tensor_scalar(out=oh2[:], in0=fm[:], scalar1=rs[:, 9:10], scalar2=None, op0=ALU.is_equal),
           reads=R_, writes=[t_rs])
        op(DVE, lambda: nc.vector.tensor_tensor(out=rs[:, 10:11], in0=rs[:, 8:9], in1=rs[:, 9:10], op=ALU.subtract), reads=R_, writes=[t_rs])
        op(ACT, lambda: nc.scalar.activation(out=rs[:, 11:12], in_=rs[:, 10:11], func=AF.Sigmoid), reads=R_, writes=[t_rs])
        op(DVE, lambda: nc.vector.tensor_tensor(out=rs[:, 12:13], in0=rs[:, 11:12], in1=rs[:, 3:4], op=ALU.mult), reads=R_, writes=[t_rs])
        op(DVE, lambda: nc.vector.tensor_tensor(out=rs[:, 13:14], in0=rs[:, 3:4], in1=rs[:, 12:13], op=ALU.subtract), reads=R_, writes=[t_rs])
        op(DVE, lambda: nc.vector.tensor_scalar(out=gw8[:], in0=oh1[:], scalar1=rs[:, 12:13], scalar2=None, op0=ALU.mult), reads=R_, writes=[t_rs])
        op(DVE, lambda: nc.vector.scalar_tensor_tensor(out=gw8[:], in0=oh2[:], scalar=rs[:, 13:14], in1=gw8[:], op0=ALU.mult, op1=ALU.add),
           reads=R_, writes=[t_rs])
        op(DVE, lambda t=t: nc.vector.tensor_tensor(out=Gd[:, t, :].rearrange("p (g e) -> p g e", g=4),
                                                    in0=ohg[:].unsqueeze(2).to_broadcast([128, 4, 8]),
                                                    in1=gw8[:].unsqueeze(1).to_broadcast([128, 4, 8]), op=ALU.mult), reads=R_, writes=[t_rt[t]])
        op(DVE, lambda: nc.vector.tensor_tensor(out=oh1[:], in0=oh1[:], in1=oh2[:], op=ALU.add), reads=R_, writes=[t_rs])
        op(DVE, lambda t=t: nc.vector.tensor_tensor(out=Md[:, t, :].rearrange("p (g e) -> p g e", g=4),
                                                    in0=ohg[:].unsqueeze(2).to_broadcast([128, 4, 8]),
                                                    in1=oh1[:].unsqueeze(1).to_broadcast([128, 4, 8]), op=ALU.mult), reads=R_, writes=[t_rt[t]])
        op(DVE, lambda t=t: nc.vector.tensor_copy(out=Mb[:, t, :], in_=Md[:, t, :]), reads=[t_rt[t]], writes=[t_rt[t]])
        op(DVE, lambda t=t: nc.vector.tensor_copy(out=Ghl[:, t, :, 0], in_=Gd[:, t, :]), reads=[t_rt[t]], writes=[t_rt[t]])
        op(DVE, lambda t=t: nc.vector.tensor_tensor(out=Gtmp[:], in0=Gd[:, t, :], in1=Ghl[:, t, :, 0], op=ALU.subtract),
           reads=[t_rt[t]], writes=[t_rs])
        op(DVE, lambda t=t: nc.vector.tensor_copy(out=Ghl[:, t, :, 1], in_=Gtmp[:]), reads=[t_rs], writes=[t_rt[t]])
        op(POOL, lambda t=t: nc.gpsimd.tensor_scalar(out=RES[:, t, :], in0=RES[:, t, :], scalar1=ALPHA, scalar2=None, op0=ALU.mult),
           reads=[t_res[t], t_Bt[t]], writes=[t_res[t]])
    for t in range(NT):
        fns = [lambda tp=tp: nc.tensor.matmul(Fp[2][:, 0:32], lhsT=CTb[:, 0:128], rhs=Mb[:, tp, :], start=(tp == 0), stop=False)
               for tp in range(t)]
        fns.append(lambda t=t: nc.tensor.matmul(Fp[2][:, 0:32], lhsT=CTb[:, 128:256], rhs=Mb[:, t, :], start=(t == 0), stop=True))
        mmgroup(PE, fns, reads=[t_rt[tp] for tp in range(t + 1)] + [t_ctb], writes=[Ft[2]])
        op(DVE, lambda t=t: nc.vector.tensor_copy(out=posd[:, t, :], in_=Fp[2][:, 0:32]), reads=[Ft[2]], writes=[t_rt[t]])

    Psel = sb(s4, "Psel", [128, NT, CAP], BF16)
    t_Psel = Tile("Psel")
    PselT = [sb(s4, f"PselT{i}", [128, NT, 128], BF16) for i in range(2)]
    t_PselT = [Tile(f"PselT{i}") for i in range(2)]
    xeT = sb(s4, "xeT", [128, KC, CAP], BF16)
    t_xeT = Tile("xeT")
    gs = sb(s4, "gs", [128, 2], F32)
    t_gs = Tile("gs")
    hg = sb(s4, "hg", [128, 512], BF16)
    t_hg = Tile("hg")
    hid = sb(s4, "hid", [128, 512], BF16)
    t_hid = Tile("hid")
    hidT = sb(s4, "hidT", [128, 4, CAP], BF16)
    t_hidT = Tile("hidT")
    yb = [sb(s4, f"yb{i}", [128, D], BF16) for i in range(2)]
    t_yb = [Tile(f"yb{i}") for i in range(2)]
    t_rt_all = t_rt

    for e in range(NEXP):
        gB, uB, dB = blk_e[e]
        pi = e % 2
        for t in range(NT):
            op(DVE, lambda t=t, e=e: nc.vector.tensor_scalar(out=Psel[:, t, :], in0=C("IOTA"), scalar1=posd[:, t, e:e + 1],
                                                             scalar2=Md[:, t, e:e + 1], op0=ALU.is_equal, op1=ALU.mult),
               reads=[t_rt[t], t_const], writes=[t_Psel])
        mmgroup(PE, [lambda t=t: nc.tensor.transpose(Tp[0][:, t * 128:(t + 1) * 128], Psel[:, t, :], ident[:]) for t in range(NT)],
                reads=[t_Psel, t_ident], writes=[Tt[0]])
        op(ACT, lambda pi=pi: nc.scalar.copy(out=PselT[pi][:], in_=Tp[0][:, 0:NT * 128].rearrange("p (a t) -> p a t", a=NT)),
           reads=[Tt[0]], writes=[t_PselT[pi]])
        mmgroup(PE, [lambda t=t, e=e: nc.tensor.matmul(Fp[5][:, 0:2], lhsT=Psel[:, t, :], rhs=Ghl[:, t, e, :], start=(t == 0), stop=(t == NT - 1))
                     for t in range(NT)], reads=[t_Psel] + t_rt_all, writes=[Ft[5]])
        op(DVE, lambda: nc.vector.reduce_sum(out=gs[:, 0:1], in_=Fp[5][:, 0:2], axis=AX.X), reads=[Ft[5]], writes=[t_gs])
        for kq in range(4):
            ai = kq % 2
            fns = []
            for kk in range(4):
                k = kq * 4 + kk
                for t in range(NT):
                    fns.append(lambda k=k, kk=kk, t=t: nc.tensor.matmul(Fp[ai][:, kk * 128:(kk + 1) * 128], lhsT=H2[:, t, k * 128:(k + 1) * 128],
                                                                        rhs=Psel[:, t, :], start=(t == 0), stop=(t == NT - 1)))
            mmgroup(PE, fns, reads=[t_Psel, t_B], writes=[Ft[ai]])
            op(ACT if kq % 2 == 0 else DVE,
               (lambda kq=kq, ai=ai: nc.scalar.copy(out=xeT[:, kq * 4:(kq + 1) * 4, :], in_=Fp[ai][:, :].rearrange("p (c s) -> p c s", c=4)))
               if kq % 2 == 0 else
               (lambda kq=kq, ai=ai: nc.vector.tensor_copy(out=xeT[:, kq * 4:(kq + 1) * 4, :], in_=Fp[ai][:, :].rearrange("p (c s) -> p c s", c=4))),
               reads=[Ft[ai]], writes=[t_xeT])
        Wg, Wgt = wget(gB)
        mmgroup(PE, [lambda k=k: nc.tensor.matmul(Fp[2][:, :], lhsT=xeT[:, k, :], rhs=Wg[:, k, :], start=(k == 0), stop=(k == KC - 1))
                     for k in range(KC)], reads=[t_xeT, Wgt], writes=[Ft[2]])
        Wu, Wut = wget(uB)
        mmgroup(PE, [lambda k=k: nc.tensor.matmul(Fp[3][:, :], lhsT=xeT[:, k, :], rhs=Wu[:, k, :], start=(k == 0), stop=(k == KC - 1))
                     for k in range(KC)], reads=[t_xeT, Wut], writes=[Ft[3]])
        op(ACT, lambda: nc.scalar.activation(out=hg[:], in_=Fp[2][:, :], func=AF.Silu), reads=[Ft[2]], writes=[t_hg])
        op(DVE, lambda: nc.vector.tensor_tensor(out=hid[:], in0=hg[:], in1=Fp[3][:, :], op=ALU.mult), reads=[t_hg, Ft[3]], writes=[t_hid])
        mmgroup(PE, [lambda c=c: nc.tensor.transpose(Tp[1][:, c * 128:(c + 1) * 128], hid[:, c * 128:(c + 1) * 128], ident[:])
                     for c in range(4)], reads=[t_hid, t_ident], writes=[Tt[1]])
        op(ACT, lambda: nc.scalar.copy(out=hidT[:], in_=Tp[1][:, 0:512].rearrange("p (c s) -> p c s", c=4)), reads=[Tt[1]], writes=[t_hidT])
        Wd, Wdt = wget(dB)
        for cb in range(4):
            ai = cb % 2
            mmgroup(PE, [lambda c=c, cb=cb: nc.tensor.matmul(Fp[ai][:, :], lhsT=hidT[:, c, :], rhs=Wd[:, c, cb * 512:(cb + 1) * 512],
                                                             start=(c == 0), stop=(c == 3)) for c in range(4)],
                    reads=[t_hidT, Wdt], writes=[Ft[ai]])
            op(DVE, lambda cb=cb, ai=ai, pi=pi: nc.vector.tensor_scalar(out=yb[pi][:, cb * 512:(cb + 1) * 512], in0=Fp[ai][:, :],
                                                                        scalar1=gs[:, 0:1], scalar2=None, op0=ALU.mult),
               reads=[Ft[ai], t_gs], writes=[t_yb[pi]])
        if e % 2 == 1:
            for t in range(NT):
                for cb in range(4):
                    ai = 4 + (t * 4 + cb) % 2
                    mmgroup(PE, [lambda q=q, t=t, cb=cb: nc.tensor.matmul(Fp[ai][:, :], lhsT=PselT[q][:, t, :], rhs=yb[q][:, cb * 512:(cb + 1) * 512],
                                                                          start=(q == 0), stop=(q == 1)) for q in range(2)],
                            reads=t_PselT + t_yb, writes=[Ft[ai]])
                    op(DVE, (lambda t=t, cb=cb, ai=ai: nc.vector.tensor_tensor(out=RES[:, t, cb * 512:(cb + 1) * 512],
                                                                               in0=RES[:, t, cb * 512:(cb + 1) * 512],
                                                                               in1=Fp[ai][:, :], op=ALU.add)),
                       reads=[Ft[ai], t_res[t]], writes=[t_res[t]])

    barrier()
    s4.close()
    lnS = Scope(hi)
    lnt = sb(lnS, "lnt3", [128, 2, D], F32)
    lnst = sb(lnS, "lnst3", [128, 4, 6], F32)
    lnmv = sb(lnS, "lnmv3", [128, 8], F32)
    hb = sb(lnS, "hb3", [128, D], BF16)
    load_ln(2)
    d_out = dsem("d_out")
    for t in range(NT):
        layernorm(t, None, None)
        dma(SP, d_out, out_d[t * 128:(t + 1) * 128, :], RES[:, t, :], src=t_res[t])
    SP.wait([(d_out.sem, d_out.count)])
    if DEBUG:
        SP.wait([(d_dbg.sem, d_dbg.count)])
    barrier()
    lnS.close()
    s2.close()
    es.close()
    return nc


def kernel(x, mem, positions, w_in, w_gla_a2, b_gla_a, g_gla_norm, w_mix_out, ln1_g, ln1_b,
           w_mq, w_mk, w_mv, w_mo, ln2_g, ln2_b, w_route_group, b_route_group,
           w_route_expert, b_route_expert, w_exp_gate, w_exp_up, w_exp_down, ln3_g, ln3_b):
    f = lambda a: np.ascontiguousarray(np.asarray(a))
    x = f(x)[0][:SEQ]
    pos = f(positions)[0].astype(np.int32)[:SEQ]
    shared = {
        "consts": CONST_ARR, "consts1": CONST1_ARR,
        "w_in": _perm_w_in(f(w_in)[0]),
        "wa2b": np.ascontiguousarray(np.concatenate([f(w_gla_a2)[0], f(b_gla_a)[0][None, :]], axis=0)),
        "gnorm": f(g_gla_norm)[0][None, :].copy(),
        "w_mix": f(w_mix_out)[0], "w_mq": f(w_mq)[0], "w_mk": f(w_mk)[0], "w_mv": f(w_mv)[0], "w_mo": f(w_mo)[0],
        "memT": np.ascontiguousarray(f(mem)[0].reshape(256, KC, 128).transpose(2, 1, 0).reshape(128, KC * 256)),
        "ln": np.ascontiguousarray(np.stack([f(ln1_g)[0], f(ln1_b)[0], f(ln2_g)[0], f(ln2_b)[0], f(ln3_g)[0], f(ln3_b)[0]])),
        "wroute": np.ascontiguousarray(np.concatenate([f(w_route_group)[0], f(w_route_expert)[0]], axis=1)),
        "broute": np.ascontiguousarray(np.concatenate([f(b_route_group)[0], f(b_route_expert)[0].reshape(-1)])[None, :]),
        "w_eg": f(w_exp_gate)[0], "w_eu": f(w_exp_up)[0], "w_ed": f(w_exp_down)[0],
    }
    in_maps = []
    for c in range(NCORE):
        npad = (NCORE - 1 - c) * TOK
        xs = np.concatenate([np.zeros((npad, D), np.float32), x[0:(c + 1) * TOK]], axis=0)
        ps = np.concatenate([np.zeros((npad,), np.int32), pos[0:(c + 1) * TOK]], axis=0)
        xTp = np.ascontiguousarray(xs.reshape(NPRE + NT, 128, KC, 128).transpose(0, 3, 2, 1)).reshape(NPRE + NT, 128, D)
        m = dict(shared)
        m["xTp"] = xTp
        m["xown"] = np.ascontiguousarray(x[c * TOK:(c + 1) * TOK])
        m["posT"] = np.ascontiguousarray(ps.reshape(NPRE + NT, 128).T)
        in_maps.append(m)
    if "nc" not in _CACHE:
        _CACHE["nc"] = build_program()
    in_maps = [{k: v for k, v in m.items() if k in DECLARED} for m in in_maps]
    res = run_bass_kernel_spmd(_CACHE["nc"], in_maps, core_ids=list(range(NCORE)))
    if DEBUG:
        _CACHE["dbg"] = res.results
    out = np.concatenate([np.asarray(r["out"]) for r in res.results], axis=0).astype(np.float32)
    return out.reshape(1, SEQ, D)
```

```python
import math
from contextlib import ExitStack
import numpy as np
import concourse.bass as bass
import concourse.mybir as mybir
from concourse.bass_utils import run_bass_kernel_spmd

F32 = mybir.dt.float32
BF16 = mybir.dt.bfloat16
I32 = mybir.dt.int32
AF = mybir.ActivationFunctionType
ALU = mybir.AluOpType
AX = mybir.AxisListType

NCORE = 8
D = 2048
SEQ = 8192
TOK = SEQ // NCORE
NT = TOK // 128
NPRE = (NCORE - 1) * NT
KC = D // 128
EPS = 1e-5
ALPHA = 2.0 ** 0.25
NEXP = 32
CAP = 128
TWO_PI = 2.0 * math.pi
GAMMAS = [1.0 - 2.0 ** (-5.0 - h) for h in range(4)]

DEBUG = False
STOP = None


def configure(seq=8192, stop=None, debug=False):
    global SEQ, TOK, NT, NPRE, STOP, DEBUG
    SEQ = seq
    TOK = SEQ // NCORE
    NT = TOK // 128
    NPRE = (NCORE - 1) * NT
    STOP = stop
    DEBUG = debug
    _CACHE.clear()


_CACHE = {}
DECLARED = []


class Tile:
    def __init__(self, name="", excl=False):
        self.name = name
        self.w = []
        self.r = []
        self.excl = excl


class DSem:
    def __init__(self, nc, es, name):
        self.sem = es.enter_context(nc.semaphore(name))
        self.count = 0


class Eng:
    def __init__(self, nc, es, e, name):
        self.e = e
        self.sem = es.enter_context(nc.semaphore(name))
        self.n = 0
        self.seen = {}

    def wait(self, tks):
        best = {}
        for sem, val in tks:
            if val > best.get(sem, 0):
                best[sem] = val
        for sem, val in best.items():
            if self.seen.get(sem, 0) < val:
                self.e.wait_ge(sem, val)
                self.seen[sem] = val

    def tick(self, ins):
        self.n += 1
        ins.then_inc(self.sem, 1)
        return (self.sem, self.n)


def op(E, fn, reads=(), writes=()):
    tks = []
    for t in reads:
        tks += t.w
        if t.excl:
            tks += [k for k in t.r if k[0] is not E.sem]
    for t in writes:
        tks += t.w
        tks += t.r
    E.wait(tks)
    ins = fn()
    tk = E.tick(ins)
    for t in reads:
        t.r = [k for k in t.r if k[0] is not E.sem] + [tk]
    for t in writes:
        t.w = [tk]
        t.r = []
    return tk


def mmgroup(E, fns, reads=(), writes=()):
    tks = []
    for t in reads:
        tks += t.w
        if t.excl:
            tks += [k for k in t.r if k[0] is not E.sem]
    for t in writes:
        tks += t.w
        tks += t.r
    E.wait(tks)
    ins = None
    for f in fns:
        ins = f()
    tk = E.tick(ins)
    for t in reads:
        t.r = [k for k in t.r if k[0] is not E.sem] + [tk]
    for t in writes:
        t.w = [tk]
        t.r = []
    return tk


def dma(Q, ds, out, in_, dst=None, src=None):
    tks = []
    dsts = [] if dst is None else (list(dst) if isinstance(dst, (list, tuple)) else [dst])
    for d in dsts:
        tks += d.w + d.r
    if src is not None:
        tks += src.w
    Q.wait(tks)
    ds.count += 16
    Q.e.dma_start(out=out, in_=in_).then_inc(ds.sem, 16)
    tk = (ds.sem, ds.count)
    for d in dsts:
        d.w = [tk]
        d.r = []
    if src is not None:
        src.r = [k for k in src.r if k[0] is not ds.sem] + [tk]
    return tk


def _consts():
    c = {}
    i = np.arange(128)
    DT = np.zeros((4, 128, 128), np.float64)
    for h, g in enumerate(GAMMAS):
        rel = i[None, :] - i[:, None]
        DT[h] = np.where(rel >= 0, np.exp(np.log(g) * np.maximum(rel, 0)), 0.0) / 16.0
    c["DT"] = DT.transpose(1, 0, 2).reshape(128, 512)
    c["CAUS"] = (i[None, :] >= i[:, None]).astype(np.float64)
    c["DQ"] = np.tile(np.stack([np.exp(np.log(g) * (i + 1.0)) for g in GAMMAS]).reshape(1, 512), (128, 1))
    dk = np.zeros((128, 8))
    for h, g in enumerate(GAMMAS):
        dk[:, 2 * h] = np.exp(np.log(g) * (127.0 - i)) / 16.0
    c["DK"] = dk
    half = np.arange(128, dtype=np.float32)
    invf = (np.float32(10000.0) ** (-half / np.float32(128.0))).astype(np.float32)
    c["INVF"] = np.tile(invf.reshape(1, 128), (128, 1))
    c["UT"] = -(i[:, None] <= i[None, :]).astype(np.float64) / 16.0
    c["UT2"] = -(i[:, None] > i[None, :]).astype(np.float64) / 16.0
    c["NCOL"] = np.full((128, 1), -1.0 / 16.0)
    c["IOTA"] = np.tile(i.reshape(1, 128).astype(np.float64), (128, 1))
    c["TRIS"] = (i[:, None] < i[None, :]).astype(np.float64)
    c["ONES"] = np.ones((128, 128))
    pers = ["IOTA", "TRIS", "ONES"]
    offs = {}
    cols1, cols2 = [], []
    o = 0
    for k, v in c.items():
        if k in pers:
            continue
        offs[k] = (1, o, v.shape[1])
        o += v.shape[1]
        cols1.append(v.astype(np.float32))
    o = 0
    for k in pers:
        v = c[k]
        offs[k] = (0, o, v.shape[1])
        o += v.shape[1]
        cols2.append(v.astype(np.float32))
    return (np.ascontiguousarray(np.concatenate(cols2, axis=1)), np.ascontiguousarray(np.concatenate(cols1, axis=1)), offs)


CONST_ARR, CONST1_ARR, COFF = _consts()

A_RET = [(h * 512, 512) for h in range(4)]
A_GLA = [(2048 + h * 384, 384) for h in range(4)]
A_LR = (3584, 16)
A_W = 3600
B_RET = [(3600 + h * 512, 512) for h in range(4)]
B_GLA = [(5648 + h * 384, 384) for h in range(4)]


def _perm_w_in(w_in):
    rq, rk, rv, rg, gq, gk, gv, gg, glr = np.split(w_in, np.cumsum([1024, 1024, 1024, 1024, 512, 512, 1024, 1024])[:8], axis=1)
    cols = []
    for h in range(4):
        cols += [rk[:, h * 256:(h + 1) * 256], rv[:, h * 256:(h + 1) * 256]]
    for h in range(4):
        cols += [gk[:, h * 128:(h + 1) * 128], gv[:, h * 256:(h + 1) * 256]]
    cols += [glr]
    for h in range(4):
        cols += [rq[:, h * 256:(h + 1) * 256], rg[:, h * 256:(h + 1) * 256]]
    for h in range(4):
        cols += [gq[:, h * 128:(h + 1) * 128], gg[:, h * 256:(h + 1) * 256]]
    return np.ascontiguousarray(np.concatenate(cols, axis=1))


def build_program():
    nc = bass.Bass("TRN2", target_bir_lowering=False)
    es = ExitStack()

    del DECLARED[:]
    need = {"1b": 0, "2": 1, "3": 2, None: 3}.get(STOP, 0)
    lvl = {"w_mix": 1, "ln": 1, "xown": 1, "w_mq": 2, "w_mk": 2, "w_mv": 2, "w_mo": 2, "memT": 2,
           "wroute": 3, "broute": 3, "w_eg": 3, "w_eu": 3, "w_ed": 3}

    def din(name, shape, dt=F32):
        if lvl.get(name, 0) > need:
            return None
        DECLARED.append(name)
        return nc.dram_tensor(name, list(shape), dt, kind="ExternalInput").ap()

    xTp = din("xTp", [NPRE + NT, 128, D])
    xown = din("xown", [TOK, D])
    posT = din("posT", [128, NPRE + NT], I32)
    consts_d = din("consts", list(CONST_ARR.shape))
    consts1_d = din("consts1", list(CONST1_ARR.shape))
    w_in = din("w_in", [D, 7184])
    wa2b_d = din("wa2b", [17, 512])
    gnorm_d = din("gnorm", [1, 256])
    w_mix = din("w_mix", [D, D])
    w_mq = din("w_mq", [D, D])
    w_mk = din("w_mk", [D, D])
    w_mv = din("w_mv", [D, D])
    w_mo = din("w_mo", [D, D])
    memT_d = din("memT", [128, KC * 256])
    ln_d = din("ln", [6, D])
    wroute_d = din("wroute", [D, 36])
    broute_d = din("broute", [1, 36])
    w_eg = din("w_eg", [NEXP, D, 512])
    w_eu = din("w_eu", [NEXP, D, 512])
    w_ed = din("w_ed", [NEXP, 512, D])
    out_d = nc.dram_tensor("out", [TOK, D], F32, kind="ExternalOutput").ap()
    if DEBUG:
        dbg_mixT = nc.dram_tensor("dbg_mixT", [128, KC * TOK], BF16, kind="ExternalOutput").ap()
        dbg_h1 = nc.dram_tensor("dbg_h1", [TOK, D], F32, kind="ExternalOutput").ap()
        dbg_h2 = nc.dram_tensor("dbg_h2", [TOK, D], F32, kind="ExternalOutput").ap()

    PE = Eng(nc, es, nc.tensor, "s_pe")
    DVE = Eng(nc, es, nc.vector, "s_dve")
    ACT = Eng(nc, es, nc.scalar, "s_act")
    POOL = Eng(nc, es, nc.gpsimd, "s_pool")
    SP = Eng(nc, es, nc.sync, "s_sp")
    ENGS = [PE, DVE, ACT, POOL, SP]
    all_dsems = []

    def dsem(name):
        d = DSem(nc, es, name)
        all_dsems.append(d)
        return d

    def barrier():
        tks = [(E.sem, E.n) for E in ENGS if E.n > 0] + [(d.sem, d.count) for d in all_dsems if d.count > 0]
        for E in ENGS:
            E.wait(tks)

    class Arena:
        def __init__(self, base, limit):
            self.base, self.top, self.limit = base, base, limit

    class Scope:
        def __init__(self, arena):
            self.arena = arena
            self.mark = arena.top

        def close(self):
            self.arena.top = self.mark

    big_holder = []

    d_dbg = dsem("d_dbg") if DEBUG else None

    def finish():
        if DEBUG:
            SP.wait([(d_dbg.sem, d_dbg.count)])
        barrier()
        es.close()
        return nc

    def sb(stack, name, shape, dt):
        if not isinstance(stack, Scope):
            return stack.enter_context(nc.sbuf_tensor(name, list(shape), dt))
        ar = stack.arena
        esz = 2 if dt == BF16 else 4
        n = 1
        for d_ in shape[1:]:
            n *= d_
        nbytes = (n * esz + 63) // 64 * 64
        off = ar.top
        assert off + nbytes <= ar.limit, (name, off, nbytes, ar.limit)
        ar.top = off + nbytes
        v = big_holder[0][0:shape[0], off // 4:(off + n * esz + 3) // 4]
        if dt != F32:
            v = v.bitcast(dt)
            v = v[:, 0:n]
        if len(shape) == 3:
            v = v.rearrange("p (a b) -> p a b", a=shape[1])
        elif len(shape) == 4:
            v = v.rearrange("p (a b c) -> p a b c", a=shape[1], b=shape[2])
        return v

    CT = sb(es, "CT", CONST_ARR.shape, F32)
    CTb = sb(es, "CTb", [128, 128 * 3], BF16)
    ident = sb(es, "ident", [128, 128], BF16)
    posf = sb(es, "posf", [128, NPRE + NT], F32)
    posi = sb(es, "posi", [128, NPRE + NT], I32)
    LO_1A = 115200
    LO_AFTER = 114688
    bigw = (nc.sbuf_bytes_remaining - 256) // 64 * 16
    big_holder.append(es.enter_context(nc.sbuf_tensor("big", [128, bigw], F32)))
    lo = Arena(0, LO_1A)
    hi = Arena(LO_1A, bigw * 4)
    s1 = Scope(hi)
    CT1 = sb(s1, "CT1", CONST1_ARR.shape, F32)
    wa2b = sb(s1, "wa2bs", [17, 512], BF16)
    gnb = sb(s1, "gnb", [128, 256], F32)
    Rst = sb(s1, "Rst", [128, 4, 512], F32)
    Rb = sb(s1, "Rb", [128, 4, 512], BF16)
    Sst = sb(s1, "Sst", [128, 4, 256], F32)
    Sb = sb(s1, "Sb", [128, 4, 256], BF16)
    NSLOT = 3
    slot_t = [Tile(f"slot{i}") for i in range(NSLOT)]
    slot_ds = [dsem(f"d_slot{i}") for i in range(NSLOT)]

    Fp = [es.enter_context(nc.psum_tensor(f"F{i}", [128, 512], F32)) for i in range(6)]
    Ft = [Tile(f"F{i}", excl=True) for i in range(6)]
    Tp = [es.enter_context(nc.psum_tensor(f"T{i}", [128, 1024], BF16)) for i in range(2)]
    Tt = [Tile(f"T{i}", excl=True) for i in range(2)]

    t_const = Tile("const")
    t_ident = Tile("ident")
    t_pos = Tile("pos")
    t_R = [Tile(f"R{h}") for h in range(4)]
    t_Rb = [Tile(f"Rb{h}") for h in range(4)]
    t_S = [Tile(f"S{h}") for h in range(4)]
    t_Sb = [Tile(f"Sb{h}") for h in range(4)]
    t_A = Tile("ACT_A")
    t_B = Tile("ACT_B")
    t_Bt = [Tile(f"ACT_B{t}") for t in range(NT)]
    t_At = [Tile(f"ACT_A{t}") for t in range(NT)]

    def C(name, lo=0, hi=None):
        which, o, w = COFF[name]
        hi = w if hi is None else hi
        return (CT1 if which else CT)[:, o + lo:o + hi]

    d_c = dsem("d_const")
    dma(SP, d_c, CT[:], consts_d)
    dma(SP, d_c, CT1[:], consts1_d)
    dma(SP, d_c, posi[:], posT)
    dma(SP, d_c, gnb[:], gnorm_d.partition_broadcast(128))
    d_c2 = dsem("d_const2")
    dma(POOL, d_c2, wa2b[:], wa2b_d)
    t_const.w = [(d_c.sem, d_c.count), (d_c2.sem, d_c2.count)]
    op(POOL, lambda: nc.gpsimd.memset(ident[:], 0.0), writes=[t_ident])
    op(POOL, lambda: nc.gpsimd.affine_select(out=ident[:], in_=ident[:], pattern=[[-1, 128]], compare_op=ALU.not_equal,
                                              fill=1.0, base=0, channel_multiplier=1), reads=[t_ident], writes=[t_ident])
    op(DVE, lambda: nc.vector.tensor_copy(out=posf[:], in_=posi[:]), reads=[t_const], writes=[t_pos])
    t_ctb = Tile("ctb")
    op(DVE, lambda: nc.vector.tensor_copy(out=CTb[:, 0:128], in_=C("ONES")), reads=[t_const], writes=[t_ctb])
    op(DVE, lambda: nc.vector.tensor_copy(out=CTb[:, 128:256], in_=C("TRIS")), reads=[t_const], writes=[t_ctb])
    for h in range(4):
        op(DVE, lambda h=h: nc.vector.memset(Rst[:, h, :], 0.0), writes=[t_R[h]])
        op(DVE, lambda h=h: nc.vector.memset(Sst[:, h, :], 0.0), writes=[t_S[h]])
        op(POOL, lambda h=h: nc.gpsimd.memset(Rb[:, h, :], 0.0), writes=[t_Rb[h]])
        op(POOL, lambda h=h: nc.gpsimd.memset(Sb[:, h, :], 0.0), writes=[t_Sb[h]])

    csb = [sb(s1, f"cs{i}", [128, 256], F32) for i in range(2)]
    t_csb = [Tile(f"cs{i}") for i in range(2)]
    tr_a = sb(s1, "tr_a", [128, 256], F32)
    tr_b = sb(s1, "tr_b", [128, 256], F32)
    tr_i = sb(s1, "tr_i", [128, 256], I32)
    t_tr = Tile("tr")
    rotA = sb(s1, "rotA", [128, 256], F32)
    rotB = sb(s1, "rotB", [128, 256], F32)
    t_rot = Tile("rot")
    kbuf = [sb(s1, f"kbuf{i}", [128, 256], BF16) for i in range(2)]
    t_k = [Tile(f"k{i}") for i in range(2)]
    gkb = [sb(s1, f"gkb{i}", [128, 128], BF16) for i in range(2)]
    t_gk = [Tile(f"gk{i}") for i in range(2)]
    vbuf = [sb(s1, f"vbuf{i}", [128, 256], BF16) for i in range(2)]
    t_v = [Tile(f"v{i}") for i in range(2)]
    vhat = [sb(s1, f"vhat{i}", [128, 256], BF16) for i in range(2)]
    t_vh = [Tile(f"vh{i}") for i in range(2)]
    glr_b = sb(s1, "glr_b", [128, 16], BF16)
    t_glr = Tile("glr")
    glrT = sb(s1, "glrT", [17, 128], BF16)
    t_glrT = Tile("glrT")
    Lg = sb(s1, "Lg", [128, 512], F32)
    t_L = Tile("L")
    etmp = sb(s1, "etmp", [128, 512], F32)
    t_et = Tile("etmp")
    gt_ekb = sb(s1, "gt_ekb", [128, 512], F32)
    gt_eb = sb(s1, "gt_eb", [128, 512], F32)
    gt_enb = sb(s1, "gt_enb", [128, 512], F32)
    gt_edec = sb(s1, "gt_edec", [128, 4], F32)
    t_gt = Tile("gt")
    op(POOL, lambda: nc.gpsimd.memset(glrT[:], 1.0), writes=[t_glrT])

    def gen_cossin(T, dst2d, dst_tile):
        G = nc.vector
        op(DVE, lambda: G.tensor_scalar(out=tr_a[:, 128:256], in0=C("INVF"), scalar1=posf[:, T:T + 1], scalar2=None,
                                         op0=ALU.mult), reads=[t_const, t_pos], writes=[t_tr])
        op(DVE, lambda: G.tensor_scalar(out=tr_a[:, 0:128], in0=tr_a[:, 128:256], scalar1=math.pi / 2, scalar2=None,
                                         op0=ALU.add), reads=[t_tr], writes=[t_tr])
        op(DVE, lambda: G.tensor_scalar(out=tr_i[:], in0=tr_a[:], scalar1=1.0 / TWO_PI, scalar2=None, op0=ALU.mult),
           reads=[t_tr], writes=[t_tr])
        op(DVE, lambda: G.tensor_copy(out=tr_b[:], in_=tr_i[:]), reads=[t_tr], writes=[t_tr])
        op(DVE, lambda: G.tensor_scalar(out=tr_b[:], in0=tr_b[:], scalar1=-TWO_PI, scalar2=None, op0=ALU.mult),
           reads=[t_tr], writes=[t_tr])
        op(DVE, lambda: G.tensor_tensor(out=tr_a[:], in0=tr_a[:], in1=tr_b[:], op=ALU.add), reads=[t_tr], writes=[t_tr])
        op(DVE, lambda: G.tensor_scalar(out=tr_b[:], in0=tr_a[:], scalar1=math.pi, scalar2=TWO_PI, op0=ALU.is_gt, op1=ALU.mult),
           reads=[t_tr], writes=[t_tr])
        op(DVE, lambda: G.tensor_tensor(out=tr_a[:], in0=tr_a[:], in1=tr_b[:], op=ALU.subtract), reads=[t_tr], writes=[t_tr])
        op(DVE, lambda: G.tensor_scalar(out=tr_b[:], in0=tr_a[:], scalar1=-math.pi, scalar2=TWO_PI, op0=ALU.is_lt, op1=ALU.mult),
           reads=[t_tr], writes=[t_tr])
        op(DVE, lambda: G.tensor_tensor(out=tr_a[:], in0=tr_a[:], in1=tr_b[:], op=ALU.add), reads=[t_tr], writes=[t_tr])
        op(ACT, lambda: nc.scalar.activation(out=dst2d, in_=tr_a[:], func=AF.Sin), reads=[t_tr], writes=[dst_tile])

    def rotary(ps_ap, out_ap, extra_reads, out_tile, cs2d, t_cs):
        p3 = ps_ap.rearrange("p (a b) -> p a b", a=2)
        cosb = cs2d[:, 0:128].unsqueeze(1).to_broadcast([128, 2, 128])
        sinb = cs2d[:, 128:256].unsqueeze(1).to_broadcast([128, 2, 128])
        op(DVE, lambda: nc.vector.tensor_tensor(out=rotA[:].rearrange("p (a b) -> p a b", a=2), in0=p3, in1=cosb, op=ALU.mult),
           reads=[t_cs] + extra_reads, writes=[t_rot])
        op(DVE, lambda: nc.vector.tensor_tensor(out=rotB[:].rearrange("p (a b) -> p a b", a=2), in0=p3, in1=sinb, op=ALU.mult),
           reads=[t_cs] + extra_reads, writes=[t_rot])
        op(DVE, lambda: nc.vector.tensor_tensor(out=out_ap[:, 0:128], in0=rotA[:, 0:128], in1=rotB[:, 128:256], op=ALU.subtract),
           reads=[t_rot], writes=[out_tile])
        op(DVE, lambda: nc.vector.tensor_tensor(out=out_ap[:, 128:256], in0=rotB[:, 0:128], in1=rotA[:, 128:256], op=ALU.add),
           reads=[t_rot], writes=[out_tile])

    def proj(xT_fn, x_tiles, w_ap_fn, w_tiles, ncols, acc_i):
        mmgroup(PE, [lambda k=k: nc.tensor.matmul(Fp[acc_i][:, 0:ncols], lhsT=xT_fn(k), rhs=w_ap_fn(k),
                                                  start=(k == 0), stop=(k == KC - 1)) for k in range(KC)],
                reads=list(x_tiles) + list(w_tiles), writes=[Ft[acc_i]])

    def gates_glr(xT_fn, x_tiles, wlr_fn, w_tiles):
        mmgroup(PE, [lambda k=k: nc.tensor.matmul(Fp[5][:, 0:16], lhsT=xT_fn(k), rhs=wlr_fn(k), start=(k == 0), stop=(k == KC - 1))
                     for k in range(KC)], reads=list(x_tiles) + list(w_tiles), writes=[Ft[5]])
        op(ACT, lambda: nc.scalar.copy(out=glr_b[:], in_=Fp[5][:, 0:16]), reads=[Ft[5]], writes=[t_glr])

    def gates_z():
        op(PE, lambda: nc.tensor.transpose(Tp[1][0:16, 0:128], glr_b[:], ident[:]), reads=[t_glr, t_ident], writes=[Tt[1]])
        op(DVE, lambda: nc.vector.tensor_copy(out=glrT[0:16, :], in_=Tp[1][0:16, 0:128]), reads=[Tt[1]], writes=[t_glrT])
        op(PE, lambda: nc.tensor.matmul(Fp[5][:, :], lhsT=glrT[:, :], rhs=wa2b[:, :], start=True, stop=True),
           reads=[t_glrT, t_const], writes=[Ft[5]])
        op(ACT, lambda: nc.scalar.activation(out=etmp[:], in_=Fp[5][:, :], func=AF.Exp, scale=-1.0), reads=[Ft[5]], writes=[t_et])
        op(ACT, lambda: nc.scalar.activation(out=Lg[:], in_=etmp[:], func=AF.Ln, bias=1.0), reads=[t_et], writes=[t_L])

    def gates_L():
        op(PE, lambda: nc.tensor.matmul(Fp[5][:, :], lhsT=C("UT2"), rhs=Lg[:], start=True, stop=True),
           reads=[t_L, t_const], writes=[Ft[5]])
        op(ACT, lambda: nc.scalar.activation(out=gt_ekb[:], in_=Fp[5][:, :], func=AF.Exp), reads=[Ft[5]], writes=[t_gt])

    def gates_dec(own):
        mmgroup(PE, [lambda h=h: nc.tensor.matmul(Fp[5][:, h:h + 1], lhsT=Lg[:, h * 128:(h + 1) * 128], rhs=C("NCOL"),
                                                  start=True, stop=True) for h in range(4)],
                reads=[t_L, t_const], writes=[Ft[5]])
        op(ACT, lambda: nc.scalar.activation(out=gt_edec[:], in_=Fp[5][:, 0:4], func=AF.Exp), reads=[Ft[5]], writes=[t_gt])
        if own:
            op(PE, lambda: nc.tensor.matmul(Fp[5][:, :], lhsT=C("UT"), rhs=Lg[:], start=True, stop=True),
               reads=[t_L, t_const], writes=[Ft[5]])
            op(ACT, lambda: nc.scalar.activation(out=gt_eb[:], in_=Fp[5][:, :], func=AF.Exp), reads=[Ft[5]], writes=[t_gt])
            op(ACT, lambda: nc.scalar.activation(out=gt_enb[:], in_=Fp[5][:, :], func=AF.Exp, scale=-1.0), reads=[Ft[5]], writes=[t_gt])

    def gla_gates(T, xT_fn, x_tiles, wlr_fn, w_tiles, own):
        gates_glr(xT_fn, x_tiles, wlr_fn, w_tiles)
        gates_z()
        gates_L()
        gates_dec(own)

    def ret_state_update(h, k_ap, kt, vh_ap, vht):
        mmgroup(PE, [lambda c=c: nc.tensor.matmul(Fp[4][:, c * 256:(c + 1) * 256], lhsT=k_ap[:, c * 128:(c + 1) * 128], rhs=vh_ap,
                                                  start=True, stop=True) for c in range(2)],
                reads=[kt, vht], writes=[Ft[4]])
        op(DVE, lambda: nc.vector.scalar_tensor_tensor(out=Rst[:, h, :], in0=Rst[:, h, :], scalar=float(GAMMAS[h] ** 128),
                                                       in1=Fp[4][:, :], op0=ALU.mult, op1=ALU.add),
           reads=[Ft[4], t_R[h]], writes=[t_R[h]])

    def gla_state_update(h, khat_ap, kt, v_ap, vt):
        op(PE, lambda: nc.tensor.matmul(Fp[4][:, 0:256], lhsT=khat_ap, rhs=v_ap, start=True, stop=True),
           reads=[kt, vt], writes=[Ft[4]])
        op(DVE, lambda: nc.vector.scalar_tensor_tensor(out=Sst[:, h, :], in0=Sst[:, h, :], scalar=gt_edec[:, h:h + 1],
                                                       in1=Fp[4][:, 0:256], op0=ALU.mult, op1=ALU.add),
           reads=[Ft[4], t_S[h], t_gt], writes=[t_S[h]])

    if STOP == "s":
        return finish()
    s1a = Scope(hi)
    s1lo = Scope(lo)
    WA = sb(s1lo, "WA", [128, KC, A_W], BF16)
    t_WA = Tile("WA")
    d_wa = dsem("d_wa")
    for k0 in range(0, KC, 4):
        dma(POOL, d_wa, WA[:, k0:k0 + 4, :], w_in[k0 * 128:(k0 + 4) * 128, 0:A_W].rearrange("(k p) n -> p k n", p=128))
    t_WA.w = [(d_wa.sem, d_wa.count)]
    xt_buf = [sb(s1a, f"xt{i}", [128, KC, 128], BF16) for i in range(2)]
    t_xt = [Tile(f"xt{i}") for i in range(2)]
    d_xt = [dsem(f"d_xt{i}") for i in range(2)]

    def load_xt(T):
        i = T % 2
        dma(POOL, d_xt[i], xt_buf[i][:], xTp[T].rearrange("p (k t) -> p k t", k=KC), dst=t_xt[i])

    load_xt(0)
    gen_cossin(0, csb[0][:], t_csb[0])
    for T in range(NPRE):
        if T + 1 < NPRE:
            load_xt(T + 1)
            gen_cossin(T + 1, csb[(T + 1) % 2][:], t_csb[(T + 1) % 2])
        xb = xt_buf[T % 2]
        xt_ = t_xt[T % 2]
        xT_fn = lambda k, xb=xb: xb[:, k, :]
        cs2d, t_cs = csb[T % 2], t_csb[T % 2]

        def ret_proj(h):
            c0, n = A_RET[h]
            ai = h % 2
            proj(xT_fn, [xt_], lambda k, c0=c0, n=n: WA[:, k, c0:c0 + n], [t_WA], n, ai)
            rotary(Fp[ai][:, 0:256], kbuf[ai], [Ft[ai]], t_k[ai], cs2d, t_cs)
            op(DVE, lambda ai=ai, h=h: nc.vector.tensor_scalar(out=vhat[ai][:], in0=Fp[ai][:, 256:512], scalar1=C("DK", 2 * h, 2 * h + 1),
                                                               scalar2=None, op0=ALU.mult), reads=[Ft[ai], t_const], writes=[t_vh[ai]])

        def ret_state(h):
            ai = h % 2
            ret_state_update(h, kbuf[ai], t_k[ai], vhat[ai][:], t_vh[ai])

        def gla_proj(h):
            c0, n = A_GLA[h]
            ai = 2 + h % 2
            bi = h % 2
            proj(xT_fn, [xt_], lambda k, c0=c0, n=n: WA[:, k, c0:c0 + n], [t_WA], n, ai)
            op(DVE, lambda ai=ai, bi=bi, h=h: nc.vector.tensor_tensor(out=gkb[bi][:], in0=Fp[ai][:, 0:128],
                                                                      in1=gt_ekb[:, h * 128:(h + 1) * 128], op=ALU.mult),
               reads=[Ft[ai], t_gt], writes=[t_gk[bi]])
            op(ACT, lambda ai=ai, bi=bi: nc.scalar.copy(out=vbuf[bi][:], in_=Fp[ai][:, 128:384]), reads=[Ft[ai]], writes=[t_v[bi]])

        def gla_state(h):
            bi = h % 2
            gla_state_update(h, gkb[bi][:], t_gk[bi], vbuf[bi][:], t_v[bi])

        wlr_fn = lambda k: WA[:, k, A_LR[0]:A_LR[0] + 16]
        gates_glr(xT_fn, [xt_], wlr_fn, [t_WA])
        ret_proj(0)
        gates_z()
        ret_proj(1)
        ret_state(0)
        gates_L()
        ret_proj(2)
        ret_state(1)
        gates_dec(False)
        ret_proj(3)
        ret_state(2)
        gla_proj(0)
        ret_state(3)
        gla_proj(1)
        gla_state(0)
        gla_proj(2)
        gla_state(1)
        gla_proj(3)
        gla_state(2)
        gla_state(3)
    for h in range(4):
        op(ACT, lambda h=h: nc.scalar.copy(out=Rb[:, h, :], in_=Rst[:, h, :]), reads=[t_R[h]], writes=[t_Rb[h]])
        op(ACT, lambda h=h: nc.scalar.copy(out=Sb[:, h, :], in_=Sst[:, h, :]), reads=[t_S[h]], writes=[t_Sb[h]])
    barrier()
    if STOP == "1a":
        return finish()
    s1a.close()
    s1lo.close()
    plo = Scope(lo)
    ACT_A = sb(plo, "ACT_A", [128, KC, TOK], BF16)
    ACT_B = sb(plo, "ACT_B", [128, KC, TOK], BF16)
    slots = [sb(plo, f"wslot{i}", [128, KC * 512], BF16) for i in range(NSLOT)]
    assert lo.top <= LO_AFTER

    blocks = []

    def wblock(ap2d, kch, ncols):
        blocks.append((ap2d, kch, ncols))
        return len(blocks) - 1

    blk_retA = [wblock(w_in[:, c0:c0 + n], KC, n) for (c0, n) in A_RET]
    blk_retB = [wblock(w_in[:, c0:c0 + n], KC, n) for (c0, n) in B_RET]
    blk_glaA = [wblock(w_in[:, c0:c0 + n], KC, n) for (c0, n) in A_GLA]
    blk_glaB = [wblock(w_in[:, c0:c0 + n], KC, n) for (c0, n) in B_GLA]
    order = []
    for h in range(4):
        order += [blk_retA[h], blk_retB[h]]
    for h in range(4):
        order += [blk_glaA[h], blk_glaB[h]]
    if need >= 1:
        blk_mix = [wblock(w_mix[:, j * 512:(j + 1) * 512], KC, 512) for j in range(4)]
        order += blk_mix
    if need >= 2:
        blk_mk = [wblock(w_mk[:, j * 512:(j + 1) * 512], KC, 512) for j in range(4)]
        blk_mv = [wblock(w_mv[:, j * 512:(j + 1) * 512], KC, 512) for j in range(4)]
        blk_mq = [wblock(w_mq[:, j * 512:(j + 1) * 512], KC, 512) for j in range(4)]
        blk_mo = [wblock(w_mo[:, j * 512:(j + 1) * 512], KC, 512) for j in range(4)]
        order += blk_mk + blk_mv + blk_mq + blk_mo
    blk_e = []
    for e in range(NEXP if need >= 3 else 0):
        g_ = wblock(w_eg[e], KC, 512)
        u_ = wblock(w_eu[e], KC, 512)
        d_ = wblock(w_ed[e], 4, 2048)
        blk_e.append((g_, u_, d_))
        order += [g_, u_, d_]
    pos_in_order = {b: i for i, b in enumerate(order)}
    issued = [0]
    blk_slot = {}

    def issue_upto(i):
        while issued[0] <= min(i, len(order) - 1):
            j = issued[0]
            b = order[j]
            ap2d, kch, ncols = blocks[b]
            s = j % NSLOT
            dst = slots[s][:, 0:kch * ncols].rearrange("p (k n) -> p k n", k=kch)
            dma(POOL, slot_ds[s], dst, ap2d.rearrange("(k p) n -> p k n", p=128), dst=slot_t[s])
            blk_slot[b] = s
            issued[0] += 1

    def wget(b, pf=2):
        i = pos_in_order[b]
        issue_upto(i + pf)
        s = blk_slot[b]
        ap2d, kch, ncols = blocks[b]
        v = slots[s][:, 0:kch * ncols].rearrange("p (k n) -> p k n", k=kch)
        return v, slot_t[s]

    d_xo = dsem("d_xo")
    for t in range(NT):
        dma(POOL, d_xo, ACT_A[:, :, t * 128:(t + 1) * 128], xTp[NPRE + t].rearrange("p (k t) -> p k t", k=KC))
    t_A.w = [(d_xo.sem, d_xo.count)]

    own = Scope(hi)
    cs_own = sb(own, "cs_own", [128, NT, 256], F32)
    t_cso = Tile("cs_own")
    g_ekb = sb(own, "g_ekb", [128, NT, 512], BF16)
    g_eb = sb(own, "g_eb", [128, NT, 512], BF16)
    g_enb = sb(own, "g_enb", [128, NT, 512], BF16)
    g_edec = sb(own, "g_edec", [128, NT, 4], F32)
    t_gown = Tile("gown")
    qbuf = sb(own, "qbuf", [128, 256], BF16)
    t_q = Tile("q")
    sgbuf = sb(own, "sgbuf", [128, 256], BF16)
    t_sg = Tile("sg")
    qkT = sb(own, "qkT", [128, 6, 128], BF16)
    t_qkT = Tile("qkT")
    sTb = sb(own, "sTb", [128, 128], BF16)
    t_sT = Tile("sT")
    stat = sb(own, "stat", [128, 8], F32)
    t_stat = Tile("stat")
    bst = sb(own, "bst", [128, 6], F32)
    ynorm = sb(own, "ynorm", [128, 256], F32)
    t_yn = Tile("yn")
    mixb = sb(own, "mixb", [128, 256], BF16)
    t_mixb = Tile("mixb")
    junk = ynorm
    t_junk = t_yn

    def xTown(t):
        return lambda k: ACT_A[:, k, t * 128:(t + 1) * 128]

    for t in range(NT):
        gen_cossin(NPRE + t, cs_own[:, t, :], t_cso)

    def finish_head_tile(hcol, t, is_ret, o_acc):
        o_ps = Fp[o_acc][:, 0:256]
        if is_ret:
            op(DVE, lambda: nc.vector.bn_stats(out=bst[:], in_=o_ps), reads=[Ft[o_acc]], writes=[t_stat])
            op(DVE, lambda: nc.vector.bn_aggr(out=stat[:, 0:2], in_=bst[:]), reads=[t_stat], writes=[t_stat])
            op(DVE, lambda: nc.vector.tensor_scalar(out=stat[:, 2:3], in0=stat[:, 1:2], scalar1=EPS, scalar2=None, op0=ALU.add),
               reads=[t_stat], writes=[t_stat])
        else:
            op(DVE, lambda: nc.vector.memset(stat[:, 0:1], 0.0), writes=[t_stat])
            op(ACT, lambda: nc.scalar.activation(out=junk[:], in_=o_ps, func=AF.Square, accum_out=stat[:, 0:1]),
               reads=[Ft[o_acc]], writes=[t_junk, t_stat])
            op(DVE, lambda: nc.vector.tensor_scalar(out=stat[:, 2:3], in0=stat[:, 0:1], scalar1=1.0 / 256.0, scalar2=EPS,
                                                    op0=ALU.mult, op1=ALU.add), reads=[t_stat], writes=[t_stat])
        op(ACT, lambda: nc.scalar.activation(out=stat[:, 3:4], in_=stat[:, 2:3], func=AF.Sqrt), reads=[t_stat], writes=[t_stat])
        op(DVE, lambda: nc.vector.reciprocal(out=stat[:, 4:5], in_=stat[:, 3:4]), reads=[t_stat], writes=[t_stat])
        if is_ret:
            op(DVE, lambda: nc.vector.tensor_scalar(out=ynorm[:], in0=o_ps, scalar1=stat[:, 0:1], scalar2=stat[:, 4:5],
                                                    op0=ALU.subtract, op1=ALU.mult), reads=[Ft[o_acc], t_stat], writes=[t_yn])
            op(DVE, lambda: nc.vector.tensor_tensor(out=mixb[:], in0=ynorm[:], in1=sgbuf[:], op=ALU.mult),
               reads=[t_yn, t_sg], writes=[t_mixb])
        else:
            op(DVE, lambda: nc.vector.scalar_tensor_tensor(out=mixb[:], in0=o_ps, scalar=stat[:, 4:5], in1=sgbuf[:],
                                                           op0=ALU.mult, op1=ALU.mult), reads=[Ft[o_acc], t_stat, t_sg], writes=[t_mixb])
        mmgroup(PE, [lambda c=c: nc.tensor.transpose(Tp[1][:, c * 128:(c + 1) * 128], mixb[:, c * 128:(c + 1) * 128], ident[:])
                     for c in range(2)], reads=[t_mixb, t_ident], writes=[Tt[1]])
        kk = hcol * 2
        op(ACT, lambda: nc.scalar.copy(out=ACT_B[:, kk:kk + 2, t * 128:(t + 1) * 128],
                                       in_=Tp[1][:, 0:256].rearrange("p (c t) -> p c t", c=2)), reads=[Tt[1]], writes=[t_Bt[t]])

    for h in range(4):
        WAv, WAt = wget(blk_retA[h], 2)
        WBv, WBt = wget(blk_retB[h], 1)
        for t in range(NT):
            pA = lambda tt: proj(xTown(tt), [t_A], lambda k: WAv[:, k, :], [WAt], 512, 0)
            pB = lambda tt: proj(xTown(tt), [t_A], lambda k: WBv[:, k, :], [WBt], 512, 1)
            if t == 0:
                pA(0)
                pB(0)
            rotary(Fp[0][:, 0:256], kbuf[0], [Ft[0]], t_k[0], cs_own[:, t, :], t_cso)
            rotary(Fp[1][:, 0:256], qbuf, [Ft[1]], t_q, cs_own[:, t, :], t_cso)
            op(ACT, lambda: nc.scalar.copy(out=vbuf[0][:], in_=Fp[0][:, 256:512]), reads=[Ft[0]], writes=[t_v[0]])
            op(DVE, lambda h=h: nc.vector.tensor_scalar(out=vhat[0][:], in0=Fp[0][:, 256:512], scalar1=C("DK", 2 * h, 2 * h + 1),
                                                        scalar2=None, op0=ALU.mult), reads=[Ft[0], t_const], writes=[t_vh[0]])
            op(ACT, lambda: nc.scalar.activation(out=sgbuf[:], in_=Fp[1][:, 256:512], func=AF.Silu), reads=[Ft[1]], writes=[t_sg])
            mmgroup(PE, [lambda c=c: nc.tensor.transpose(Tp[0][:, c * 128:(c + 1) * 128], qbuf[:, c * 128:(c + 1) * 128], ident[:])
                         for c in range(2)] +
                        [lambda c=c: nc.tensor.transpose(Tp[0][:, (2 + c) * 128:(3 + c) * 128], kbuf[0][:, c * 128:(c + 1) * 128], ident[:])
                         for c in range(2)], reads=[t_q, t_k[0], t_ident], writes=[Tt[0]])
            if t + 1 < NT:
                pA(t + 1)
            op(ACT, lambda: nc.scalar.copy(out=qkT[:, 0:4, :], in_=Tp[0][:, 0:512].rearrange("p (c t) -> p c t", c=4)),
               reads=[Tt[0]], writes=[t_qkT])
            op(DVE, lambda h=h: nc.vector.tensor_tensor(out=qkT[:, 4:6, :], in0=Tp[0][:, 0:256].rearrange("p (c t) -> p c t", c=2),
                                                        in1=C("DQ", h * 128, (h + 1) * 128).unsqueeze(1).to_broadcast([128, 2, 128]),
                                                        op=ALU.mult), reads=[Tt[0], t_const], writes=[t_qkT])
            mmgroup(PE, [lambda c=c: nc.tensor.matmul(Fp[2][:, 0:128], lhsT=qkT[:, 2 + c, :], rhs=qkT[:, c, :], start=(c == 0), stop=(c == 1))
                         for c in range(2)], reads=[t_qkT], writes=[Ft[2]])
            if t + 1 < NT:
                pB(t + 1)
            op(DVE, lambda h=h: nc.vector.tensor_tensor(out=sTb[:], in0=Fp[2][:, 0:128], in1=C("DT", h * 128, (h + 1) * 128), op=ALU.mult),
               reads=[Ft[2], t_const], writes=[t_sT])
            mmgroup(PE, [lambda: nc.tensor.matmul(Fp[3][:, 0:256], lhsT=sTb[:], rhs=vbuf[0][:], start=True, stop=False)] +
                        [lambda c=c, h=h: nc.tensor.matmul(Fp[3][:, 0:256], lhsT=qkT[:, 4 + c, :], rhs=Rb[:, h, c * 256:(c + 1) * 256],
                                                           start=False, stop=(c == 1)) for c in range(2)],
                    reads=[t_sT, t_v[0], t_qkT, t_Rb[h]], writes=[Ft[3]])
            ret_state_update(h, kbuf[0], t_k[0], vhat[0][:], t_vh[0])
            op(ACT, lambda h=h: nc.scalar.copy(out=Rb[:, h, :], in_=Rst[:, h, :]), reads=[t_R[h]], writes=[t_Rb[h]])
            finish_head_tile(h, t, True, 3)

    WAv0, WAt0 = None, None
    d_lr = dsem("d_lr")
    wlr = sb(own, "wlr", [128, KC, 16], BF16)
    t_wlr = Tile("wlr")
    dma(POOL, d_lr, wlr[:], w_in[:, A_LR[0]:A_LR[0] + 16].rearrange("(k p) n -> p k n", p=128), dst=t_wlr)
    for t in range(NT):
        gla_gates(NPRE + t, xTown(t), [t_A], lambda k: wlr[:, k, :], [t_wlr], own=True)
        op(DVE, lambda t=t: nc.vector.tensor_copy(out=g_ekb[:, t, :], in_=gt_ekb[:]), reads=[t_gt], writes=[t_gown])
        op(DVE, lambda t=t: nc.vector.tensor_copy(out=g_eb[:, t, :], in_=gt_eb[:]), reads=[t_gt], writes=[t_gown])
        op(DVE, lambda t=t: nc.vector.tensor_copy(out=g_enb[:, t, :], in_=gt_enb[:]), reads=[t_gt], writes=[t_gown])
        op(DVE, lambda t=t: nc.vector.tensor_copy(out=g_edec[:, t, :], in_=gt_edec[:]), reads=[t_gt], writes=[t_gown])

    for h in range(4):
        WAv, WAt = wget(blk_glaA[h], 2)
        WBv, WBt = wget(blk_glaB[h], 1)
        hs = slice(h * 128, (h + 1) * 128)
        for t in range(NT):
            pA = lambda tt: proj(xTown(tt), [t_A], lambda k: WAv[:, k, :], [WAt], 384, 0)
            pB = lambda tt: proj(xTown(tt), [t_A], lambda k: WBv[:, k, :], [WBt], 384, 1)
            if t == 0:
                pA(0)
                pB(0)
            op(DVE, lambda t=t: nc.vector.tensor_tensor(out=kbuf[0][:, 0:128], in0=Fp[0][:, 0:128], in1=g_enb[:, t, hs], op=ALU.mult),
               reads=[Ft[0], t_gown], writes=[t_k[0]])
            op(DVE, lambda t=t: nc.vector.tensor_tensor(out=kbuf[1][:, 0:128], in0=Fp[0][:, 0:128], in1=g_ekb[:, t, hs], op=ALU.mult),
               reads=[Ft[0], t_gown], writes=[t_k[1]])
            op(DVE, lambda t=t: nc.vector.scalar_tensor_tensor(out=qbuf[:, 0:128], in0=Fp[1][:, 0:128], scalar=128.0 ** -0.5,
                                                               in1=g_eb[:, t, hs], op0=ALU.mult, op1=ALU.mult),
               reads=[Ft[1], t_gown], writes=[t_q])
            op(ACT, lambda: nc.scalar.copy(out=vbuf[0][:], in_=Fp[0][:, 128:384]), reads=[Ft[0]], writes=[t_v[0]])
            op(ACT, lambda: nc.scalar.activation(out=ynorm[:], in_=Fp[1][:, 128:384], func=AF.Silu), reads=[Ft[1]], writes=[t_yn])
            op(DVE, lambda: nc.vector.tensor_tensor(out=sgbuf[:], in0=ynorm[:], in1=gnb[:], op=ALU.mult),
               reads=[t_yn, t_const], writes=[t_sg])
            mmgroup(PE, [lambda: nc.tensor.transpose(Tp[0][:, 0:128], qbuf[:, 0:128], ident[:]),
                         lambda: nc.tensor.transpose(Tp[0][:, 128:256], kbuf[0][:, 0:128], ident[:])],
                    reads=[t_q, t_k[0], t_ident], writes=[Tt[0]])
            if t + 1 < NT:
                pA(t + 1)
            op(ACT, lambda: nc.scalar.copy(out=qkT[:, 0:2, :], in_=Tp[0][:, 0:256].rearrange("p (c t) -> p c t", c=2)),
               reads=[Tt[0]], writes=[t_qkT])
            op(PE, lambda: nc.tensor.matmul(Fp[2][:, 0:128], lhsT=qkT[:, 1, :], rhs=qkT[:, 0, :], start=True, stop=True),
               reads=[t_qkT], writes=[Ft[2]])
            if t + 1 < NT:
                pB(t + 1)
            op(DVE, lambda: nc.vector.tensor_tensor(out=sTb[:], in0=Fp[2][:, 0:128], in1=C("CAUS"), op=ALU.mult),
               reads=[Ft[2], t_const], writes=[t_sT])
            mmgroup(PE, [lambda: nc.tensor.matmul(Fp[3][:, 0:256], lhsT=sTb[:], rhs=vbuf[0][:], start=True, stop=False),
                         lambda h=h: nc.tensor.matmul(Fp[3][:, 0:256], lhsT=qkT[:, 0, :], rhs=Sb[:, h, :], start=False, stop=True)],
                    reads=[t_sT, t_v[0], t_qkT, t_Sb[h]], writes=[Ft[3]])
            op(PE, lambda: nc.tensor.matmul(Fp[4][:, 0:256], lhsT=kbuf[1][:, 0:128], rhs=vbuf[0][:], start=True, stop=True),
               reads=[t_k[1], t_v[0]], writes=[Ft[4]])
            op(DVE, lambda h=h, t=t: nc.vector.scalar_tensor_tensor(out=Sst[:, h, :], in0=Sst[:, h, :], scalar=g_edec[:, t, h:h + 1],
                                                                    in1=Fp[4][:, 0:256], op0=ALU.mult, op1=ALU.add),
               reads=[Ft[4], t_S[h], t_gown], writes=[t_S[h]])
            op(ACT, lambda h=h: nc.scalar.copy(out=Sb[:, h, :], in_=Sst[:, h, :]), reads=[t_S[h]], writes=[t_Sb[h]])
            finish_head_tile(4 + h, t, False, 3)

    if DEBUG:
        for t in range(NT):
            t_B.w += t_Bt[t].w
        dma(SP, d_dbg, dbg_mixT, ACT_B[:].rearrange("p k t -> p (k t)"), src=t_B)
    barrier()
    own.close()
    s1.close()
    if STOP == "1b":
        return finish()

    assert hi.top == hi.base
    hi.base = hi.top = LO_AFTER
    s2 = Scope(hi)
    RES = sb(s2, "RES", [128, NT, D], F32)
    t_res = [Tile(f"res{t}") for t in range(NT)]
    t_ln = Tile("ln")
    d_ln = dsem("d_ln")
    d_x = dsem("d_x")
    for t in range(NT):
        dma(SP, d_x, RES[:, t, :], xown[t * 128:(t + 1) * 128, :])
    for t in range(NT):
        t_res[t].w = [(d_x.sem, d_x.count)]
    t_lnst = Tile("lnst")
    t_hb = Tile("hb")
    lnS = Scope(hi)
    lnt = sb(lnS, "lnt", [128, 2, D], F32)
    lnst = sb(lnS, "lnst", [128, 4, 6], F32)
    lnmv = sb(lnS, "lnmv", [128, 8], F32)
    hb = sb(lnS, "hb", [128, D], BF16)

    def load_ln(i):
        dma(SP, d_ln, lnt[:, 0, :], ln_d[2 * i:2 * i + 1, :].partition_broadcast(128), dst=t_ln)
        dma(SP, d_ln, lnt[:, 1, :], ln_d[2 * i + 1:2 * i + 2, :].partition_broadcast(128), dst=t_ln)

    def linear(AT, at_tiles, blks, epilogue, ntiles=NT):
        for j, b in enumerate(blks):
            Wv, Wt = wget(b)
            for t in range(ntiles):
                ai = (j * ntiles + t) % 2
                mmgroup(PE, [lambda k=k, t=t: nc.tensor.matmul(Fp[ai][:, :], lhsT=AT[:, k, t * 128:(t + 1) * 128], rhs=Wv[:, k, :],
                                                               start=(k == 0), stop=(k == KC - 1)) for k in range(KC)],
                        reads=list(at_tiles(t)) + [Wt], writes=[Ft[ai]])
                epilogue(j, t, ai)

    def resid_epi(j, t, ai):
        op(DVE, lambda: nc.vector.scalar_tensor_tensor(out=RES[:, t, j * 512:(j + 1) * 512], in0=RES[:, t, j * 512:(j + 1) * 512],
                                                       scalar=ALPHA, in1=Fp[ai][:, :], op0=ALU.mult, op1=ALU.add),
           reads=[Ft[ai], t_res[t]], writes=[t_res[t]])

    def layernorm(t, dstT, dst_tiles, want_tok_bf16=None):
        for c in range(4):
            op(DVE, lambda c=c: nc.vector.bn_stats(out=lnst[:, c, :], in_=RES[:, t, c * 512:(c + 1) * 512]),
               reads=[t_res[t]], writes=[t_lnst])
        op(DVE, lambda: nc.vector.bn_aggr(out=lnmv[:, 0:2], in_=lnst[:].rearrange("p a b -> p (a b)")), reads=[t_lnst], writes=[t_lnst])
        op(DVE, lambda: nc.vector.tensor_scalar(out=lnmv[:, 2:3], in0=lnmv[:, 1:2], scalar1=EPS, scalar2=None, op0=ALU.add),
           reads=[t_lnst], writes=[t_lnst])
        op(ACT, lambda: nc.scalar.activation(out=lnmv[:, 3:4], in_=lnmv[:, 2:3], func=AF.Sqrt), reads=[t_lnst], writes=[t_lnst])
        op(DVE, lambda: nc.vector.reciprocal(out=lnmv[:, 4:5], in_=lnmv[:, 3:4]), reads=[t_lnst], writes=[t_lnst])
        op(DVE, lambda: nc.vector.tensor_scalar(out=RES[:, t, :], in0=RES[:, t, :], scalar1=lnmv[:, 0:1], scalar2=lnmv[:, 4:5],
                                                op0=ALU.subtract, op1=ALU.mult), reads=[t_res[t], t_lnst], writes=[t_res[t]])
        op(DVE, lambda: nc.vector.tensor_tensor(out=RES[:, t, :], in0=RES[:, t, :], in1=lnt[:, 0, :], op=ALU.mult),
           reads=[t_res[t], t_ln], writes=[t_res[t]])
        op(DVE, lambda: nc.vector.tensor_tensor(out=RES[:, t, :], in0=RES[:, t, :], in1=lnt[:, 1, :], op=ALU.add),
           reads=[t_res[t], t_ln], writes=[t_res[t]])
        if dstT is not None:
            hbt = hb[:] if want_tok_bf16 is None else want_tok_bf16
            hbt_tile = t_hb
            op(ACT, lambda: nc.scalar.copy(out=hbt, in_=RES[:, t, :]), reads=[t_res[t]], writes=[hbt_tile])
            for half in range(2):
                mmgroup(PE, [lambda c=c: nc.tensor.transpose(Tp[half][:, c * 128:(c + 1) * 128],
                                                             hbt[:, (half * 8 + c) * 128:(half * 8 + c + 1) * 128], ident[:])
                             for c in range(8)], reads=[hbt_tile, t_ident], writes=[Tt[half]])
                op(ACT if half == 0 else DVE,
                   (lambda half=half: nc.scalar.copy(out=dstT[:, half * 8:(half + 1) * 8, t * 128:(t + 1) * 128],
                                                     in_=Tp[half][:].rearrange("p (c t) -> p c t", c=8))) if half == 0 else
                   (lambda half=half: nc.vector.tensor_copy(out=dstT[:, half * 8:(half + 1) * 8, t * 128:(t + 1) * 128],
                                                            in_=Tp[half][:].rearrange("p (c t) -> p c t", c=8))),
                   reads=[Tt[half]], writes=[dst_tiles[t]])

    load_ln(0)
    linear(ACT_B, lambda t: [t_Bt[t]], blk_mix, resid_epi)
    for t in range(NT):
        layernorm(t, ACT_A, t_At)
    if DEBUG:
        for t in range(NT):
            dma(SP, d_dbg, dbg_h1[t * 128:(t + 1) * 128, :], RES[:, t, :], src=t_res[t])

    barrier()
    lnS.close()
    if STOP == "2":
        return finish()
    s3 = Scope(hi)
    memT = ACT_B[:, :, 0:256]
    mem_tiles = [t_Bt[0], t_Bt[1]]
    d_mem = dsem("d_mem")
    dma(POOL, d_mem, memT, memT_d.rearrange("p (k m) -> p k m", k=KC), dst=mem_tiles)
    KT = sb(s3, "KT", [128, KC, 256], BF16)
    t_KT = Tile("KT")
    Vm = sb(s3, "Vm", [128, 2, D], BF16)
    t_Vm = Tile("Vm")
    kvb = sb(s3, "kvb", [128, 512], BF16)
    t_kvb = Tile("kvb")
    qb = sb(s3, "qb", [128, 512], BF16)
    t_qb = Tile("qb")
    qTh = sb(s3, "qTh", [128, 4, 128], BF16)
    t_qTh = Tile("qTh")
    pexp = sb(s3, "pexp", [128, 256], BF16)
    t_pexp = Tile("pexp")
    pT = sb(s3, "pT", [128, 2, 128], BF16)
    t_pT = Tile("pT")
    sm = sb(s3, "sm", [128, 8], F32)
    t_sm = Tile("sm")
    ob = sb(s3, "ob", [128, 512], BF16)
    t_ob = Tile("ob")

    def k_epi(j, mt, ai):
        op(ACT, lambda: nc.scalar.copy(out=kvb[:], in_=Fp[ai][:, :]), reads=[Ft[ai]], writes=[t_kvb])
        mmgroup(PE, [lambda c=c: nc.tensor.transpose(Tp[0][:, c * 128:(c + 1) * 128], kvb[:, c * 128:(c + 1) * 128], ident[:])
                     for c in range(4)], reads=[t_kvb, t_ident], writes=[Tt[0]])
        op(DVE, lambda: nc.vector.tensor_copy(out=KT[:, j * 4:(j + 1) * 4, mt * 128:(mt + 1) * 128],
                                              in_=Tp[0][:, 0:512].rearrange("p (c t) -> p c t", c=4)), reads=[Tt[0]], writes=[t_KT])

    def v_epi(j, mt, ai):
        op(ACT, lambda: nc.scalar.copy(out=Vm[:, mt, j * 512:(j + 1) * 512], in_=Fp[ai][:, :]), reads=[Ft[ai]], writes=[t_Vm])

    linear(memT, lambda t: mem_tiles, blk_mk, k_epi, ntiles=2)
    linear(memT, lambda t: mem_tiles, blk_mv, v_epi, ntiles=2)

    SCL = 512.0 ** -0.5

    def q_epi(j, t, ai):
        op(ACT, lambda: nc.scalar.mul(out=qb[:], in_=Fp[ai][:, :], mul=SCL), reads=[Ft[ai]], writes=[t_qb])
        mmgroup(PE, [lambda c=c: nc.tensor.transpose(Tp[0][:, c * 128:(c + 1) * 128], qb[:, c * 128:(c + 1) * 128], ident[:])
                     for c in range(4)], reads=[t_qb, t_ident], writes=[Tt[0]])
        op(DVE, lambda: nc.vector.tensor_copy(out=qTh[:], in_=Tp[0][:, 0:512].rearrange("p (c t) -> p c t", c=4)),
           reads=[Tt[0]], writes=[t_qTh])
        mmgroup(PE, [lambda c=c: nc.tensor.matmul(Fp[2][:, 0:256], lhsT=qTh[:, c, :], rhs=KT[:, j * 4 + c, :], start=(c == 0), stop=(c == 3))
                     for c in range(4)], reads=[t_qTh, t_KT], writes=[Ft[2]])
        op(DVE, lambda: nc.vector.reduce_max(out=sm[:, 0:1], in_=Fp[2][:, 0:256], axis=AX.X), reads=[Ft[2]], writes=[t_sm])
        op(DVE, lambda: nc.vector.tensor_scalar(out=sm[:, 2:3], in0=sm[:, 0:1], scalar1=-1.0, scalar2=None, op0=ALU.mult),
           reads=[t_sm], writes=[t_sm])
        op(DVE, lambda: nc.vector.memset(sm[:, 4:5], 0.0), writes=[t_sm])
        op(ACT, lambda: nc.scalar.activation(out=pexp[:], in_=Fp[2][:, 0:256], func=AF.Exp, bias=sm[:, 2:3], accum_out=sm[:, 4:5]),
           reads=[Ft[2], t_sm], writes=[t_pexp, t_sm])
        op(DVE, lambda: nc.vector.reciprocal(out=sm[:, 6:7], in_=sm[:, 4:5]), reads=[t_sm], writes=[t_sm])
        mmgroup(PE, [lambda c=c: nc.tensor.transpose(Tp[1][:, c * 128:(c + 1) * 128], pexp[:, c * 128:(c + 1) * 128], ident[:])
                     for c in range(2)], reads=[t_pexp, t_ident], writes=[Tt[1]])
        op(DVE, lambda: nc.vector.tensor_copy(out=pT[:], in_=Tp[1][:, 0:256].rearrange("p (c t) -> p c t", c=2)),
           reads=[Tt[1]], writes=[t_pT])
        mmgroup(PE, [lambda mt=mt: nc.tensor.matmul(Fp[3][:, :], lhsT=pT[:, mt, :], rhs=Vm[:, mt, j * 512:(j + 1) * 512],
                                                    start=(mt == 0), stop=(mt == 1)) for mt in range(2)],
                reads=[t_pT, t_Vm], writes=[Ft[3]])
        op(DVE, lambda: nc.vector.tensor_scalar(out=ob[:], in0=Fp[3][:, :], scalar1=sm[:, 6:7], scalar2=None, op0=ALU.mult),
           reads=[Ft[3], t_sm], writes=[t_ob])
        mmgroup(PE, [lambda c=c: nc.tensor.transpose(Tp[0][:, c * 128:(c + 1) * 128], ob[:, c * 128:(c + 1) * 128], ident[:])
                     for c in range(4)], reads=[t_ob, t_ident], writes=[Tt[0]])
        op(DVE, lambda: nc.vector.tensor_copy(out=ACT_B[:, j * 4:(j + 1) * 4, t * 128:(t + 1) * 128],
                                              in_=Tp[0][:, 0:512].rearrange("p (c t) -> p c t", c=4)), reads=[Tt[0]], writes=[t_Bt[t]])

    linear(ACT_A, lambda t: [t_At[t]], blk_mq, q_epi)
    linear(ACT_B, lambda t: [t_Bt[t]], blk_mo, resid_epi)
    barrier()
    s3.close()

    lnS = Scope(hi)
    lnt = sb(lnS, "lnt2", [128, 2, D], F32)
    lnst = sb(lnS, "lnst2", [128, 4, 6], F32)
    lnmv = sb(lnS, "lnmv2", [128, 8], F32)
    hb = sb(lnS, "hb2", [128, D], BF16)
    load_ln(1)
    for t in range(NT):
        layernorm(t, ACT_A, t_At)
    if DEBUG:
        for t in range(NT):
            dma(SP, d_dbg, dbg_h2[t * 128:(t + 1) * 128, :], RES[:, t, :], src=t_res[t])
    barrier()
    lnS.close()
    if STOP == "3":
        return finish()
    s4 = Scope(hi)
    wr = sb(s4, "wr", [128, KC, 36], BF16)
    t_wr = Tile("wr")
    d_wr = dsem("d_wr")
    dma(POOL, d_wr, wr[:], wroute_d.rearrange("(k p) n -> p k n", p=128), dst=t_wr)
    brt = sb(s4, "brt", [128, 36], F32)
    d_wr2 = dsem("d_wr2")
    dma(SP, d_wr2, brt[:], broute_d.partition_broadcast(128))
    t_wr.w = [(d_wr.sem, d_wr.count), (d_wr2.sem, d_wr2.count)]
    lg = sb(s4, "lg", [128, 36], F32)
    t_lg = Tile("lg")
    rs = sb(s4, "rs", [128, 16], F32)
    t_rs = Tile("rs")
    ohg = sb(s4, "ohg", [128, 4], F32)
    f48 = sb(s4, "f48", [128, 32], F32)
    fsel = sb(s4, "fsel", [128, 8], F32)
    oh1 = sb(s4, "oh1", [128, 8], F32)
    oh2 = sb(s4, "oh2", [128, 8], F32)
    fm = sb(s4, "fm", [128, 8], F32)
    gw8 = sb(s4, "gw8", [128, 8], F32)
    Gd = sb(s4, "Gd", [128, NT, 32], F32)
    Md = sb(s4, "Md", [128, NT, 32], F32)
    Mb = sb(s4, "Mb", [128, NT, 32], BF16)
    Ghl = sb(s4, "Ghl", [128, NT, 32, 2], BF16)
    Gtmp = sb(s4, "Gtmp", [128, 32], F32)
    posd = sb(s4, "posd", [128, NT, 32], F32)
    t_rt = [Tile(f"rt{t}") for t in range(NT)]
    t_M = Tile("M")

    H2 = ACT_B[:].rearrange("p k t -> p (k t)").rearrange("p (a d) -> p a d", a=NT)
    for t in range(NT):
        op(ACT, lambda t=t: nc.scalar.copy(out=H2[:, t, :], in_=RES[:, t, :]), reads=[t_res[t]], writes=t_Bt + [t_B])
    for t in range(NT):
        mmgroup(PE, [lambda k=k, t=t: nc.tensor.matmul(Fp[2][:, 0:36], lhsT=ACT_A[:, k, t * 128:(t + 1) * 128], rhs=wr[:, k, :],
                                                       start=(k == 0), stop=(k == KC - 1)) for k in range(KC)],
                reads=[t_At[t], t_wr], writes=[Ft[2]])
        R_ = [t_lg, t_rs]
        op(DVE, lambda: nc.vector.tensor_tensor(out=lg[:], in0=Fp[2][:, 0:36], in1=brt[:], op=ALU.add), reads=[Ft[2], t_wr], writes=[t_lg])
        op(DVE, lambda: nc.vector.reduce_max(out=rs[:, 0:1], in_=lg[:, 0:4], axis=AX.X), reads=[t_lg], writes=[t_rs])
        op(DVE, lambda: nc.vector.tensor_scalar(out=ohg[:], in0=lg[:, 0:4], scalar1=rs[:, 0:1], scalar2=None, op0=ALU.is_equal),
           reads=R_, writes=[t_rs])
        op(DVE, lambda: nc.vector.tensor_scalar(out=rs[:, 14:15], in0=rs[:, 0:1], scalar1=-1.0, scalar2=None, op0=ALU.mult),
           reads=R_, writes=[t_rs])
        op(DVE, lambda: nc.vector.memset(rs[:, 2:3], 0.0), reads=R_, writes=[t_rs])
        op(ACT, lambda: nc.scalar.activation(out=rs[:, 4:8], in_=lg[:, 0:4], func=AF.Exp, bias=rs[:, 14:15], accum_out=rs[:, 2:3]),
           reads=R_, writes=[t_rs])
        op(DVE, lambda: nc.vector.reciprocal(out=rs[:, 3:4], in_=rs[:, 2:3]), reads=R_, writes=[t_rs])
        op(DVE, lambda: nc.vector.tensor_tensor(out=f48[:].rearrange("p (g e) -> p g e", g=4),
                                                in0=lg[:, 4:36].rearrange("p (g e) -> p g e", g=4),
                                                in1=ohg[:].unsqueeze(2).to_broadcast([128, 4, 8]), op=ALU.mult), reads=R_, writes=[t_rs])
        op(DVE, lambda: nc.vector.reduce_sum(out=fsel[:], in_=f48[:].rearrange("p (g e) -> p e g", g=4), axis=AX.X),
           reads=R_, writes=[t_rs])
        op(DVE, lambda: nc.vector.reduce_max(out=rs[:, 8:9], in_=fsel[:], axis=AX.X), reads=R_, writes=[t_rs])
        op(DVE, lambda: nc.vector.tensor_scalar(out=oh1[:], in0=fsel[:], scalar1=rs[:, 8:9], scalar2=None, op0=ALU.is_equal),
           reads=R_, writes=[t_rs])
        op(DVE, lambda: nc.vector.scalar_tensor_tensor(out=fm[:], in0=oh1[:], scalar=-1e30, in1=fsel[:], op0=ALU.mult, op1=ALU.add),
           reads=R_, writes=[t_rs])
        op(DVE, lambda: nc.vector.reduce_max(out=rs[:, 9:10], in_=fm[:], axis=AX.X), reads=R_, writes=[t_rs])
        op(DVE, lambda: nc.vector.tensor_scalar(out=oh2[:], in0=fm[:], scalar1=rs[:, 9:10], scalar2=None, op0=ALU.is_equal),
           reads=R_, writes=[t_rs])
        op(DVE, lambda: nc.vector.tensor_tensor(out=rs[:, 10:11], in0=rs[:, 8:9], in1=rs[:, 9:10], op=ALU.subtract), reads=R_, writes=[t_rs])
        op(ACT, lambda: nc.scalar.activation(out=rs[:, 11:12], in_=rs[:, 10:11], func=AF.Sigmoid), reads=R_, writes=[t_rs])
        op(DVE, lambda: nc.vector.tensor_tensor(out=rs[:, 12:13], in0=rs[:, 11:12], in1=rs[:, 3:4], op=ALU.mult), reads=R_, writes=[t_rs])
        op(DVE, lambda: nc.vector.tensor_tensor(out=rs[:, 13:14], in0=rs[:, 3:4], in1=rs[:, 12:13], op=ALU.subtract), reads=R_, writes=[t_rs])
        op(DVE, lambda: nc.vector.tensor_scalar(out=gw8[:], in0=oh1[:], scalar1=rs[:, 12:13], scalar2=None, op0=ALU.mult), reads=R_, writes=[t_rs])
        op(DVE, lambda: nc.vector.scalar_tensor_tensor(out=gw8[:], in0=oh2[:], scalar=rs[:, 13:14], in1=gw8[:], op0=ALU.mult, op1=ALU.add),
           reads=R_, writes=[t_rs])
        op(DVE, lambda t=t: nc.vector.tensor_tensor(out=Gd[:, t, :].rearrange("p (g e) -> p g e", g=4),
                                                    in0=ohg[:].unsqueeze(2).to_broadcast([128, 4, 8]),
                                                    in1=gw8[:].unsqueeze(1).to_broadcast([128, 4, 8]), op=ALU.mult), reads=R_, writes=[t_rt[t]])
        op(DVE, lambda: nc.vector.tensor_tensor(out=oh1[:], in0=oh1[:], in1=oh2[:], op=ALU.add), reads=R_, writes=[t_rs])
        op(DVE, lambda t=t: nc.vector.tensor_tensor(out=Md[:, t, :].rearrange("p (g e) -> p g e", g=4),
                                                    in0=ohg[:].unsqueeze(2).to_broadcast([128, 4, 8]),
                                                    in1=oh1[:].unsqueeze(1).to_broadcast([128, 4, 8]), op=ALU.mult), reads=R_, writes=[t_rt[t]])
        op(DVE, lambda t=t: nc.vector.tensor_copy(out=Mb[:, t, :], in_=Md[:, t, :]), reads=[t_rt[t]], writes=[t_rt[t]])
        op(DVE, lambda t=t: nc.vector.tensor_copy(out=Ghl[:, t, :, 0], in_=Gd[:, t, :]), reads=[t_rt[t]], writes=[t_rt[t]])
        op(DVE, lambda t=t: nc.vector.tensor_tensor(out=Gtmp[:], in0=Gd[:, t, :], in1=Ghl[:, t, :, 0], op=ALU.subtract),
           reads=[t_rt[t]], writes=[t_rs])
        op(DVE, lambda t=t: nc.vector.tensor_copy(out=Ghl[:, t, :, 1], in_=Gtmp[:]), reads=[t_rs], writes=[t_rt[t]])
        op(POOL, lambda t=t: nc.gpsimd.tensor_scalar(out=RES[:, t, :], in0=RES[:, t, :], scalar1=ALPHA, scalar2=None, op0=ALU.mult),
           reads=[t_res[t], t_Bt[t]], writes=[t_res[t]])
    for t in range(NT):
        fns = [lambda tp=tp: nc.tensor.matmul(Fp[2][:, 0:32], lhsT=CTb[:, 0:128], rhs=Mb[:, tp, :], start=(tp == 0), stop=False)
               for tp in range(t)]
        fns.append(lambda t=t: nc.tensor.matmul(Fp[2][:, 0:32], lhsT=CTb[:, 128:256], rhs=Mb[:, t, :], start=(t == 0), stop=True))
        mmgroup(PE, fns, reads=[t_rt[tp] for tp in range(t + 1)] + [t_ctb], writes=[Ft[2]])
        op(DVE, lambda t=t: nc.vector.tensor_copy(out=posd[:, t, :], in_=Fp[2][:, 0:32]), reads=[Ft[2]], writes=[t_rt[t]])

    Psel = sb(s4, "Psel", [128, NT, CAP], BF16)
    t_Psel = Tile("Psel")
    PselT = [sb(s4, f"PselT{i}", [128, NT, 128], BF16) for i in range(2)]
    t_PselT = [Tile(f"PselT{i}") for i in range(2)]
    xeT = sb(s4, "xeT", [128, KC, CAP], BF16)
    t_xeT = Tile("xeT")
    gs = sb(s4, "gs", [128, 2], F32)
    t_gs = Tile("gs")
    hg = sb(s4, "hg", [128, 512], BF16)
    t_hg = Tile("hg")
    hid = sb(s4, "hid", [128, 512], BF16)
    t_hid = Tile("hid")
    hidT = sb(s4, "hidT", [128, 4, CAP], BF16)
    t_hidT = Tile("hidT")
    yb = [sb(s4, f"yb{i}", [128, D], BF16) for i in range(2)]
    t_yb = [Tile(f"yb{i}") for i in range(2)]
    t_rt_all = t_rt

    for e in range(NEXP):
        gB, uB, dB = blk_e[e]
        pi = e % 2
        for t in range(NT):
            op(DVE, lambda t=t, e=e: nc.vector.tensor_scalar(out=Psel[:, t, :], in0=C("IOTA"), scalar1=posd[:, t, e:e + 1],
                                                             scalar2=Md[:, t, e:e + 1], op0=ALU.is_equal, op1=ALU.mult),
               reads=[t_rt[t], t_const], writes=[t_Psel])
        mmgroup(PE, [lambda t=t: nc.tensor.transpose(Tp[0][:, t * 128:(t + 1) * 128], Psel[:, t, :], ident[:]) for t in range(NT)],
                reads=[t_Psel, t_ident], writes=[Tt[0]])
        op(ACT, lambda pi=pi: nc.scalar.copy(out=PselT[pi][:], in_=Tp[0][:, 0:NT * 128].rearrange("p (a t) -> p a t", a=NT)),
           reads=[Tt[0]], writes=[t_PselT[pi]])
        mmgroup(PE, [lambda t=t, e=e: nc.tensor.matmul(Fp[5][:, 0:2], lhsT=Psel[:, t, :], rhs=Ghl[:, t, e, :], start=(t == 0), stop=(t == NT - 1))
                     for t in range(NT)], reads=[t_Psel] + t_rt_all, writes=[Ft[5]])
        op(DVE, lambda: nc.vector.reduce_sum(out=gs[:, 0:1], in_=Fp[5][:, 0:2], axis=AX.X), reads=[Ft[5]], writes=[t_gs])
        for kq in range(4):
            ai = kq % 2
            fns = []
            for kk in range(4):
                k = kq * 4 + kk
                for t in range(NT):
                    fns.append(lambda k=k, kk=kk, t=t: nc.tensor.matmul(Fp[ai][:, kk * 128:(kk + 1) * 128], lhsT=H2[:, t, k * 128:(k + 1) * 128],
                                                                        rhs=Psel[:, t, :], start=(t == 0), stop=(t == NT - 1)))
            mmgroup(PE, fns, reads=[t_Psel, t_B], writes=[Ft[ai]])
            op(ACT if kq % 2 == 0 else DVE,
               (lambda kq=kq, ai=ai: nc.scalar.copy(out=xeT[:, kq * 4:(kq + 1) * 4, :], in_=Fp[ai][:, :].rearrange("p (c s) -> p c s", c=4)))
               if kq % 2 == 0 else
               (lambda kq=kq, ai=ai: nc.vector.tensor_copy(out=xeT[:, kq * 4:(kq + 1) * 4, :], in_=Fp[ai][:, :].rearrange("p (c s) -> p c s", c=4))),
               reads=[Ft[ai]], writes=[t_xeT])
        Wg, Wgt = wget(gB)
        mmgroup(PE, [lambda k=k: nc.tensor.matmul(Fp[2][:, :], lhsT=xeT[:, k, :], rhs=Wg[:, k, :], start=(k == 0), stop=(k == KC - 1))
                     for k in range(KC)], reads=[t_xeT, Wgt], writes=[Ft[2]])
        Wu, Wut = wget(uB)
        mmgroup(PE, [lambda k=k: nc.tensor.matmul(Fp[3][:, :], lhsT=xeT[:, k, :], rhs=Wu[:, k, :], start=(k == 0), stop=(k == KC - 1))
                     for k in range(KC)], reads=[t_xeT, Wut], writes=[Ft[3]])
        op(ACT, lambda: nc.scalar.activation(out=hg[:], in_=Fp[2][:, :], func=AF.Silu), reads=[Ft[2]], writes=[t_hg])
        op(DVE, lambda: nc.vector.tensor_tensor(out=hid[:], in0=hg[:], in1=Fp[3][:, :], op=ALU.mult), reads=[t_hg, Ft[3]], writes=[t_hid])
        mmgroup(PE, [lambda c=c: nc.tensor.transpose(Tp[1][:, c * 128:(c + 1) * 128], hid[:, c * 128:(c + 1) * 128], ident[:])
                     for c in range(4)], reads=[t_hid, t_ident], writes=[Tt[1]])
        op(ACT, lambda: nc.scalar.copy(out=hidT[:], in_=Tp[1][:, 0:512].rearrange("p (c s) -> p c s", c=4)), reads=[Tt[1]], writes=[t_hidT])
        Wd, Wdt = wget(dB)
        for cb in range(4):
            ai = cb % 2
            mmgroup(PE, [lambda c=c, cb=cb: nc.tensor.matmul(Fp[ai][:, :], lhsT=hidT[:, c, :], rhs=Wd[:, c, cb * 512:(cb + 1) * 512],
                                                             start=(c == 0), stop=(c == 3)) for c in range(4)],
                    reads=[t_hidT, Wdt], writes=[Ft[ai]])
            op(DVE, lambda cb=cb, ai=ai, pi=pi: nc.vector.tensor_scalar(out=yb[pi][:, cb * 512:(cb + 1) * 512], in0=Fp[ai][:, :],
                                                                        scalar1=gs[:, 0:1], scalar2=None, op0=ALU.mult),
               reads=[Ft[ai], t_gs], writes=[t_yb[pi]])
        if e % 2 == 1:
            for t in range(NT):
                for cb in range(4):
                    ai = 4 + (t * 4 + cb) % 2
                    mmgroup(PE, [lambda q=q, t=t, cb=cb: nc.tensor.matmul(Fp[ai][:, :], lhsT=PselT[q][:, t, :], rhs=yb[q][:, cb * 512:(cb + 1) * 512],
                                                                          start=(q == 0), stop=(q == 1)) for q in range(2)],
                            reads=t_PselT + t_yb, writes=[Ft[ai]])
                    op(DVE, (lambda t=t, cb=cb, ai=ai: nc.vector.tensor_tensor(out=RES[:, t, cb * 512:(cb + 1) * 512],
                                                                               in0=RES[:, t, cb * 512:(cb + 1) * 512],
                                                                               in1=Fp[ai][:, :], op=ALU.add)),
                       reads=[Ft[ai], t_res[t]], writes=[t_res[t]])

    barrier()
    s4.close()
    lnS = Scope(hi)
    lnt = sb(lnS, "lnt3", [128, 2, D], F32)
    lnst = sb(lnS, "lnst3", [128, 4, 6], F32)
    lnmv = sb(lnS, "lnmv3", [128, 8], F32)
    hb = sb(lnS, "hb3", [128, D], BF16)
    load_ln(2)
    d_out = dsem("d_out")
    for t in range(NT):
        layernorm(t, None, None)
        dma(SP, d_out, out_d[t * 128:(t + 1) * 128, :], RES[:, t, :], src=t_res[t])
    SP.wait([(d_out.sem, d_out.count)])
    if DEBUG:
        SP.wait([(d_dbg.sem, d_dbg.count)])
    barrier()
    lnS.close()
    s2.close()
    es.close()
    return nc


def kernel(x, mem, positions, w_in, w_gla_a2, b_gla_a, g_gla_norm, w_mix_out, ln1_g, ln1_b,
           w_mq, w_mk, w_mv, w_mo, ln2_g, ln2_b, w_route_group, b_route_group,
           w_route_expert, b_route_expert, w_exp_gate, w_exp_up, w_exp_down, ln3_g, ln3_b):
    f = lambda a: np.ascontiguousarray(np.asarray(a))
    x = f(x)[0][:SEQ]
    pos = f(positions)[0].astype(np.int32)[:SEQ]
    shared = {
        "consts": CONST_ARR, "consts1": CONST1_ARR,
        "w_in": _perm_w_in(f(w_in)[0]),
        "wa2b": np.ascontiguousarray(np.concatenate([f(w_gla_a2)[0], f(b_gla_a)[0][None, :]], axis=0)),
        "gnorm": f(g_gla_norm)[0][None, :].copy(),
        "w_mix": f(w_mix_out)[0], "w_mq": f(w_mq)[0], "w_mk": f(w_mk)[0], "w_mv": f(w_mv)[0], "w_mo": f(w_mo)[0],
        "memT": np.ascontiguousarray(f(mem)[0].reshape(256, KC, 128).transpose(2, 1, 0).reshape(128, KC * 256)),
        "ln": np.ascontiguousarray(np.stack([f(ln1_g)[0], f(ln1_b)[0], f(ln2_g)[0], f(ln2_b)[0], f(ln3_g)[0], f(ln3_b)[0]])),
        "wroute": np.ascontiguousarray(np.concatenate([f(w_route_group)[0], f(w_route_expert)[0]], axis=1)),
        "broute": np.ascontiguousarray(np.concatenate([f(b_route_group)[0], f(b_route_expert)[0].reshape(-1)])[None, :]),
        "w_eg": f(w_exp_gate)[0], "w_eu": f(w_exp_up)[0], "w_ed": f(w_exp_down)[0],
    }
    in_maps = []
    for c in range(NCORE):
        npad = (NCORE - 1 - c) * TOK
        xs = np.concatenate([np.zeros((npad, D), np.float32), x[0:(c + 1) * TOK]], axis=0)
        ps = np.concatenate([np.zeros((npad,), np.int32), pos[0:(c + 1) * TOK]], axis=0)
        xTp = np.ascontiguousarray(xs.reshape(NPRE + NT, 128, KC, 128).transpose(0, 3, 2, 1)).reshape(NPRE + NT, 128, D)
        m = dict(shared)
        m["xTp"] = xTp
        m["xown"] = np.ascontiguousarray(x[c * TOK:(c + 1) * TOK])
        m["posT"] = np.ascontiguousarray(ps.reshape(NPRE + NT, 128).T)
        in_maps.append(m)
    if "nc" not in _CACHE:
        _CACHE["nc"] = build_program()
    in_maps = [{k: v for k, v in m.items() if k in DECLARED} for m in in_maps]
    res = run_bass_kernel_spmd(_CACHE["nc"], in_maps, core_ids=list(range(NCORE)))
    if DEBUG:
        _CACHE["dbg"] = res.results
    out = np.concatenate([np.asarray(r["out"]) for r in res.results], axis=0).astype(np.float32)
    return out.reshape(1, SEQ, D)
```

```python
import math
from contextlib import ExitStack
import numpy as np
import concourse.bass as bass
import concourse.mybir as mybir
from concourse.bass_utils import run_bass_kernel_spmd

F32 = mybir.dt.float32
BF16 = mybir.dt.bfloat16
I32 = mybir.dt.int32
AF = mybir.ActivationFunctionType
ALU = mybir.AluOpType
AX = mybir.AxisListType

NCORE = 8
D = 2048
SEQ = 8192
TOK = SEQ // NCORE
NT = TOK // 128
NPRE = (NCORE - 1) * NT
KC = D // 128
EPS = 1e-5
ALPHA = 2.0 ** 0.25
NEXP = 32
CAP = 128
TWO_PI = 2.0 * math.pi
GAMMAS = [1.0 - 2.0 ** (-5.0 - h) for h in range(4)]

DEBUG = False
STOP = None


def configure(seq=8192, stop=None, debug=False):
    global SEQ, TOK, NT, NPRE, STOP, DEBUG
    SEQ = seq
    TOK = SEQ // NCORE
    NT = TOK // 128
    NPRE = (NCORE - 1) * NT
    STOP = stop
    DEBUG = debug
    _CACHE.clear()


_CACHE = {}
DECLARED = []


class Tile:
    def __init__(self, name="", excl=False):
        self.name = name
        self.w = []
        self.r = []
        self.excl = excl


class DSem:
    def __init__(self, nc, es, name):
        self.sem = es.enter_context(nc.semaphore(name))
        self.count = 0


class Eng:
    def __init__(self, nc, es, e, name):
        self.e = e
        self.sem = es.enter_context(nc.semaphore(name))
        self.n = 0
        self.seen = {}

    def wait(self, tks):
        best = {}
        for sem, val in tks:
            if val > best.get(sem, 0):
                best[sem] = val
        for sem, val in best.items():
            if self.seen.get(sem, 0) < val:
                self.e.wait_ge(sem, val)
                self.seen[sem] = val

    def tick(self, ins):
        self.n += 1
        ins.then_inc(self.sem, 1)
        return (self.sem, self.n)


def op(E, fn, reads=(), writes=()):
    tks = []
    for t in reads:
        tks += t.w
        if t.excl:
            tks += [k for k in t.r if k[0] is not E.sem]
    for t in writes:
        tks += t.w
        tks += t.r
    E.wait(tks)
    ins = fn()
    tk = E.tick(ins)
    for t in reads:
        t.r = [k for k in t.r if k[0] is not E.sem] + [tk]
    for t in writes:
        t.w = [tk]
        t.r = []
    return tk


def mmgroup(E, fns, reads=(), writes=()):
    tks = []
    for t in reads:
        tks += t.w
        if t.excl:
            tks += [k for k in t.r if k[0] is not E.sem]
    for t in writes:
        tks += t.w
        tks += t.r
    E.wait(tks)
    ins = None
    for f in fns:
        ins = f()
    tk = E.tick(ins)
    for t in reads:
        t.r = [k for k in t.r if k[0] is not E.sem] + [tk]
    for t in writes:
        t.w = [tk]
        t.r = []
    return tk


def dma(Q, ds, out, in_, dst=None, src=None):
    tks = []
    dsts = [] if dst is None else (list(dst) if isinstance(dst, (list, tuple)) else [dst])
    for d in dsts:
        tks += d.w + d.r
    if src is not None:
        tks += src.w
    Q.wait(tks)
    ds.count += 16
    Q.e.dma_start(out=out, in_=in_).then_inc(ds.sem, 16)
    tk = (ds.sem, ds.count)
    for d in dsts:
        d.w = [tk]
        d.r = []
    if src is not None:
        src.r = [k for k in src.r if k[0] is not ds.sem] + [tk]
    return tk


def _consts():
    c = {}
    i = np.arange(128)
    DT = np.zeros((4, 128, 128), np.float64)
    for h, g in enumerate(GAMMAS):
        rel = i[None, :] - i[:, None]
        DT[h] = np.where(rel >= 0, np.exp(np.log(g) * np.maximum(rel, 0)), 0.0) / 16.0
    c["DT"] = DT.transpose(1, 0, 2).reshape(128, 512)
    c["CAUS"] = (i[None, :] >= i[:, None]).astype(np.float64)
    c["DQ"] = np.tile(np.stack([np.exp(np.log(g) * (i + 1.0)) for g in GAMMAS]).reshape(1, 512), (128, 1))
    dk = np.zeros((128, 8))
    for h, g in enumerate(GAMMAS):
        dk[:, 2 * h] = np.exp(np.log(g) * (127.0 - i)) / 16.0
    c["DK"] = dk
    half = np.arange(128, dtype=np.float32)
    invf = (np.float32(10000.0) ** (-half / np.float32(128.0))).astype(np.float32)
    c["INVF"] = np.tile(invf.reshape(1, 128), (128, 1))
    c["UT"] = -(i[:, None] <= i[None, :]).astype(np.float64) / 16.0
    c["UT2"] = -(i[:, None] > i[None, :]).astype(np.float64) / 16.0
    c["NCOL"] = np.full((128, 1), -1.0 / 16.0)
    c["IOTA"] = np.tile(i.reshape(1, 128).astype(np.float64), (128, 1))
    c["TRIS"] = (i[:, None] < i[None, :]).astype(np.float64)
    c["ONES"] = np.ones((128, 128))
    pers = ["IOTA", "TRIS", "ONES"]
    offs = {}
    cols1, cols2 = [], []
    o = 0
    for k, v in c.items():
        if k in pers:
            continue
        offs[k] = (1, o, v.shape[1])
        o += v.shape[1]
        cols1.append(v.astype(np.float32))
    o = 0
    for k in pers:
        v = c[k]
        offs[k] = (0, o, v.shape[1])
        o += v.shape[1]
        cols2.append(v.astype(np.float32))
    return (np.ascontiguousarray(np.concatenate(cols2, axis=1)), np.ascontiguousarray(np.concatenate(cols1, axis=1)), offs)


CONST_ARR, CONST1_ARR, COFF = _consts()

A_RET = [(h * 512, 512) for h in range(4)]
A_GLA = [(2048 + h * 384, 384) for h in range(4)]
A_LR = (3584, 16)
A_W = 3600
B_RET = [(3600 + h * 512, 512) for h in range(4)]
B_GLA = [(5648 + h * 384, 384) for h in range(4)]


def _perm_w_in(w_in):
    rq, rk, rv, rg, gq, gk, gv, gg, glr = np.split(w_in, np.cumsum([1024, 1024, 1024, 1024, 512, 512, 1024, 1024])[:8], axis=1)
    cols = []
    for h in range(4):
        cols += [rk[:, h * 256:(h + 1) * 256], rv[:, h * 256:(h + 1) * 256]]
    for h in range(4):
        cols += [gk[:, h * 128:(h + 1) * 128], gv[:, h * 256:(h + 1) * 256]]
    cols += [glr]
    for h in range(4):
        cols += [rq[:, h * 256:(h + 1) * 256], rg[:, h * 256:(h + 1) * 256]]
    for h in range(4):
        cols += [gq[:, h * 128:(h + 1) * 128], gg[:, h * 256:(h + 1) * 256]]
    return np.ascontiguousarray(np.concatenate(cols, axis=1))


def build_program():
    nc = bass.Bass("TRN2", target_bir_lowering=False)
    es = ExitStack()

    del DECLARED[:]
    need = {"1b": 0, "2": 1, "3": 2, None: 3}.get(STOP, 0)
    lvl = {"w_mix": 1, "ln": 1, "xown": 1, "w_mq": 2, "w_mk": 2, "w_mv": 2, "w_mo": 2, "memT": 2,
           "wroute": 3, "broute": 3, "w_eg": 3, "w_eu": 3, "w_ed": 3}

    def din(name, shape, dt=F32):
        if lvl.get(name, 0) > need:
            return None
        DECLARED.append(name)
        return nc.dram_tensor(name, list(shape), dt, kind="ExternalInput").ap()

    xTp = din("xTp", [NPRE + NT, 128, D])
    xown = din("xown", [TOK, D])
    posT = din("posT", [128, NPRE + NT], I32)
    consts_d = din("consts", list(CONST_ARR.shape))
    consts1_d = din("consts1", list(CONST1_ARR.shape))
    w_in = din("w_in", [D, 7184])
    wa2b_d = din("wa2b", [17, 512])
    gnorm_d = din("gnorm", [1, 256])
    w_mix = din("w_mix", [D, D])
    w_mq = din("w_mq", [D, D])
    w_mk = din("w_mk", [D, D])
    w_mv = din("w_mv", [D, D])
    w_mo = din("w_mo", [D, D])
    memT_d = din("memT", [128, KC * 256])
    ln_d = din("ln", [6, D])
    wroute_d = din("wroute", [D, 36])
    broute_d = din("broute", [1, 36])
    w_eg = din("w_eg", [NEXP, D, 512])
    w_eu = din("w_eu", [NEXP, D, 512])
    w_ed = din("w_ed", [NEXP, 512, D])
    out_d = nc.dram_tensor("out", [TOK, D], F32, kind="ExternalOutput").ap()
    if DEBUG:
        dbg_mixT = nc.dram_tensor("dbg_mixT", [128, KC * TOK], BF16, kind="ExternalOutput").ap()
        dbg_h1 = nc.dram_tensor("dbg_h1", [TOK, D], F32, kind="ExternalOutput").ap()
        dbg_h2 = nc.dram_tensor("dbg_h2", [TOK, D], F32, kind="ExternalOutput").ap()

    PE = Eng(nc, es, nc.tensor, "s_pe")
    DVE = Eng(nc, es, nc.vector, "s_dve")
    ACT = Eng(nc, es, nc.scalar, "s_act")
    POOL = Eng(nc, es, nc.gpsimd, "s_pool")
    SP = Eng(nc, es, nc.sync, "s_sp")
    ENGS = [PE, DVE, ACT, POOL, SP]
    all_dsems = []

    def dsem(name):
        d = DSem(nc, es, name)
        all_dsems.append(d)
        return d

    def barrier():
        tks = [(E.sem, E.n) for E in ENGS if E.n > 0] + [(d.sem, d.count) for d in all_dsems if d.count > 0]
        for E in ENGS:
            E.wait(tks)

    class Arena:
        def __init__(self, base, limit):
            self.base, self.top, self.limit = base, base, limit

    class Scope:
        def __init__(self, arena):
            self.arena = arena
            self.mark = arena.top

        def close(self):
            self.arena.top = self.mark

    big_holder = []

    d_dbg = dsem("d_dbg") if DEBUG else None

    def finish():
        if DEBUG:
            SP.wait([(d_dbg.sem, d_dbg.count)])
        barrier()
        es.close()
        return nc

    def sb(stack, name, shape, dt):
        if not isinstance(stack, Scope):
            return stack.enter_context(nc.sbuf_tensor(name, list(shape), dt))
        ar = stack.arena
        esz = 2 if dt == BF16 else 4
        n = 1
        for d_ in shape[1:]:
            n *= d_
        nbytes = (n * esz + 63) // 64 * 64
        off = ar.top
        assert off + nbytes <= ar.limit, (name, off, nbytes, ar.limit)
        ar.top = off + nbytes
        v = big_holder[0][0:shape[0], off // 4:(off + n * esz + 3) // 4]
        if dt != F32:
            v = v.bitcast(dt)
            v = v[:, 0:n]
        if len(shape) == 3:
            v = v.rearrange("p (a b) -> p a b", a=shape[1])
        elif len(shape) == 4:
            v = v.rearrange("p (a b c) -> p a b c", a=shape[1], b=shape[2])
        return v

    CT = sb(es, "CT", CONST_ARR.shape, F32)
    CTb = sb(es, "CTb", [128, 128 * 3], BF16)
    ident = sb(es, "ident", [128, 128], BF16)
    posf = sb(es, "posf", [128, NPRE + NT], F32)
    posi = sb(es, "posi", [128, NPRE + NT], I32)
    LO_1A = 115200
    LO_AFTER = 114688
    bigw = (nc.sbuf_bytes_remaining - 256) // 64 * 16
    big_holder.append(es.enter_context(nc.sbuf_tensor("big", [128, bigw], F32)))
    lo = Arena(0, LO_1A)
    hi = Arena(LO_1A, bigw * 4)
    s1 = Scope(hi)
    CT1 = sb(s1, "CT1", CONST1_ARR.shape, F32)
    wa2b = sb(s1, "wa2bs", [17, 512], BF16)
    gnb = sb(s1, "gnb", [128, 256], F32)
    Rst = sb(s1, "Rst", [128, 4, 512], F32)
    Rb = sb(s1, "Rb", [128, 4, 512], BF16)
    Sst = sb(s1, "Sst", [128, 4, 256], F32)
    Sb = sb(s1, "Sb", [128, 4, 256], BF16)
    NSLOT = 3
    slot_t = [Tile(f"slot{i}") for i in range(NSLOT)]
    slot_ds = [dsem(f"d_slot{i}") for i in range(NSLOT)]

    Fp = [es.enter_context(nc.psum_tensor(f"F{i}", [128, 512], F32)) for i in range(6)]
    Ft = [Tile(f"F{i}", excl=True) for i in range(6)]
    Tp = [es.enter_context(nc.psum_tensor(f"T{i}", [128, 1024], BF16)) for i in range(2)]
    Tt = [Tile(f"T{i}", excl=True) for i in range(2)]

    t_const = Tile("const")
    t_ident = Tile("ident")
    t_pos = Tile("pos")
    t_R = [Tile(f"R{h}") for h in range(4)]
    t_Rb = [Tile(f"Rb{h}") for h in range(4)]
    t_S = [Tile(f"S{h}") for h in range(4)]
    t_Sb = [Tile(f"Sb{h}") for h in range(4)]
    t_A = Tile("ACT_A")
    t_B = Tile("ACT_B")
    t_Bt = [Tile(f"ACT_B{t}") for t in range(NT)]
    t_At = [Tile(f"ACT_A{t}") for t in range(NT)]

    def C(name, lo=0, hi=None):
        which, o, w = COFF[name]
        hi = w if hi is None else hi
        return (CT1 if which else CT)[:, o + lo:o + hi]

    d_c = dsem("d_const")
    dma(SP, d_c, CT[:], consts_d)
    dma(SP, d_c, CT1[:], consts1_d)
    dma(SP, d_c, posi[:], posT)
    dma(SP, d_c, gnb[:], gnorm_d.partition_broadcast(128))
    d_c2 = dsem("d_const2")
    dma(POOL, d_c2, wa2b[:], wa2b_d)
    t_const.w = [(d_c.sem, d_c.count), (d_c2.sem, d_c2.count)]
    op(POOL, lambda: nc.gpsimd.memset(ident[:], 0.0), writes=[t_ident])
    op(POOL, lambda: nc.gpsimd.affine_select(out=ident[:], in_=ident[:], pattern=[[-1, 128]], compare_op=ALU.not_equal,
                                              fill=1.0, base=0, channel_multiplier=1), reads=[t_ident], writes=[t_ident])
    op(DVE, lambda: nc.vector.tensor_copy(out=posf[:], in_=posi[:]), reads=[t_const], writes=[t_pos])
    t_ctb = Tile("ctb")
    op(DVE, lambda: nc.vector.tensor_copy(out=CTb[:, 0:128], in_=C("ONES")), reads=[t_const], writes=[t_ctb])
    op(DVE, lambda: nc.vector.tensor_copy(out=CTb[:, 128:256], in_=C("TRIS")), reads=[t_const], writes=[t_ctb])
    for h in range(4):
        op(DVE, lambda h=h: nc.vector.memset(Rst[:, h, :], 0.0), writes=[t_R[h]])
        op(DVE, lambda h=h: nc.vector.memset(Sst[:, h, :], 0.0), writes=[t_S[h]])
        op(POOL, lambda h=h: nc.gpsimd.memset(Rb[:, h, :], 0.0), writes=[t_Rb[h]])
        op(POOL, lambda h=h: nc.gpsimd.memset(Sb[:, h, :], 0.0), writes=[t_Sb[h]])

    csb = [sb(s1, f"cs{i}", [128, 256], F32) for i in range(2)]
    t_csb = [Tile(f"cs{i}") for i in range(2)]
    tr_a = sb(s1, "tr_a", [128, 256], F32)
    tr_b = sb(s1, "tr_b", [128, 256], F32)
    tr_i = sb(s1, "tr_i", [128, 256], I32)
    t_tr = Tile("tr")
    rotA = sb(s1, "rotA", [128, 256], F32)
    rotB = sb(s1, "rotB", [128, 256], F32)
    t_rot = Tile("rot")
    kbuf = [sb(s1, f"kbuf{i}", [128, 256], BF16) for i in range(2)]
    t_k = [Tile(f"k{i}") for i in range(2)]
    gkb = [sb(s1, f"gkb{i}", [128, 128], BF16) for i in range(2)]
    t_gk = [Tile(f"gk{i}") for i in range(2)]
    vbuf = [sb(s1, f"vbuf{i}", [128, 256], BF16) for i in range(2)]
    t_v = [Tile(f"v{i}") for i in range(2)]
    vhat = [sb(s1, f"vhat{i}", [128, 256], BF16) for i in range(2)]
    t_vh = [Tile(f"vh{i}") for i in range(2)]
    glr_b = sb(s1, "glr_b", [128, 16], BF16)
    t_glr = Tile("glr")
    glrT = sb(s1, "glrT", [17, 128], BF16)
    t_glrT = Tile("glrT")
    Lg = sb(s1, "Lg", [128, 512], F32)
    t_L = Tile("L")
    etmp = sb(s1, "etmp", [128, 512], F32)
    t_et = Tile("etmp")
    gt_ekb = sb(s1, "gt_ekb", [128, 512], F32)
    gt_eb = sb(s1, "gt_eb", [128, 512], F32)
    gt_enb = sb(s1, "gt_enb", [128, 512], F32)
    gt_edec = sb(s1, "gt_edec", [128, 4], F32)
    t_gt = Tile("gt")
    op(POOL, lambda: nc.gpsimd.memset(glrT[:], 1.0), writes=[t_glrT])

    def gen_cossin(T, dst2d, dst_tile):
        G = nc.vector
        op(DVE, lambda: G.tensor_scalar(out=tr_a[:, 128:256], in0=C("INVF"), scalar1=posf[:, T:T + 1], scalar2=None,
                                         op0=ALU.mult), reads=[t_const, t_pos], writes=[t_tr])
        op(DVE, lambda: G.tensor_scalar(out=tr_a[:, 0:128], in0=tr_a[:, 128:256], scalar1=math.pi / 2, scalar2=None,
                                         op0=ALU.add), reads=[t_tr], writes=[t_tr])
        op(DVE, lambda: G.tensor_scalar(out=tr_i[:], in0=tr_a[:], scalar1=1.0 / TWO_PI, scalar2=None, op0=ALU.mult),
           reads=[t_tr], writes=[t_tr])
        op(DVE, lambda: G.tensor_copy(out=tr_b[:], in_=tr_i[:]), reads=[t_tr], writes=[t_tr])
        op(DVE, lambda: G.tensor_scalar(out=tr_b[:], in0=tr_b[:], scalar1=-TWO_PI, scalar2=None, op0=ALU.mult),
           reads=[t_tr], writes=[t_tr])
        op(DVE, lambda: G.tensor_tensor(out=tr_a[:], in0=tr_a[:], in1=tr_b[:], op=ALU.add), reads=[t_tr], writes=[t_tr])
        op(DVE, lambda: G.tensor_scalar(out=tr_b[:], in0=tr_a[:], scalar1=math.pi, scalar2=TWO_PI, op0=ALU.is_gt, op1=ALU.mult),
           reads=[t_tr], writes=[t_tr])
        op(DVE, lambda: G.tensor_tensor(out=tr_a[:], in0=tr_a[:], in1=tr_b[:], op=ALU.subtract), reads=[t_tr], writes=[t_tr])
        op(DVE, lambda: G.tensor_scalar(out=tr_b[:], in0=tr_a[:], scalar1=-math.pi, scalar2=TWO_PI, op0=ALU.is_lt, op1=ALU.mult),
           reads=[t_tr], writes=[t_tr])
        op(DVE, lambda: G.tensor_tensor(out=tr_a[:], in0=tr_a[:], in1=tr_b[:], op=ALU.add), reads=[t_tr], writes=[t_tr])
        op(ACT, lambda: nc.scalar.activation(out=dst2d, in_=tr_a[:], func=AF.Sin), reads=[t_tr], writes=[dst_tile])

    def rotary(ps_ap, out_ap, extra_reads, out_tile, cs2d, t_cs):
        p3 = ps_ap.rearrange("p (a b) -> p a b", a=2)
        cosb = cs2d[:, 0:128].unsqueeze(1).to_broadcast([128, 2, 128])
        sinb = cs2d[:, 128:256].unsqueeze(1).to_broadcast([128, 2, 128])
        op(DVE, lambda: nc.vector.tensor_tensor(out=rotA[:].rearrange("p (a b) -> p a b", a=2), in0=p3, in1=cosb, op=ALU.mult),
           reads=[t_cs] + extra_reads, writes=[t_rot])
        op(DVE, lambda: nc.vector.tensor_tensor(out=rotB[:].rearrange("p (a b) -> p a b", a=2), in0=p3, in1=sinb, op=ALU.mult),
           reads=[t_cs] + extra_reads, writes=[t_rot])
        op(DVE, lambda: nc.vector.tensor_tensor(out=out_ap[:, 0:128], in0=rotA[:, 0:128], in1=rotB[:, 128:256], op=ALU.subtract),
           reads=[t_rot], writes=[out_tile])
        op(DVE, lambda: nc.vector.tensor_tensor(out=out_ap[:, 128:256], in0=rotB[:, 0:128], in1=rotA[:, 128:256], op=ALU.add),
           reads=[t_rot], writes=[out_tile])

    def proj(xT_fn, x_tiles, w_ap_fn, w_tiles, ncols, acc_i):
        mmgroup(PE, [lambda k=k: nc.tensor.matmul(Fp[acc_i][:, 0:ncols], lhsT=xT_fn(k), rhs=w_ap_fn(k),
                                                  start=(k == 0), stop=(k == KC - 1)) for k in range(KC)],
                reads=list(x_tiles) + list(w_tiles), writes=[Ft[acc_i]])

    def gates_glr(xT_fn, x_tiles, wlr_fn, w_tiles):
        mmgroup(PE, [lambda k=k: nc.tensor.matmul(Fp[5][:, 0:16], lhsT=xT_fn(k), rhs=wlr_fn(k), start=(k == 0), stop=(k == KC - 1))
                     for k in range(KC)], reads=list(x_tiles) + list(w_tiles), writes=[Ft[5]])
        op(ACT, lambda: nc.scalar.copy(out=glr_b[:], in_=Fp[5][:, 0:16]), reads=[Ft[5]], writes=[t_glr])

    def gates_z():
        op(PE, lambda: nc.tensor.transpose(Tp[1][0:16, 0:128], glr_b[:], ident[:]), reads=[t_glr, t_ident], writes=[Tt[1]])
        op(DVE, lambda: nc.vector.tensor_copy(out=glrT[0:16, :], in_=Tp[1][0:16, 0:128]), reads=[Tt[1]], writes=[t_glrT])
        op(PE, lambda: nc.tensor.matmul(Fp[5][:, :], lhsT=glrT[:, :], rhs=wa2b[:, :], start=True, stop=True),
           reads=[t_glrT, t_const], writes=[Ft[5]])
        op(ACT, lambda: nc.scalar.activation(out=etmp[:], in_=Fp[5][:, :], func=AF.Exp, scale=-1.0), reads=[Ft[5]], writes=[t_et])
        op(ACT, lambda: nc.scalar.activation(out=Lg[:], in_=etmp[:], func=AF.Ln, bias=1.0), reads=[t_et], writes=[t_L])

    def gates_L():
        op(PE, lambda: nc.tensor.matmul(Fp[5][:, :], lhsT=C("UT2"), rhs=Lg[:], start=True, stop=True),
           reads=[t_L, t_const], writes=[Ft[5]])
        op(ACT, lambda: nc.scalar.activation(out=gt_ekb[:], in_=Fp[5][:, :], func=AF.Exp), reads=[Ft[5]], writes=[t_gt])

    def gates_dec(own):
        mmgroup(PE, [lambda h=h: nc.tensor.matmul(Fp[5][:, h:h + 1], lhsT=Lg[:, h * 128:(h + 1) * 128], rhs=C("NCOL"),
                                                  start=True, stop=True) for h in range(4)],
                reads=[t_L, t_const], writes=[Ft[5]])
        op(ACT, lambda: nc.scalar.activation(out=gt_edec[:], in_=Fp[5][:, 0:4], func=AF.Exp), reads=[Ft[5]], writes=[t_gt])
        if own:
            op(PE, lambda: nc.tensor.matmul(Fp[5][:, :], lhsT=C("UT"), rhs=Lg[:], start=True, stop=True),
               reads=[t_L, t_const], writes=[Ft[5]])
            op(ACT, lambda: nc.scalar.activation(out=gt_eb[:], in_=Fp[5][:, :], func=AF.Exp), reads=[Ft[5]], writes=[t_gt])
            op(ACT, lambda: nc.scalar.activation(out=gt_enb[:], in_=Fp[5][:, :], func=AF.Exp, scale=-1.0), reads=[Ft[5]], writes=[t_gt])

    def gla_gates(T, xT_fn, x_tiles, wlr_fn, w_tiles, own):
        gates_glr(xT_fn, x_tiles, wlr_fn, w_tiles)
        gates_z()
        gates_L()
        gates_dec(own)

    def ret_state_update(h, k_ap, kt, vh_ap, vht):
        mmgroup(PE, [lambda c=c: nc.tensor.matmul(Fp[4][:, c * 256:(c + 1) * 256], lhsT=k_ap[:, c * 128:(c + 1) * 128], rhs=vh_ap,
                                                  start=True, stop=True) for c in range(2)],
                reads=[kt, vht], writes=[Ft[4]])
        op(DVE, lambda: nc.vector.scalar_tensor_tensor(out=Rst[:, h, :], in0=Rst[:, h, :], scalar=float(GAMMAS[h] ** 128),
                                                       in1=Fp[4][:, :], op0=ALU.mult, op1=ALU.add),
           reads=[Ft[4], t_R[h]], writes=[t_R[h]])

    def gla_state_update(h, khat_ap, kt, v_ap, vt):
        op(PE, lambda: nc.tensor.matmul(Fp[4][:, 0:256], lhsT=khat_ap, rhs=v_ap, start=True, stop=True),
           reads=[kt, vt], writes=[Ft[4]])
        op(DVE, lambda: nc.vector.scalar_tensor_tensor(out=Sst[:, h, :], in0=Sst[:, h, :], scalar=gt_edec[:, h:h + 1],
                                                       in1=Fp[4][:, 0:256], op0=ALU.mult, op1=ALU.add),
           reads=[Ft[4], t_S[h], t_gt], writes=[t_S[h]])

    if STOP == "s":
        return finish()
    s1a = Scope(hi)
    s1lo = Scope(lo)
    WA = sb(s1lo, "WA", [128, KC, A_W], BF16)
    t_WA = Tile("WA")
    d_wa = dsem("d_wa")
    for k0 in range(0, KC, 4):
        dma(POOL, d_wa, WA[:, k0:k0 + 4, :], w_in[k0 * 128:(k0 + 4) * 128, 0:A_W].rearrange("(k p) n -> p k n", p=128))
    t_WA.w = [(d_wa.sem, d_wa.count)]
    xt_buf = [sb(s1a, f"xt{i}", [128, KC, 128], BF16) for i in range(2)]
    t_xt = [Tile(f"xt{i}") for i in range(2)]
    d_xt = [dsem(f"d_xt{i}") for i in range(2)]

    def load_xt(T):
        i = T % 2
        dma(POOL, d_xt[i], xt_buf[i][:], xTp[T].rearrange("p (k t) -> p k t", k=KC), dst=t_xt[i])

    load_xt(0)
    gen_cossin(0, csb[0][:], t_csb[0])
    for T in range(NPRE):
        if T + 1 < NPRE:
            load_xt(T + 1)
            gen_cossin(T + 1, csb[(T + 1) % 2][:], t_csb[(T + 1) % 2])
        xb = xt_buf[T % 2]
        xt_ = t_xt[T % 2]
        xT_fn = lambda k, xb=xb: xb[:, k, :]
        cs2d, t_cs = csb[T % 2], t_csb[T % 2]

        def ret_proj(h):
            c0, n = A_RET[h]
            ai = h % 2
            proj(xT_fn, [xt_], lambda k, c0=c0, n=n: WA[:, k, c0:c0 + n], [t_WA], n, ai)
            rotary(Fp[ai][:, 0:256], kbuf[ai], [Ft[ai]], t_k[ai], cs2d, t_cs)
            op(DVE, lambda ai=ai, h=h: nc.vector.tensor_scalar(out=vhat[ai][:], in0=Fp[ai][:, 256:512], scalar1=C("DK", 2 * h, 2 * h + 1),
                                                               scalar2=None, op0=ALU.mult), reads=[Ft[ai], t_const], writes=[t_vh[ai]])

        def ret_state(h):
            ai = h % 2
            ret_state_update(h, kbuf[ai], t_k[ai], vhat[ai][:], t_vh[ai])

        def gla_proj(h):
            c0, n = A_GLA[h]
            ai = 2 + h % 2
            bi = h % 2
            proj(xT_fn, [xt_], lambda k, c0=c0, n=n: WA[:, k, c0:c0 + n], [t_WA], n, ai)
            op(DVE, lambda ai=ai, bi=bi, h=h: nc.vector.tensor_tensor(out=gkb[bi][:], in0=Fp[ai][:, 0:128],
                                                                      in1=gt_ekb[:, h * 128:(h + 1) * 128], op=ALU.mult),
               reads=[Ft[ai], t_gt], writes=[t_gk[bi]])
            op(ACT, lambda ai=ai, bi=bi: nc.scalar.copy(out=vbuf[bi][:], in_=Fp[ai][:, 128:384]), reads=[Ft[ai]], writes=[t_v[bi]])

        def gla_state(h):
            bi = h % 2
            gla_state_update(h, gkb[bi][:], t_gk[bi], vbuf[bi][:], t_v[bi])

        wlr_fn = lambda k: WA[:, k, A_LR[0]:A_LR[0] + 16]
        gates_glr(xT_fn, [xt_], wlr_fn, [t_WA])
        ret_proj(0)
        gates_z()
        ret_proj(1)
        ret_state(0)
        gates_L()
        ret_proj(2)
        ret_state(1)
        gates_dec(False)
        ret_proj(3)
        ret_state(2)
        gla_proj(0)
        ret_state(3)
        gla_proj(1)
        gla_state(0)
        gla_proj(2)
        gla_state(1)
        gla_proj(3)
        gla_state(2)
        gla_state(3)
    for h in range(4):
        op(ACT, lambda h=h: nc.scalar.copy(out=Rb[:, h, :], in_=Rst[:, h, :]), reads=[t_R[h]], writes=[t_Rb[h]])
        op(ACT, lambda h=h: nc.scalar.copy(out=Sb[:, h, :], in_=Sst[:, h, :]), reads=[t_S[h]], writes=[t_Sb[h]])
    barrier()
    if STOP == "1a":
        return finish()
    s1a.close()
    s1lo.close()
    plo = Scope(lo)
    ACT_A = sb(plo, "ACT_A", [128, KC, TOK], BF16)
    ACT_B = sb(plo, "ACT_B", [128, KC, TOK], BF16)
    slots = [sb(plo, f"wslot{i}", [128, KC * 512], BF16) for i in range(NSLOT)]
    assert lo.top <= LO_AFTER

    blocks = []

    def wblock(ap2d, kch, ncols):
        blocks.append((ap2d, kch, ncols))
        return len(blocks) - 1

    blk_retA = [wblock(w_in[:, c0:c0 + n], KC, n) for (c0, n) in A_RET]
    blk_retB = [wblock(w_in[:, c0:c0 + n], KC, n) for (c0, n) in B_RET]
    blk_glaA = [wblock(w_in[:, c0:c0 + n], KC, n) for (c0, n) in A_GLA]
    blk_glaB = [wblock(w_in[:, c0:c0 + n], KC, n) for (c0, n) in B_GLA]
    order = []
    for h in range(4):
        order += [blk_retA[h], blk_retB[h]]
    for h in range(4):
        order += [blk_glaA[h], blk_glaB[h]]
    if need >= 1:
        blk_mix = [wblock(w_mix[:, j * 512:(j + 1) * 512], KC, 512) for j in range(4)]
        order += blk_mix
    if need >= 2:
        blk_mk = [wblock(w_mk[:, j * 512:(j + 1) * 512], KC, 512) for j in range(4)]
        blk_mv = [wblock(w_mv[:, j * 512:(j + 1) * 512], KC, 512) for j in range(4)]
        blk_mq = [wblock(w_mq[:, j * 512:(j + 1) * 512], KC, 512) for j in range(4)]
        blk_mo = [wblock(w_mo[:, j * 512:(j + 1) * 512], KC, 512) for j in range(4)]
        order += blk_mk + blk_mv + blk_mq + blk_mo
    blk_e = []
    for e in range(NEXP if need >= 3 else 0):
        g_ = wblock(w_eg[e], KC, 512)
        u_ = wblock(w_eu[e], KC, 512)
        d_ = wblock(w_ed[e], 4, 2048)
        blk_e.append((g_, u_, d_))
        order += [g_, u_, d_]
    pos_in_order = {b: i for i, b in enumerate(order)}
    issued = [0]
    blk_slot = {}

    def issue_upto(i):
        while issued[0] <= min(i, len(order) - 1):
            j = issued[0]
            b = order[j]
            ap2d, kch, ncols = blocks[b]
            s = j % NSLOT
            dst = slots[s][:, 0:kch * ncols].rearrange("p (k n) -> p k n", k=kch)
            dma(POOL, slot_ds[s], dst, ap2d.rearrange("(k p) n -> p k n", p=128), dst=slot_t[s])
            blk_slot[b] = s
            issued[0] += 1

    def wget(b, pf=2):
        i = pos_in_order[b]
        issue_upto(i + pf)
        s = blk_slot[b]
        ap2d, kch, ncols = blocks[b]
        v = slots[s][:, 0:kch * ncols].rearrange("p (k n) -> p k n", k=kch)
        return v, slot_t[s]

    d_xo = dsem("d_xo")
    for t in range(NT):
        dma(POOL, d_xo, ACT_A[:, :, t * 128:(t + 1) * 128], xTp[NPRE + t].rearrange("p (k t) -> p k t", k=KC))
    t_A.w = [(d_xo.sem, d_xo.count)]

    own = Scope(hi)
    cs_own = sb(own, "cs_own", [128, NT, 256], F32)
    t_cso = Tile("cs_own")
    g_ekb = sb(own, "g_ekb", [128, NT, 512], BF16)
    g_eb = sb(own, "g_eb", [128, NT, 512], BF16)
    g_enb = sb(own, "g_enb", [128, NT, 512], BF16)
    g_edec = sb(own, "g_edec", [128, NT, 4], F32)
    t_gown = Tile("gown")
    qbuf = sb(own, "qbuf", [128, 256], BF16)
    t_q = Tile("q")
    sgbuf = sb(own, "sgbuf", [128, 256], BF16)
    t_sg = Tile("sg")
    qkT = sb(own, "qkT", [128, 6, 128], BF16)
    t_qkT = Tile("qkT")
    sTb = sb(own, "sTb", [128, 128], BF16)
    t_sT = Tile("sT")
    stat = sb(own, "stat", [128, 8], F32)
    t_stat = Tile("stat")
    bst = sb(own, "bst", [128, 6], F32)
    ynorm = sb(own, "ynorm", [128, 256], F32)
    t_yn = Tile("yn")
    mixb = sb(own, "mixb", [128, 256], BF16)
    t_mixb = Tile("mixb")
    junk = ynorm
    t_junk = t_yn

    def xTown(t):
        return lambda k: ACT_A[:, k, t * 128:(t + 1) * 128]

    for t in range(NT):
        gen_cossin(NPRE + t, cs_own[:, t, :], t_cso)

    def finish_head_tile(hcol, t, is_ret, o_acc):
        o_ps = Fp[o_acc][:, 0:256]
        if is_ret:
            op(DVE, lambda: nc.vector.bn_stats(out=bst[:], in_=o_ps), reads=[Ft[o_acc]], writes=[t_stat])
            op(DVE, lambda: nc.vector.bn_aggr(out=stat[:, 0:2], in_=bst[:]), reads=[t_stat], writes=[t_stat])
            op(DVE, lambda: nc.vector.tensor_scalar(out=stat[:, 2:3], in0=stat[:, 1:2], scalar1=EPS, scalar2=None, op0=ALU.add),
               reads=[t_stat], writes=[t_stat])
        else:
            op(DVE, lambda: nc.vector.memset(stat[:, 0:1], 0.0), writes=[t_stat])
            op(ACT, lambda: nc.scalar.activation(out=junk[:], in_=o_ps, func=AF.Square, accum_out=stat[:, 0:1]),
               reads=[Ft[o_acc]], writes=[t_junk, t_stat])
            op(DVE, lambda: nc.vector.tensor_scalar(out=stat[:, 2:3], in0=stat[:, 0:1], scalar1=1.0 / 256.0, scalar2=EPS,
                                                    op0=ALU.mult, op1=ALU.add), reads=[t_stat], writes=[t_stat])
        op(ACT, lambda: nc.scalar.activation(out=stat[:, 3:4], in_=stat[:, 2:3], func=AF.Sqrt), reads=[t_stat], writes=[t_stat])
        op(DVE, lambda: nc.vector.reciprocal(out=stat[:, 4:5], in_=stat[:, 3:4]), reads=[t_stat], writes=[t_stat])
        if is_ret:
            op(DVE, lambda: nc.vector.tensor_scalar(out=ynorm[:], in0=o_ps, scalar1=stat[:, 0:1], scalar2=stat[:, 4:5],
                                                    op0=ALU.subtract, op1=ALU.mult), reads=[Ft[o_acc], t_stat], writes=[t_yn])
            op(DVE, lambda: nc.vector.tensor_tensor(out=mixb[:], in0=ynorm[:], in1=sgbuf[:], op=ALU.mult),
               reads=[t_yn, t_sg], writes=[t_mixb])
        else:
            op(DVE, lambda: nc.vector.scalar_tensor_tensor(out=mixb[:], in0=o_ps, scalar=stat[:, 4:5], in1=sgbuf[:],
                                                           op0=ALU.mult, op1=ALU.mult), reads=[Ft[o_acc], t_stat, t_sg], writes=[t_mixb])
        mmgroup(PE, [lambda c=c: nc.tensor.transpose(Tp[1][:, c * 128:(c + 1) * 128], mixb[:, c * 128:(c + 1) * 128], ident[:])
                     for c in range(2)], reads=[t_mixb, t_ident], writes=[Tt[1]])
        kk = hcol * 2
        op(ACT, lambda: nc.scalar.copy(out=ACT_B[:, kk:kk + 2, t * 128:(t + 1) * 128],
                                       in_=Tp[1][:, 0:256].rearrange("p (c t) -> p c t", c=2)), reads=[Tt[1]], writes=[t_Bt[t]])

    for h in range(4):
        WAv, WAt = wget(blk_retA[h], 2)
        WBv, WBt = wget(blk_retB[h], 1)
        for t in range(NT):
            pA = lambda tt: proj(xTown(tt), [t_A], lambda k: WAv[:, k, :], [WAt], 512, 0)
            pB = lambda tt: proj(xTown(tt), [t_A], lambda k: WBv[:, k, :], [WBt], 512, 1)
            if t == 0:
                pA(0)
                pB(0)
            rotary(Fp[0][:, 0:256], kbuf[0], [Ft[0]], t_k[0], cs_own[:, t, :], t_cso)
            rotary(Fp[1][:, 0:256], qbuf, [Ft[1]], t_q, cs_own[:, t, :], t_cso)
            op(ACT, lambda: nc.scalar.copy(out=vbuf[0][:], in_=Fp[0][:, 256:512]), reads=[Ft[0]], writes=[t_v[0]])
            op(DVE, lambda h=h: nc.vector.tensor_scalar(out=vhat[0][:], in0=Fp[0][:, 256:512], scalar1=C("DK", 2 * h, 2 * h + 1),
                                                        scalar2=None, op0=ALU.mult), reads=[Ft[0], t_const], writes=[t_vh[0]])
            op(ACT, lambda: nc.scalar.activation(out=sgbuf[:], in_=Fp[1][:, 256:512], func=AF.Silu), reads=[Ft[1]], writes=[t_sg])
            mmgroup(PE, [lambda c=c: nc.tensor.transpose(Tp[0][:, c * 128:(c + 1) * 128], qbuf[:, c * 128:(c + 1) * 128], ident[:])
                         for c in range(2)] +
                        [lambda c=c: nc.tensor.transpose(Tp[0][:, (2 + c) * 128:(3 + c) * 128], kbuf[0][:, c * 128:(c + 1) * 128], ident[:])
                         for c in range(2)], reads=[t_q, t_k[0], t_ident], writes=[Tt[0]])
            if t + 1 < NT:
                pA(t + 1)
            op(ACT, lambda: nc.scalar.copy(out=qkT[:, 0:4, :], in_=Tp[0][:, 0:512].rearrange("p (c t) -> p c t", c=4)),
               reads=[Tt[0]], writes=[t_qkT])
            op(DVE, lambda h=h: nc.vector.tensor_tensor(out=qkT[:, 4:6, :], in0=Tp[0][:, 0:256].rearrange("p (c t) -> p c t", c=2),
                                                        in1=C("DQ", h * 128, (h + 1) * 128).unsqueeze(1).to_broadcast([128, 2, 128]),
                                                        op=ALU.mult), reads=[Tt[0], t_const], writes=[t_qkT])
            mmgroup(PE, [lambda c=c: nc.tensor.matmul(Fp[2][:, 0:128], lhsT=qkT[:, 2 + c, :], rhs=qkT[:, c, :], start=(c == 0), stop=(c == 1))
                         for c in range(2)], reads=[t_qkT], writes=[Ft[2]])
            if t + 1 < NT:
                pB(t + 1)
            op(DVE, lambda h=h: nc.vector.tensor_tensor(out=sTb[:], in0=Fp[2][:, 0:128], in1=C("DT", h * 128, (h + 1) * 128), op=ALU.mult),
               reads=[Ft[2], t_const], writes=[t_sT])
            mmgroup(PE, [lambda: nc.tensor.matmul(Fp[3][:, 0:256], lhsT=sTb[:], rhs=vbuf[0][:], start=True, stop=False)] +
                        [lambda c=c, h=h: nc.tensor.matmul(Fp[3][:, 0:256], lhsT=qkT[:, 4 + c, :], rhs=Rb[:, h, c * 256:(c + 1) * 256],
                                                           start=False, stop=(c == 1)) for c in range(2)],
                    reads=[t_sT, t_v[0], t_qkT, t_Rb[h]], writes=[Ft[3]])
            ret_state_update(h, kbuf[0], t_k[0], vhat[0][:], t_vh[0])
            op(ACT, lambda h=h: nc.scalar.copy(out=Rb[:, h, :], in_=Rst[:, h, :]), reads=[t_R[h]], writes=[t_Rb[h]])
            finish_head_tile(h, t, True, 3)

    WAv0, WAt0 = None, None
    d_lr = dsem("d_lr")
    wlr = sb(own, "wlr", [128, KC, 16], BF16)
    t_wlr = Tile("wlr")
    dma(POOL, d_lr, wlr[:], w_in[:, A_LR[0]:A_LR[0] + 16].rearrange("(k p) n -> p k n", p=128), dst=t_wlr)
    for t in range(NT):
        gla_gates(NPRE + t, xTown(t), [t_A], lambda k: wlr[:, k, :], [t_wlr], own=True)
        op(DVE, lambda t=t: nc.vector.tensor_copy(out=g_ekb[:, t, :], in_=gt_ekb[:]), reads=[t_gt], writes=[t_gown])
        op(DVE, lambda t=t: nc.vector.tensor_copy(out=g_eb[:, t, :], in_=gt_eb[:]), reads=[t_gt], writes=[t_gown])
        op(DVE, lambda t=t: nc.vector.tensor_copy(out=g_enb[:, t, :], in_=gt_enb[:]), reads=[t_gt], writes=[t_gown])
        op(DVE, lambda t=t: nc.vector.tensor_copy(out=g_edec[:, t, :], in_=gt_edec[:]), reads=[t_gt], writes=[t_gown])

    for h in range(4):
        WAv, WAt = wget(blk_glaA[h], 2)
        WBv, WBt = wget(blk_glaB[h], 1)
        hs = slice(h * 128, (h + 1) * 128)
        for t in range(NT):
            pA = lambda tt: proj(xTown(tt), [t_A], lambda k: WAv[:, k, :], [WAt], 384, 0)
            pB = lambda tt: proj(xTown(tt), [t_A], lambda k: WBv[:, k, :], [WBt], 384, 1)
            if t == 0:
                pA(0)
                pB(0)
            op(DVE, lambda t=t: nc.vector.tensor_tensor(out=kbuf[0][:, 0:128], in0=Fp[0][:, 0:128], in1=g_enb[:, t, hs], op=ALU.mult),
               reads=[Ft[0], t_gown], writes=[t_k[0]])
            op(DVE, lambda t=t: nc.vector.tensor_tensor(out=kbuf[1][:, 0:128], in0=Fp[0][:, 0:128], in1=g_ekb[:, t, hs], op=ALU.mult),
               reads=[Ft[0], t_gown], writes=[t_k[1]])
            op(DVE, lambda t=t: nc.vector.scalar_tensor_tensor(out=qbuf[:, 0:128], in0=Fp[1][:, 0:128], scalar=128.0 ** -0.5,
                                                               in1=g_eb[:, t, hs], op0=ALU.mult, op1=ALU.mult),
               reads=[Ft[1], t_gown], writes=[t_q])
            op(ACT, lambda: nc.scalar.copy(out=vbuf[0][:], in_=Fp[0][:, 128:384]), reads=[Ft[0]], writes=[t_v[0]])
            op(ACT, lambda: nc.scalar.activation(out=ynorm[:], in_=Fp[1][:, 128:384], func=AF.Silu), reads=[Ft[1]], writes=[t_yn])
            op(DVE, lambda: nc.vector.tensor_tensor(out=sgbuf[:], in0=ynorm[:], in1=gnb[:], op=ALU.mult),
               reads=[t_yn, t_const], writes=[t_sg])
            mmgroup(PE, [lambda: nc.tensor.transpose(Tp[0][:, 0:128], qbuf[:, 0:128], ident[:]),
                         lambda: nc.tensor.transpose(Tp[0][:, 128:256], kbuf[0][:, 0:128], ident[:])],
                    reads=[t_q, t_k[0], t_ident], writes=[Tt[0]])
            if t + 1 < NT:
                pA(t + 1)
            op(ACT, lambda: nc.scalar.copy(out=qkT[:, 0:2, :], in_=Tp[0][:, 0:256].rearrange("p (c t) -> p c t", c=2)),
               reads=[Tt[0]], writes=[t_qkT])
            op(PE, lambda: nc.tensor.matmul(Fp[2][:, 0:128], lhsT=qkT[:, 1, :], rhs=qkT[:, 0, :], start=True, stop=True),
               reads=[t_qkT], writes=[Ft[2]])
            if t + 1 < NT:
                pB(t + 1)
            op(DVE, lambda: nc.vector.tensor_tensor(out=sTb[:], in0=Fp[2][:, 0:128], in1=C("CAUS"), op=ALU.mult),
               reads=[Ft[2], t_const], writes=[t_sT])
            mmgroup(PE, [lambda: nc.tensor.matmul(Fp[3][:, 0:256], lhsT=sTb[:], rhs=vbuf[0][:], start=True, stop=False),
                         lambda h=h: nc.tensor.matmul(Fp[3][:, 0:256], lhsT=qkT[:, 0, :], rhs=Sb[:, h, :], start=False, stop=True)],
                    reads=[t_sT, t_v[0], t_qkT, t_Sb[h]], writes=[Ft[3]])
            op(PE, lambda: nc.tensor.matmul(Fp[4][:, 0:256], lhsT=kbuf[1][:, 0:128], rhs=vbuf[0][:], start=True, stop=True),
               reads=[t_k[1], t_v[0]], writes=[Ft[4]])
            op(DVE, lambda h=h, t=t: nc.vector.scalar_tensor_tensor(out=Sst[:, h, :], in0=Sst[:, h, :], scalar=g_edec[:, t, h:h + 1],
                                                                    in1=Fp[4][:, 0:256], op0=ALU.mult, op1=ALU.add),
               reads=[Ft[4], t_S[h], t_gown], writes=[t_S[h]])
            op(ACT, lambda h=h: nc.scalar.copy(out=Sb[:, h, :], in_=Sst[:, h, :]), reads=[t_S[h]], writes=[t_Sb[h]])
            finish_head_tile(4 + h, t, False, 3)

    if DEBUG:
        for t in range(NT):
            t_B.w += t_Bt[t].w
        dma(SP, d_dbg, dbg_mixT, ACT_B[:].rearrange("p k t -> p (k t)"), src=t_B)
    barrier()
    own.close()
    s1.close()
    if STOP == "1b":
        return finish()

    assert hi.top == hi.base
    hi.base = hi.top = LO_AFTER
    s2 = Scope(hi)
    RES = sb(s2, "RES", [128, NT, D], F32)
    t_res = [Tile(f"res{t}") for t in range(NT)]
    t_ln = Tile("ln")
    d_ln = dsem("d_ln")
    d_x = dsem("d_x")
    for t in range(NT):
        dma(SP, d_x, RES[:, t, :], xown[t * 128:(t + 1) * 128, :])
    for t in range(NT):
        t_res[t].w = [(d_x.sem, d_x.count)]
    t_lnst = Tile("lnst")
    t_hb = Tile("hb")
    lnS = Scope(hi)
    lnt = sb(lnS, "lnt", [128, 2, D], F32)
    lnst = sb(lnS, "lnst", [128, 4, 6], F32)
    lnmv = sb(lnS, "lnmv", [128, 8], F32)
    hb = sb(lnS, "hb", [128, D], BF16)

    def load_ln(i):
        dma(SP, d_ln, lnt[:, 0, :], ln_d[2 * i:2 * i + 1, :].partition_broadcast(128), dst=t_ln)
        dma(SP, d_ln, lnt[:, 1, :], ln_d[2 * i + 1:2 * i + 2, :].partition_broadcast(128), dst=t_ln)

    def linear(AT, at_tiles, blks, epilogue, ntiles=NT):
        pend = None
        for j, b in enumerate(blks):
            Wv, Wt = wget(b)
            for t in range(ntiles):
                ai = (j * ntiles + t) % 2
                mmgroup(PE, [lambda k=k, t=t, ai=ai, Wv=Wv: nc.tensor.matmul(Fp[ai][:, :], lhsT=AT[:, k, t * 128:(t + 1) * 128], rhs=Wv[:, k, :],
                                                                             start=(k == 0), stop=(k == KC - 1)) for k in range(KC)],
                        reads=list(at_tiles(t)) + [Wt], writes=[Ft[ai]])
                if pend is not None:
                    epilogue(*pend)
                pend = (j, t, ai)
        if pend is not None:
            epilogue(*pend)

    def resid_epi(j, t, ai):
        op(DVE, lambda: nc.vector.scalar_tensor_tensor(out=RES[:, t, j * 512:(j + 1) * 512], in0=RES[:, t, j * 512:(j + 1) * 512],
                                                       scalar=ALPHA, in1=Fp[ai][:, :], op0=ALU.mult, op1=ALU.add),
           reads=[Ft[ai], t_res[t]], writes=[t_res[t]])

    def layernorm(t, dstT, dst_tiles, want_tok_bf16=None):
        for c in range(4):
            op(DVE, lambda c=c: nc.vector.bn_stats(out=lnst[:, c, :], in_=RES[:, t, c * 512:(c + 1) * 512]),
               reads=[t_res[t]], writes=[t_lnst])
        op(DVE, lambda: nc.vector.bn_aggr(out=lnmv[:, 0:2], in_=lnst[:].rearrange("p a b -> p (a b)")), reads=[t_lnst], writes=[t_lnst])
        op(DVE, lambda: nc.vector.tensor_scalar(out=lnmv[:, 2:3], in0=lnmv[:, 1:2], scalar1=EPS, scalar2=None, op0=ALU.add),
           reads=[t_lnst], writes=[t_lnst])
        op(ACT, lambda: nc.scalar.activation(out=lnmv[:, 3:4], in_=lnmv[:, 2:3], func=AF.Sqrt), reads=[t_lnst], writes=[t_lnst])
        op(DVE, lambda: nc.vector.reciprocal(out=lnmv[:, 4:5], in_=lnmv[:, 3:4]), reads=[t_lnst], writes=[t_lnst])
        op(DVE, lambda: nc.vector.tensor_scalar(out=RES[:, t, :], in0=RES[:, t, :], scalar1=lnmv[:, 0:1], scalar2=lnmv[:, 4:5],
                                                op0=ALU.subtract, op1=ALU.mult), reads=[t_res[t], t_lnst], writes=[t_res[t]])
        op(DVE, lambda: nc.vector.tensor_tensor(out=RES[:, t, :], in0=RES[:, t, :], in1=lnt[:, 0, :], op=ALU.mult),
           reads=[t_res[t], t_ln], writes=[t_res[t]])
        op(DVE, lambda: nc.vector.tensor_tensor(out=RES[:, t, :], in0=RES[:, t, :], in1=lnt[:, 1, :], op=ALU.add),
           reads=[t_res[t], t_ln], writes=[t_res[t]])
        if dstT is not None:
            hbt = hb[:] if want_tok_bf16 is None else want_tok_bf16
            hbt_tile = t_hb
            op(ACT, lambda: nc.scalar.copy(out=hbt, in_=RES[:, t, :]), reads=[t_res[t]], writes=[hbt_tile])
            for half in range(2):
                mmgroup(PE, [lambda c=c: nc.tensor.transpose(Tp[half][:, c * 128:(c + 1) * 128],
                                                             hbt[:, (half * 8 + c) * 128:(half * 8 + c + 1) * 128], ident[:])
                             for c in range(8)], reads=[hbt_tile, t_ident], writes=[Tt[half]])
                op(ACT if half == 0 else DVE,
                   (lambda half=half: nc.scalar.copy(out=dstT[:, half * 8:(half + 1) * 8, t * 128:(t + 1) * 128],
                                                     in_=Tp[half][:].rearrange("p (c t) -> p c t", c=8))) if half == 0 else
                   (lambda half=half: nc.vector.tensor_copy(out=dstT[:, half * 8:(half + 1) * 8, t * 128:(t + 1) * 128],
                                                            in_=Tp[half][:].rearrange("p (c t) -> p c t", c=8))),
                   reads=[Tt[half]], writes=[dst_tiles[t]])

    load_ln(0)
    linear(ACT_B, lambda t: [t_Bt[t]], blk_mix, resid_epi)
    for t in range(NT):
        layernorm(t, ACT_A, t_At)
    if DEBUG:
        for t in range(NT):
            dma(SP, d_dbg, dbg_h1[t * 128:(t + 1) * 128, :], RES[:, t, :], src=t_res[t])

    barrier()
    lnS.close()
    if STOP == "2":
        return finish()
    s3 = Scope(hi)
    memT = ACT_B[:, :, 0:256]
    mem_tiles = [t_Bt[0], t_Bt[1]]
    d_mem = dsem("d_mem")
    dma(POOL, d_mem, memT, memT_d.rearrange("p (k m) -> p k m", k=KC), dst=mem_tiles)
    KT = sb(s3, "KT", [128, KC, 256], BF16)
    t_KT = Tile("KT")
    Vm = sb(s3, "Vm", [128, 2, D], BF16)
    t_Vm = Tile("Vm")
    kvb = sb(s3, "kvb", [128, 512], BF16)
    t_kvb = Tile("kvb")
    qb = sb(s3, "qb", [128, 512], BF16)
    t_qb = Tile("qb")
    qTh = sb(s3, "qTh", [128, 4, 128], BF16)
    t_qTh = Tile("qTh")
    pexp = sb(s3, "pexp", [128, 256], BF16)
    t_pexp = Tile("pexp")
    pT = sb(s3, "pT", [128, 2, 128], BF16)
    t_pT = Tile("pT")
    sm = sb(s3, "sm", [128, 8], F32)
    t_sm = Tile("sm")
    ob = sb(s3, "ob", [128, 512], BF16)
    t_ob = Tile("ob")

    def k_epi(j, mt, ai):
        op(ACT, lambda: nc.scalar.copy(out=kvb[:], in_=Fp[ai][:, :]), reads=[Ft[ai]], writes=[t_kvb])
        mmgroup(PE, [lambda c=c: nc.tensor.transpose(Tp[0][:, c * 128:(c + 1) * 128], kvb[:, c * 128:(c + 1) * 128], ident[:])
                     for c in range(4)], reads=[t_kvb, t_ident], writes=[Tt[0]])
        op(DVE, lambda: nc.vector.tensor_copy(out=KT[:, j * 4:(j + 1) * 4, mt * 128:(mt + 1) * 128],
                                              in_=Tp[0][:, 0:512].rearrange("p (c t) -> p c t", c=4)), reads=[Tt[0]], writes=[t_KT])

    def v_epi(j, mt, ai):
        op(ACT, lambda: nc.scalar.copy(out=Vm[:, mt, j * 512:(j + 1) * 512], in_=Fp[ai][:, :]), reads=[Ft[ai]], writes=[t_Vm])

    linear(memT, lambda t: mem_tiles, blk_mk, k_epi, ntiles=2)
    linear(memT, lambda t: mem_tiles, blk_mv, v_epi, ntiles=2)

    SCL = 512.0 ** -0.5

    def q_epi(j, t, ai):
        op(ACT, lambda: nc.scalar.mul(out=qb[:], in_=Fp[ai][:, :], mul=SCL), reads=[Ft[ai]], writes=[t_qb])
        mmgroup(PE, [lambda c=c: nc.tensor.transpose(Tp[0][:, c * 128:(c + 1) * 128], qb[:, c * 128:(c + 1) * 128], ident[:])
                     for c in range(4)], reads=[t_qb, t_ident], writes=[Tt[0]])
        op(DVE, lambda: nc.vector.tensor_copy(out=qTh[:], in_=Tp[0][:, 0:512].rearrange("p (c t) -> p c t", c=4)),
           reads=[Tt[0]], writes=[t_qTh])
        mmgroup(PE, [lambda c=c: nc.tensor.matmul(Fp[2][:, 0:256], lhsT=qTh[:, c, :], rhs=KT[:, j * 4 + c, :], start=(c == 0), stop=(c == 3))
                     for c in range(4)], reads=[t_qTh, t_KT], writes=[Ft[2]])
        op(DVE, lambda: nc.vector.reduce_max(out=sm[:, 0:1], in_=Fp[2][:, 0:256], axis=AX.X), reads=[Ft[2]], writes=[t_sm])
        op(DVE, lambda: nc.vector.tensor_scalar(out=sm[:, 2:3], in0=sm[:, 0:1], scalar1=-1.0, scalar2=None, op0=ALU.mult),
           reads=[t_sm], writes=[t_sm])
        op(DVE, lambda: nc.vector.memset(sm[:, 4:5], 0.0), writes=[t_sm])
        op(ACT, lambda: nc.scalar.activation(out=pexp[:], in_=Fp[2][:, 0:256], func=AF.Exp, bias=sm[:, 2:3], accum_out=sm[:, 4:5]),
           reads=[Ft[2], t_sm], writes=[t_pexp, t_sm])
        op(DVE, lambda: nc.vector.reciprocal(out=sm[:, 6:7], in_=sm[:, 4:5]), reads=[t_sm], writes=[t_sm])
        mmgroup(PE, [lambda c=c: nc.tensor.transpose(Tp[1][:, c * 128:(c + 1) * 128], pexp[:, c * 128:(c + 1) * 128], ident[:])
                     for c in range(2)], reads=[t_pexp, t_ident], writes=[Tt[1]])
        op(DVE, lambda: nc.vector.tensor_copy(out=pT[:], in_=Tp[1][:, 0:256].rearrange("p (c t) -> p c t", c=2)),
           reads=[Tt[1]], writes=[t_pT])
        mmgroup(PE, [lambda mt=mt: nc.tensor.matmul(Fp[3][:, :], lhsT=pT[:, mt, :], rhs=Vm[:, mt, j * 512:(j + 1) * 512],
                                                    start=(mt == 0), stop=(mt == 1)) for mt in range(2)],
                reads=[t_pT, t_Vm], writes=[Ft[3]])
        op(DVE, lambda: nc.vector.tensor_scalar(out=ob[:], in0=Fp[3][:, :], scalar1=sm[:, 6:7], scalar2=None, op0=ALU.mult),
           reads=[Ft[3], t_sm], writes=[t_ob])
        mmgroup(PE, [lambda c=c: nc.tensor.transpose(Tp[0][:, c * 128:(c + 1) * 128], ob[:, c * 128:(c + 1) * 128], ident[:])
                     for c in range(4)], reads=[t_ob, t_ident], writes=[Tt[0]])
        op(DVE, lambda: nc.vector.tensor_copy(out=ACT_B[:, j * 4:(j + 1) * 4, t * 128:(t + 1) * 128],
                                              in_=Tp[0][:, 0:512].rearrange("p (c t) -> p c t", c=4)), reads=[Tt[0]], writes=[t_Bt[t]])

    linear(ACT_A, lambda t: [t_At[t]], blk_mq, q_epi)
    linear(ACT_B, lambda t: [t_Bt[t]], blk_mo, resid_epi)
    barrier()
    s3.close()

    lnS = Scope(hi)
    lnt = sb(lnS, "lnt2", [128, 2, D], F32)
    lnst = sb(lnS, "lnst2", [128, 4, 6], F32)
    lnmv = sb(lnS, "lnmv2", [128, 8], F32)
    hb = sb(lnS, "hb2", [128, D], BF16)
    load_ln(1)
    for t in range(NT):
        layernorm(t, ACT_A, t_At)
    if DEBUG:
        for t in range(NT):
            dma(SP, d_dbg, dbg_h2[t * 128:(t + 1) * 128, :], RES[:, t, :], src=t_res[t])
    barrier()
    lnS.close()
    if STOP == "3":
        return finish()
    s4 = Scope(hi)
    wr = sb(s4, "wr", [128, KC, 36], BF16)
    t_wr = Tile("wr")
    d_wr = dsem("d_wr")
    dma(POOL, d_wr, wr[:], wroute_d.rearrange("(k p) n -> p k n", p=128), dst=t_wr)
    brt = sb(s4, "brt", [128, 36], F32)
    d_wr2 = dsem("d_wr2")
    dma(SP, d_wr2, brt[:], broute_d.partition_broadcast(128))
    t_wr.w = [(d_wr.sem, d_wr.count), (d_wr2.sem, d_wr2.count)]
    lg = sb(s4, "lg", [128, 36], F32)
    t_lg = Tile("lg")
    rs = sb(s4, "rs", [128, 16], F32)
    t_rs = Tile("rs")
    ohg = sb(s4, "ohg", [128, 4], F32)
    f48 = sb(s4, "f48", [128, 32], F32)
    fsel = sb(s4, "fsel", [128, 8], F32)
    oh1 = sb(s4, "oh1", [128, 8], F32)
    oh2 = sb(s4, "oh2", [128, 8], F32)
    fm = sb(s4, "fm", [128, 8], F32)
    gw8 = sb(s4, "gw8", [128, 8], F32)
    Gd = sb(s4, "Gd", [128, NT, 32], F32)
    Md = sb(s4, "Md", [128, NT, 32], F32)
    Mb = sb(s4, "Mb", [128, NT, 32], BF16)
    Ghl = sb(s4, "Ghl", [128, NT, 32, 2], BF16)
    Gtmp = sb(s4, "Gtmp", [128, 32], F32)
    posd = sb(s4, "posd", [128, NT, 32], F32)
    t_rt = [Tile(f"rt{t}") for t in range(NT)]
    t_M = Tile("M")

    H2 = ACT_B[:].rearrange("p k t -> p (k t)").rearrange("p (a d) -> p a d", a=NT)
    for t in range(NT):
        op(ACT, lambda t=t: nc.scalar.copy(out=H2[:, t, :], in_=RES[:, t, :]), reads=[t_res[t]], writes=t_Bt + [t_B])
    for t in range(NT):
        mmgroup(PE, [lambda k=k, t=t: nc.tensor.matmul(Fp[2][:, 0:36], lhsT=ACT_A[:, k, t * 128:(t + 1) * 128], rhs=wr[:, k, :],
                                                       start=(k == 0), stop=(k == KC - 1)) for k in range(KC)],
                reads=[t_At[t], t_wr], writes=[Ft[2]])
        R_ = [t_lg, t_rs]
        op(DVE, lambda: nc.vector.tensor_tensor(out=lg[:], in0=Fp[2][:, 0:36], in1=brt[:], op=ALU.add), reads=[Ft[2], t_wr], writes=[t_lg])
        op(DVE, lambda: nc.vector.reduce_max(out=rs[:, 0:1], in_=lg[:, 0:4], axis=AX.X), reads=[t_lg], writes=[t_rs])
        op(DVE, lambda: nc.vector.tensor_scalar(out=ohg[:], in0=lg[:, 0:4], scalar1=rs[:, 0:1], scalar2=None, op0=ALU.is_equal),
           reads=R_, writes=[t_rs])
        op(DVE, lambda: nc.vector.tensor_scalar(out=rs[:, 14:15], in0=rs[:, 0:1], scalar1=-1.0, scalar2=None, op0=ALU.mult),
           reads=R_, writes=[t_rs])
        op(DVE, lambda: nc.vector.memset(rs[:, 2:3], 0.0), reads=R_, writes=[t_rs])
        op(ACT, lambda: nc.scalar.activation(out=rs[:, 4:8], in_=lg[:, 0:4], func=AF.Exp, bias=rs[:, 14:15], accum_out=rs[:, 2:3]),
           reads=R_, writes=[t_rs])
        op(DVE, lambda: nc.vector.reciprocal(out=rs[:, 3:4], in_=rs[:, 2:3]), reads=R_, writes=[t_rs])
        op(DVE, lambda: nc.vector.tensor_tensor(out=f48[:].rearrange("p (g e) -> p g e", g=4),
                                                in0=lg[:, 4:36].rearrange("p (g e) -> p g e", g=4),
                                                in1=ohg[:].unsqueeze(2).to_broadcast([128, 4, 8]), op=ALU.mult), reads=R_, writes=[t_rs])
        op(DVE, lambda: nc.vector.reduce_sum(out=fsel[:], in_=f48[:].rearrange("p (g e) -> p e g", g=4), axis=AX.X),
           reads=R_, writes=[t_rs])
        op(DVE, lambda: nc.vector.reduce_max(out=rs[:, 8:9], in_=fsel[:], axis=AX.X), reads=R_, writes=[t_rs])
        op(DVE, lambda: nc.vector.tensor_scalar(out=oh1[:], in0=fsel[:], scalar1=rs[:, 8:9], scalar2=None, op0=ALU.is_equal),
           reads=R_, writes=[t_rs])
        op(DVE, lambda: nc.vector.scalar_tensor_tensor(out=fm[:], in0=oh1[:], scalar=-1e30, in1=fsel[:], op0=ALU.mult, op1=ALU.add),
           reads=R_, writes=[t_rs])
        op(DVE, lambda: nc.vector.reduce_max(out=rs[:, 9:10], in_=fm[:], axis=AX.X), reads=R_, writes=[t_rs])
        op(DVE, lambda: nc.vector.tensor_scalar(out=oh2[:], in0=fm[:], scalar1=rs[:, 9:10], scalar2=None, op0=ALU.is_equal),
           reads=R_, writes=[t_rs])
        op(DVE, lambda: nc.vector.tensor_tensor(out=rs[:, 10:11], in0=rs[:, 8:9], in1=rs[:, 9:10], op=ALU.subtract), reads=R_, writes=[t_rs])
        op(ACT, lambda: nc.scalar.activation(out=rs[:, 11:12], in_=rs[:, 10:11], func=AF.Sigmoid), reads=R_, writes=[t_rs])
        op(DVE, lambda: nc.vector.tensor_tensor(out=rs[:, 12:13], in0=rs[:, 11:12], in1=rs[:, 3:4], op=ALU.mult), reads=R_, writes=[t_rs])
        op(DVE, lambda: nc.vector.tensor_tensor(out=rs[:, 13:14], in0=rs[:, 3:4], in1=rs[:, 12:13], op=ALU.subtract), reads=R_, writes=[t_rs])
        op(DVE, lambda: nc.vector.tensor_scalar(out=gw8[:], in0=oh1[:], scalar1=rs[:, 12:13], scalar2=None, op0=ALU.mult), reads=R_, writes=[t_rs])
        op(DVE, lambda: nc.vector.scalar_tensor_tensor(out=gw8[:], in0=oh2[:], scalar=rs[:, 13:14], in1=gw8[:], op0=ALU.mult, op1=ALU.add),
           reads=R_, writes=[t_rs])
        op(DVE, lambda t=t: nc.vector.tensor_tensor(out=Gd[:, t, :].rearrange("p (g e) -> p g e", g=4),
                                                    in0=ohg[:].unsqueeze(2).to_broadcast([128, 4, 8]),
                                                    in1=gw8[:].unsqueeze(1).to_broadcast([128, 4, 8]), op=ALU.mult), reads=R_, writes=[t_rt[t]])
        op(DVE, lambda: nc.vector.tensor_tensor(out=oh1[:], in0=oh1[:], in1=oh2[:], op=ALU.add), reads=R_, writes=[t_rs])
        op(DVE, lambda t=t: nc.vector.tensor_tensor(out=Md[:, t, :].rearrange("p (g e) -> p g e", g=4),
                                                    in0=ohg[:].unsqueeze(2).to_broadcast([128, 4, 8]),
                                                    in1=oh1[:].unsqueeze(1).to_broadcast([128, 4, 8]), op=ALU.mult), reads=R_, writes=[t_rt[t]])
        op(DVE, lambda t=t: nc.vector.tensor_copy(out=Mb[:, t, :], in_=Md[:, t, :]), reads=[t_rt[t]], writes=[t_rt[t]])
        op(DVE, lambda t=t: nc.vector.tensor_copy(out=Ghl[:, t, :, 0], in_=Gd[:, t, :]), reads=[t_rt[t]], writes=[t_rt[t]])
        op(DVE, lambda t=t: nc.vector.tensor_tensor(out=Gtmp[:], in0=Gd[:, t, :], in1=Ghl[:, t, :, 0], op=ALU.subtract),
           reads=[t_rt[t]], writes=[t_rs])
        op(DVE, lambda t=t: nc.vector.tensor_copy(out=Ghl[:, t, :, 1], in_=Gtmp[:]), reads=[t_rs], writes=[t_rt[t]])
        op(POOL, lambda t=t: nc.gpsimd.tensor_scalar(out=RES[:, t, :], in0=RES[:, t, :], scalar1=ALPHA, scalar2=None, op0=ALU.mult),
           reads=[t_res[t], t_Bt[t]], writes=[t_res[t]])
    for t in range(NT):
        fns = [lambda tp=tp: nc.tensor.matmul(Fp[2][:, 0:32], lhsT=CTb[:, 0:128], rhs=Mb[:, tp, :], start=(tp == 0), stop=False)
               for tp in range(t)]
        fns.append(lambda t=t: nc.tensor.matmul(Fp[2][:, 0:32], lhsT=CTb[:, 128:256], rhs=Mb[:, t, :], start=(t == 0), stop=True))
        mmgroup(PE, fns, reads=[t_rt[tp] for tp in range(t + 1)] + [t_ctb], writes=[Ft[2]])
        op(DVE, lambda t=t: nc.vector.tensor_copy(out=posd[:, t, :], in_=Fp[2][:, 0:32]), reads=[Ft[2]], writes=[t_rt[t]])

    Psel = sb(s4, "Psel", [128, NT, CAP], BF16)
    t_Psel = Tile("Psel")
    PselT = [sb(s4, f"PselT{i}", [128, NT, 128], BF16) for i in range(2)]
    t_PselT = [Tile(f"PselT{i}") for i in range(2)]
    xeT = sb(s4, "xeT", [128, KC, CAP], BF16)
    t_xeT = Tile("xeT")
    gs = sb(s4, "gs", [128, 2], F32)
    t_gs = Tile("gs")
    hg = sb(s4, "hg", [128, 512], BF16)
    t_hg = Tile("hg")
    hid = sb(s4, "hid", [128, 512], BF16)
    t_hid = Tile("hid")
    hidT = sb(s4, "hidT", [128, 4, CAP], BF16)
    t_hidT = Tile("hidT")
    yb = [sb(s4, f"yb{i}", [128, D], BF16) for i in range(2)]
    t_yb = [Tile(f"yb{i}") for i in range(2)]
    t_rt_all = t_rt

    for e in range(NEXP):
        gB, uB, dB = blk_e[e]
        pi = e % 2
        for t in range(NT):
            op(DVE, lambda t=t, e=e: nc.vector.tensor_scalar(out=Psel[:, t, :], in0=C("IOTA"), scalar1=posd[:, t, e:e + 1],
                                                             scalar2=Md[:, t, e:e + 1], op0=ALU.is_equal, op1=ALU.mult),
               reads=[t_rt[t], t_const], writes=[t_Psel])
        mmgroup(PE, [lambda t=t: nc.tensor.transpose(Tp[0][:, t * 128:(t + 1) * 128], Psel[:, t, :], ident[:]) for t in range(NT)],
                reads=[t_Psel, t_ident], writes=[Tt[0]])
        op(ACT, lambda pi=pi: nc.scalar.copy(out=PselT[pi][:], in_=Tp[0][:, 0:NT * 128].rearrange("p (a t) -> p a t", a=NT)),
           reads=[Tt[0]], writes=[t_PselT[pi]])
        mmgroup(PE, [lambda t=t, e=e: nc.tensor.matmul(Fp[5][:, 0:2], lhsT=Psel[:, t, :], rhs=Ghl[:, t, e, :], start=(t == 0), stop=(t == NT - 1))
                     for t in range(NT)], reads=[t_Psel] + t_rt_all, writes=[Ft[5]])
        op(DVE, lambda: nc.vector.reduce_sum(out=gs[:, 0:1], in_=Fp[5][:, 0:2], axis=AX.X), reads=[Ft[5]], writes=[t_gs])
        for kq in range(4):
            ai = kq % 2
            fns = []
            for kk in range(4):
                k = kq * 4 + kk
                for t in range(NT):
                    fns.append(lambda k=k, kk=kk, t=t: nc.tensor.matmul(Fp[ai][:, kk * 128:(kk + 1) * 128], lhsT=H2[:, t, k * 128:(k + 1) * 128],
                                                                        rhs=Psel[:, t, :], start=(t == 0), stop=(t == NT - 1)))
            mmgroup(PE, fns, reads=[t_Psel, t_B], writes=[Ft[ai]])
            op(ACT if kq % 2 == 0 else DVE,
               (lambda kq=kq, ai=ai: nc.scalar.copy(out=xeT[:, kq * 4:(kq + 1) * 4, :], in_=Fp[ai][:, :].rearrange("p (c s) -> p c s", c=4)))
               if kq % 2 == 0 else
               (lambda kq=kq, ai=ai: nc.vector.tensor_copy(out=xeT[:, kq * 4:(kq + 1) * 4, :], in_=Fp[ai][:, :].rearrange("p (c s) -> p c s", c=4))),
               reads=[Ft[ai]], writes=[t_xeT])
        Wg, Wgt = wget(gB)
        mmgroup(PE, [lambda k=k: nc.tensor.matmul(Fp[2][:, :], lhsT=xeT[:, k, :], rhs=Wg[:, k, :], start=(k == 0), stop=(k == KC - 1))
                     for k in range(KC)], reads=[t_xeT, Wgt], writes=[Ft[2]])
        Wu, Wut = wget(uB)
        mmgroup(PE, [lambda k=k: nc.tensor.matmul(Fp[3][:, :], lhsT=xeT[:, k, :], rhs=Wu[:, k, :], start=(k == 0), stop=(k == KC - 1))
                     for k in range(KC)], reads=[t_xeT, Wut], writes=[Ft[3]])
        op(ACT, lambda: nc.scalar.activation(out=hg[:], in_=Fp[2][:, :], func=AF.Silu), reads=[Ft[2]], writes=[t_hg])
        op(DVE, lambda: nc.vector.tensor_tensor(out=hid[:], in0=hg[:], in1=Fp[3][:, :], op=ALU.mult), reads=[t_hg, Ft[3]], writes=[t_hid])
        mmgroup(PE, [lambda c=c: nc.tensor.transpose(Tp[1][:, c * 128:(c + 1) * 128], hid[:, c * 128:(c + 1) * 128], ident[:])
                     for c in range(4)], reads=[t_hid, t_ident], writes=[Tt[1]])
        op(ACT, lambda: nc.scalar.copy(out=hidT[:], in_=Tp[1][:, 0:512].rearrange("p (c s) -> p c s", c=4)), reads=[Tt[1]], writes=[t_hidT])
        Wd, Wdt = wget(dB)
        for cb in range(4):
            ai = cb % 2
            mmgroup(PE, [lambda c=c, cb=cb: nc.tensor.matmul(Fp[ai][:, :], lhsT=hidT[:, c, :], rhs=Wd[:, c, cb * 512:(cb + 1) * 512],
                                                             start=(c == 0), stop=(c == 3)) for c in range(4)],
                    reads=[t_hidT, Wdt], writes=[Ft[ai]])
            op(DVE, lambda cb=cb, ai=ai, pi=pi: nc.vector.tensor_scalar(out=yb[pi][:, cb * 512:(cb + 1) * 512], in0=Fp[ai][:, :],
                                                                        scalar1=gs[:, 0:1], scalar2=None, op0=ALU.mult),
               reads=[Ft[ai], t_gs], writes=[t_yb[pi]])
        if e % 2 == 1:
            for t in range(NT):
                for cb in range(4):
                    ai = 4 + (t * 4 + cb) % 2
                    mmgroup(PE, [lambda q=q, t=t, cb=cb: nc.tensor.matmul(Fp[ai][:, :], lhsT=PselT[q][:, t, :], rhs=yb[q][:, cb * 512:(cb + 1) * 512],
                                                                          start=(q == 0), stop=(q == 1)) for q in range(2)],
                            reads=t_PselT + t_yb, writes=[Ft[ai]])
                    op(DVE, (lambda t=t, cb=cb, ai=ai: nc.vector.tensor_tensor(out=RES[:, t, cb * 512:(cb + 1) * 512],
                                                                               in0=RES[:, t, cb * 512:(cb + 1) * 512],
                                                                               in1=Fp[ai][:, :], op=ALU.add)),
                       reads=[Ft[ai], t_res[t]], writes=[t_res[t]])

    barrier()
    s4.close()
    lnS = Scope(hi)
    lnt = sb(lnS, "lnt3", [128, 2, D], F32)
    lnst = sb(lnS, "lnst3", [128, 4, 6], F32)
    lnmv = sb(lnS, "lnmv3", [128, 8], F32)
    hb = sb(lnS, "hb3", [128, D], BF16)
    load_ln(2)
    d_out = dsem("d_out")
    for t in range(NT):
        layernorm(t, None, None)
        dma(SP, d_out, out_d[t * 128:(t + 1) * 128, :], RES[:, t, :], src=t_res[t])
    SP.wait([(d_out.sem, d_out.count)])
    if DEBUG:
        SP.wait([(d_dbg.sem, d_dbg.count)])
    barrier()
    lnS.close()
    s2.close()
    es.close()
    return nc


def kernel(x, mem, positions, w_in, w_gla_a2, b_gla_a, g_gla_norm, w_mix_out, ln1_g, ln1_b,
           w_mq, w_mk, w_mv, w_mo, ln2_g, ln2_b, w_route_group, b_route_group,
           w_route_expert, b_route_expert, w_exp_gate, w_exp_up, w_exp_down, ln3_g, ln3_b):
    f = lambda a: np.ascontiguousarray(np.asarray(a))
    x = f(x)[0][:SEQ]
    pos = f(positions)[0].astype(np.int32)[:SEQ]
    shared = {
        "consts": CONST_ARR, "consts1": CONST1_ARR,
        "w_in": _perm_w_in(f(w_in)[0]),
        "wa2b": np.ascontiguousarray(np.concatenate([f(w_gla_a2)[0], f(b_gla_a)[0][None, :]], axis=0)),
        "gnorm": f(g_gla_norm)[0][None, :].copy(),
        "w_mix": f(w_mix_out)[0], "w_mq": f(w_mq)[0], "w_mk": f(w_mk)[0], "w_mv": f(w_mv)[0], "w_mo": f(w_mo)[0],
        "memT": np.ascontiguousarray(f(mem)[0].reshape(256, KC, 128).transpose(2, 1, 0).reshape(128, KC * 256)),
        "ln": np.ascontiguousarray(np.stack([f(ln1_g)[0], f(ln1_b)[0], f(ln2_g)[0], f(ln2_b)[0], f(ln3_g)[0], f(ln3_b)[0]])),
        "wroute": np.ascontiguousarray(np.concatenate([f(w_route_group)[0], f(w_route_expert)[0]], axis=1)),
        "broute": np.ascontiguousarray(np.concatenate([f(b_route_group)[0], f(b_route_expert)[0].reshape(-1)])[None, :]),
        "w_eg": f(w_exp_gate)[0], "w_eu": f(w_exp_up)[0], "w_ed": f(w_exp_down)[0],
    }
    in_maps = []
    for c in range(NCORE):
        npad = (NCORE - 1 - c) * TOK
        xs = np.concatenate([np.zeros((npad, D), np.float32), x[0:(c + 1) * TOK]], axis=0)
        ps = np.concatenate([np.zeros((npad,), np.int32), pos[0:(c + 1) * TOK]], axis=0)
        xTp = np.ascontiguousarray(xs.reshape(NPRE + NT, 128, KC, 128).transpose(0, 3, 2, 1)).reshape(NPRE + NT, 128, D)
        m = dict(shared)
        m["xTp"] = xTp
        m["xown"] = np.ascontiguousarray(x[c * TOK:(c + 1) * TOK])
        m["posT"] = np.ascontiguousarray(ps.reshape(NPRE + NT, 128).T)
        in_maps.append(m)
    if "nc" not in _CACHE:
        _CACHE["nc"] = build_program()
    in_maps = [{k: v for k, v in m.items() if k in DECLARED} for m in in_maps]
    res = run_bass_kernel_spmd(_CACHE["nc"], in_maps, core_ids=list(range(NCORE)))
    if DEBUG:
        _CACHE["dbg"] = res.results
    out = np.concatenate([np.asarray(r["out"]) for r in res.results], axis=0).astype(np.float32)
    return out.reshape(1, SEQ, D)
```
